# Optimizing a Trainium2 kernel written in Bass

```python
import jax, jax.numpy as jnp
from jax import lax
import numpy as np

D_MODEL = 1024
BATCH = 4
SEQ = 8192
DEPTH = 4

N_MIXERS = 3
RMS_EPS = 1e-6
LN_EPS = 1e-5
CONV_WIDTH = 3
SGU_CHUNK = 128
SGU_HALF = 2 * D_MODEL
SGU_GROUPS = 8
SGU_GROUP_DIM = SGU_HALF // SGU_GROUPS
RET_HEADS = 4
RET_QK_DIM = D_MODEL // RET_HEADS
RET_V_DIM = 2 * D_MODEL // RET_HEADS
RET_CHUNK = 128
ROPE_BASE = 10000.0
D_FF = -(-8 * D_MODEL // (3 * 256)) * 256
N_CONV = (DEPTH + 2) // 3
N_SGU = (DEPTH + 1) // 3
N_RET = DEPTH // 3

kernel_name = "hybrid_conv_sgu_retention_trunk"


def rmsnorm(x, g):
    xf = x.astype(jnp.float32)
    y = xf * lax.rsqrt(jnp.mean(xf * xf, axis=-1, keepdims=True) + RMS_EPS)
    return (y * g.astype(jnp.float32)).astype(x.dtype)


def layernorm(x, g, b):
    xf = x.astype(jnp.float32)
    mu = jnp.mean(xf, axis=-1, keepdims=True)
    xc = xf - mu
    y = xc * lax.rsqrt(jnp.mean(xc * xc, axis=-1, keepdims=True) + LN_EPS)
    return (y * g.astype(jnp.float32) + b.astype(jnp.float32)).astype(x.dtype)


def short_conv_mixer(h, w_in, conv_w, w_out):
    bch = h @ w_in
    b_gate, c_gate, z = jnp.split(bch, 3, axis=-1)
    cz = c_gate * z
    conv = lax.conv_general_dilated(
        cz, conv_w[:, None, :].astype(cz.dtype), window_strides=(1,),
        padding=[(CONV_WIDTH - 1, 0)], dimension_numbers=("NWC", "WIO", "NWC"),
        feature_group_count=D_MODEL)
    return (b_gate * conv) @ w_out


def chunked_sgu_mixer(h, w_in, ln_g, ln_b, w_s, b_s, w_out):
    bsz, seq = h.shape[0], h.shape[1]
    n_chunks = seq // SGU_CHUNK
    zz = jax.nn.gelu(h @ w_in, approximate=False)
    u, v = jnp.split(zz, 2, axis=-1)
    v = layernorm(v, ln_g, ln_b)
    v = v.reshape(bsz, n_chunks, SGU_CHUNK, SGU_GROUPS, SGU_GROUP_DIM)
    causal = jnp.tril(jnp.ones((SGU_CHUNK, SGU_CHUNK), dtype=bool))
    w = jnp.where(causal[None], w_s, jnp.zeros((), w_s.dtype))
    s = jnp.einsum("gts,bcsgd->bctgd", w, v) + b_s.T[None, None, :, :, None]
    s = s.reshape(bsz, seq, SGU_HALF)
    return (u * s) @ w_out


def rotary(x, cos, sin):
    half = x.shape[-1] // 2
    x1, x2 = x[..., :half], x[..., half:]
    return jnp.concatenate([x1 * cos - x2 * sin, x2 * cos + x1 * sin], axis=-1)


def retention_mixer(h, w_in, w_out):
    bsz, seq = h.shape[0], h.shape[1]
    n_chunks = seq // RET_CHUNK
    proj = h @ w_in
    q, k, v, g = jnp.split(proj, [D_MODEL, 2 * D_MODEL, 4 * D_MODEL], axis=-1)
    q = q.astype(jnp.float32).reshape(bsz, seq, RET_HEADS, RET_QK_DIM).transpose(0, 2, 1, 3)
    k = k.astype(jnp.float32).reshape(bsz, seq, RET_HEADS, RET_QK_DIM).transpose(0, 2, 1, 3)
    v = v.astype(jnp.float32).reshape(bsz, seq, RET_HEADS, RET_V_DIM).transpose(0, 2, 1, 3)
    half = RET_QK_DIM // 2
    pos = jnp.arange(seq, dtype=jnp.float32)
    inv_freq = ROPE_BASE ** (-(jnp.arange(half, dtype=jnp.float32) / half))
    ang = pos[:, None] * inv_freq[None, :]
    cos, sin = jnp.cos(ang), jnp.sin(ang)
    q = rotary(q, cos, sin)
    k = rotary(k, cos, sin) * (RET_QK_DIM ** -0.5)
    log_gamma = jnp.log(1.0 - 2.0 ** (-5.0 - jnp.arange(RET_HEADS, dtype=jnp.float32)))
    idx = jnp.arange(RET_CHUNK, dtype=jnp.float32)
    diff = idx[:, None] - idx[None, :]
    d_inner = jnp.where(diff[None] >= 0,
                        jnp.exp(jnp.maximum(diff, 0.0)[None] * log_gamma[:, None, None]), 0.0)
    k_decay = jnp.exp((RET_CHUNK - 1 - idx)[None, :] * log_gamma[:, None])
    q_decay = jnp.exp((idx + 1.0)[None, :] * log_gamma[:, None])
    chunk_decay = jnp.exp(RET_CHUNK * log_gamma)
    qc = q.reshape(bsz, RET_HEADS, n_chunks, RET_CHUNK, RET_QK_DIM)
    kc = k.reshape(bsz, RET_HEADS, n_chunks, RET_CHUNK, RET_QK_DIM)
    vc = v.reshape(bsz, RET_HEADS, n_chunks, RET_CHUNK, RET_V_DIM)
    scores = jnp.einsum("bhcjd,bhcmd->bhcjm", qc, kc) * d_inner[None, :, None]
    inner = jnp.einsum("bhcjm,bhcme->bhcje", scores, vc)

    def step(state, inp):
        q_i, k_i, v_i = inp
        cross = jnp.einsum("bhjd,bhde->bhje", q_i, state) * q_decay[None, :, :, None]
        new_state = state * chunk_decay[None, :, None, None] + jnp.einsum(
            "bhmd,bhme->bhde", k_i * k_decay[None, :, :, None], v_i)
        return new_state, cross

    state0 = jnp.zeros((bsz, RET_HEADS, RET_QK_DIM, RET_V_DIM), jnp.float32)
    _, cross = lax.scan(step, state0, (jnp.moveaxis(qc, 2, 0), jnp.moveaxis(kc, 2, 0), jnp.moveaxis(vc, 2, 0)))
    ret = inner + jnp.moveaxis(cross, 0, 2)
    o = ret.reshape(bsz, RET_HEADS, seq, RET_V_DIM).transpose(0, 2, 1, 3)
    o = o * lax.rsqrt(jnp.mean(o * o, axis=-1, keepdims=True) + RMS_EPS)
    o = o.reshape(bsz, seq, RET_HEADS * RET_V_DIM)
    y = jax.nn.silu(g.astype(jnp.float32)) * o
    return y.astype(h.dtype) @ w_out


def swiglu_ffn(h, w_gate, w_up, w_down):
    return (jax.nn.silu(h @ w_gate) * (h @ w_up)) @ w_down


def setup_inputs(seed: int = 0) -> dict:
    key = jax.random.key(seed)
    ks = jax.random.split(key, 20)
    f32 = jnp.float32
    nrm = lambda k, shape, scale: jax.random.normal(k, shape, f32) * scale
    D = D_MODEL
    return {
        "x": nrm(ks[0], (BATCH, SEQ, D), 1.0),
        "norm_mix_g": 1.0 + nrm(ks[1], (DEPTH, D), 0.01),
        "norm_ffn_g": 1.0 + nrm(ks[2], (DEPTH, D), 0.01),
        "final_norm_g": 1.0 + nrm(ks[3], (D,), 0.01),
        "conv_w_in": nrm(ks[4], (N_CONV, D, 3 * D), D ** -0.5),
        "conv_w": nrm(ks[5], (N_CONV, CONV_WIDTH, D), CONV_WIDTH ** -0.5),
        "conv_w_out": nrm(ks[6], (N_CONV, D, D), D ** -0.5),
        "sgu_w_in": nrm(ks[7], (N_SGU, D, 2 * SGU_HALF), D ** -0.5),
        "sgu_ln_g": 1.0 + nrm(ks[8], (N_SGU, SGU_HALF), 0.01),
        "sgu_ln_b": nrm(ks[9], (N_SGU, SGU_HALF), 0.01),
        "sgu_w_s": nrm(ks[10], (N_SGU, SGU_GROUPS, SGU_CHUNK, SGU_CHUNK), SGU_CHUNK ** -0.5),
        "sgu_b_s": 1.0 + nrm(ks[11], (N_SGU, SGU_GROUPS, SGU_CHUNK), 0.1),
        "sgu_w_out": nrm(ks[12], (N_SGU, SGU_HALF, D), SGU_HALF ** -0.5),
        "ret_w_in": nrm(ks[13], (N_RET, D, 6 * D), D ** -0.5),
        "ret_w_out": nrm(ks[14], (N_RET, 2 * D, D), (2 * D) ** -0.5),
        "ffn_w_gate": nrm(ks[15], (DEPTH, D, D_FF), D ** -0.5),
        "ffn_w_up": nrm(ks[16], (DEPTH, D, D_FF), D ** -0.5),
        "ffn_w_down": nrm(ks[17], (DEPTH, D_FF, D), D_FF ** -0.5),
    }


def reference(x, norm_mix_g, norm_ffn_g, final_norm_g, conv_w_in, conv_w, conv_w_out,
              sgu_w_in, sgu_ln_g, sgu_ln_b, sgu_w_s, sgu_b_s, sgu_w_out,
              ret_w_in, ret_w_out, ffn_w_gate, ffn_w_up, ffn_w_down):
    for i in range(DEPTH):
        kind = i % N_MIXERS
        j = i // N_MIXERS
        h = rmsnorm(x, norm_mix_g[i])
        if kind == 0:
            m = short_conv_mixer(h, conv_w_in[j], conv_w[j], conv_w_out[j])
        elif kind == 1:
            m = chunked_sgu_mixer(h, sgu_w_in[j], sgu_ln_g[j], sgu_ln_b[j], sgu_w_s[j], sgu_b_s[j], sgu_w_out[j])
        else:
            m = retention_mixer(h, ret_w_in[j], ret_w_out[j])
        x = x + m
        h = rmsnorm(x, norm_ffn_g[i])
        x = x + swiglu_ffn(h, ffn_w_gate[i], ffn_w_up[i], ffn_w_down[i])
    return rmsnorm(x, final_norm_g)
```

```python
import numpy as np
from contextlib import ExitStack
import concourse.bass as bass
import concourse.mybir as mybir
from concourse.bass_utils import run_bass_kernel_spmd

F32 = mybir.dt.float32
BF16 = mybir.dt.bfloat16
AF = mybir.ActivationFunctionType
ALU = mybir.AluOpType

D = 1024
KC = 8
DFF = 2816
FC = 22
RMS_EPS = 1e-6
LN_EPS = 1e-5
HEADS = 4
SEQ = 8192
BATCH = 4
COMPUTE = ("pe", "act", "dve")


class T:
    __slots__ = ("ap", "keys")

    def __init__(self, ap, keys):
        self.ap = ap
        self.keys = tuple(keys)


class Rec:
    def __init__(self, nc, es):
        self.nc = nc
        self.es = es
        self.ops = {e: [] for e in ("pe", "act", "dve", "pool", "sp")}
        self.sem = {}
        self.cnt = {}
        for e in COMPUTE:
            self.sem[e] = es.enter_context(nc.semaphore("s_" + e))
            self.cnt[e] = 0
        self.waited = {}
        self.lastw = {}
        self.readers = {}

    def stream(self, name):
        self.sem[name] = self.es.enter_context(self.nc.semaphore(name))
        self.cnt[name] = 0
        return name

    def _need(self, waits, eng, prod, val):
        if prod == eng and eng == "pe":
            return
        k = (eng, prod)
        if self.waited.get(k, 0) >= val:
            return
        self.waited[k] = val
        waits.append((self.sem[prod], val * (1 if prod in COMPUTE else 16)))

    def emit(self, eng, fn, reads, writes, prod=None, n=1):
        prod = prod or eng
        waits = []
        for t in reads:
            for k in t.keys:
                lw = self.lastw.get(k)
                if lw:
                    self._need(waits, eng, *lw)
        for t in writes:
            for k in t.keys:
                lw = self.lastw.get(k)
                if lw:
                    self._need(waits, eng, *lw)
                for p, v in self.readers.get(k, {}).items():
                    self._need(waits, eng, p, v)
        self.cnt[prod] += n
        val = self.cnt[prod]
        for t in writes:
            for k in t.keys:
                self.lastw[k] = (prod, val)
                self.readers[k] = {}
        for t in reads:
            for k in t.keys:
                self.readers.setdefault(k, {})[prod] = val
        self.ops[eng].append((waits, fn, self.sem[prod], 1 if prod in COMPUTE else 16))

    def final_wait(self, eng, prods):
        waits = [(self.sem[p], self.cnt[p] * (1 if p in COMPUTE else 16)) for p in prods if self.cnt[p]]
        self.ops[eng].append((waits, None, None, 0))

    def run(self, block):
        def mk(e):
            def body(eng):
                for waits, fn, sem, inc in self.ops[e]:
                    for sm, v in waits:
                        eng.wait_ge(sm, v)
                    if fn is None:
                        continue
                    r = fn(eng)
                    if isinstance(r, (list, tuple)):
                        for ins in r:
                            ins.then_inc(sem, inc)
                    else:
                        r.then_inc(sem, inc)
            return body
        block.tensor(mk("pe"))
        block.scalar(mk("act"))
        block.vector(mk("dve"))
        block.gpsimd(mk("pool"))
        block.sync(mk("sp"))


def build(npre, nseg, SUB):
    G = SUB * 512
    NCH = G // 128
    NG = npre + nseg
    NTOK = NG * G
    RN = 16384 * SUB
    nc = bass.Bass("TRN2", target_bir_lowering=False)

    def din(name, shape):
        return nc.dram_tensor(name, list(shape), F32, kind="ExternalInput").ap()

    x_d = din("x", [NTOK, D])
    nmg_d = din("norm_mix_g", [4, D])
    nfg_d = din("norm_ffn_g", [4, D])
    fng_d = din("final_norm_g", [D])
    cwin_d = din("conv_w_in", [2, D, 3 * D])
    cw_d = din("conv_w", [2, 3, D])
    cwout_d = din("conv_w_out", [2, D, D])
    swin_d = din("sgu_w_in", [1, D, 4096])
    slg_d = din("sgu_ln_g", [1, 2048])
    slb_d = din("sgu_ln_b", [1, 2048])
    sws_d = din("sgu_w_s", [1, 8, 128, 128])
    sbs_d = din("sgu_b_s", [1, 8, 128])
    swout_d = din("sgu_w_out", [1, 2048, D])
    rwin_d = din("ret_w_in", [1, D, 6 * D])
    rwout_d = din("ret_w_out", [1, 2048, D])
    fwg_d = din("ffn_w_gate", [4, D, DFF])
    fwu_d = din("ffn_w_up", [4, D, DFF])
    fwd_d = din("ffn_w_down", [4, DFF, D])
    cos_d = din("t_cos", [128, NTOK])
    sin_d = din("t_sin", [128, NTOK])
    dT_d = din("t_dT", [128, 512])
    kdec_d = din("t_kdec", [128, 1024])
    qdec_d = din("t_qdec", [128, 4])
    id_d = din("t_ident", [128, 128])
    tri_d = din("t_tri", [128, 128])
    y_d = nc.dram_tensor("y", [nseg * G, D], F32, kind="ExternalOutput").ap()
    C_dram = nc.dram_tensor("c_scr", [128, 2048], F32).ap()

    gam = [1.0 - 2.0 ** (-5.0 - h) for h in range(HEADS)]
    cdec = [float(np.exp(128.0 * np.log(g_))) for g_ in gam]

    with ExitStack() as es:
        def sb(name, shape, dt):
            return es.enter_context(nc.sbuf_tensor(name, list(shape), dt))

        def ps(name, shape, dt):
            return es.enter_context(nc.psum_tensor(name, list(shape), dt))

        xT = sb("xT", [128, KC, G], F32)
        hT = sb("hT", [128, KC, G], BF16)
        R = sb("R", [128, RN], BF16)
        S32 = sb("S32", [128, 8, 512], F32)
        Sbf = sb("Sbf", [128, 8, 512], BF16)
        NW = 6
        wbuf = sb("wbuf", [128, NW, 2048], BF16)
        mixc = sb("mixc", [128, 4096], F32)
        sq = sb("sq", [128, 4, 512], BF16)
        rstd = sb("rstd", [128, 2, 512], F32)
        NTMP = 4
        tmp = sb("tmp", [128, NTMP, 512], F32)
        kd = sb("kd", [128, 2, 1024], BF16)
        sTb = sb("sTb", [128, 2, 512], BF16)
        vhb = sb("vhb", [128, 2, 2048], BF16) if SUB == 1 else None
        WT = sb("WT", [128, 8, 128], BF16)
        ident = sb("ident", [128, 128], F32)
        identb = sb("identb", [128, 128], BF16)
        onesb = sb("onesb", [128, 128], BF16)
        gT = sb("gT", [128, KC, 32], F32)
        halo = sb("halo", [128, 2, KC, 2], F32)
        qdec = sb("qdec", [128, 4], F32)
        st = sb("st", [128, 64], F32)
        onbuf = sb("onbuf", [128, 4, 512], BF16)

        NB = 8
        pb = [ps("pb%d" % i, [128, 512], F32) for i in range(NB)]

        rec = Rec(nc, es)
        for nm in ["w0", "w1", "w2", "w3", "w4", "w5", "xs0", "xs1", "os0", "os1", "cst", "mc", "cs"]:
            rec.stream(nm)

        def rkeys(off, n):
            return [("R", i) for i in range(off // 512, (off + n - 1) // 512 + 1)]

        def Rb(off, n):
            return T(R[:, off:off + n], rkeys(off, n))

        def Rf(off, n):
            return T(R[:, off:off + 2 * n].bitcast(F32), rkeys(off, 2 * n))

        bank_i = [0]

        def nb():
            i = bank_i[0]
            bank_i[0] = (i + 1) % NB
            return T(pb[i][:], [("pb", i)])

        tb_i = [0]

        def ntb():
            b = nb()
            return T(b.ap.bitcast(BF16), b.keys)

        tmp_i = [0]

        def ntmp():
            i = tmp_i[0]
            tmp_i[0] = (i + 1) % NTMP
            return T(tmp[:, i, :], [("tmp", i)])

        sq_i = [0]

        def nsq():
            i = sq_i[0]
            sq_i[0] = (i + 1) % 4
            return T(sq[:, i, :], [("sq", i)])

        def V(ap, *keys):
            return T(ap, keys)

        def sub_of(t0):
            return t0 // 512

        def xv(kc, s):
            return T(xT[:, kc, s * 512:(s + 1) * 512], [("x", kc, s)])

        def hv(kc, s):
            return T(hT[:, kc, s * 512:(s + 1) * 512], [("h", kc, s)])

        def hvc(kc, ch):
            return T(hT[:, kc, ch * 128:(ch + 1) * 128], [("h", kc, ch // 4)])

        PE_N = [0]
        CS = [list(range(SUB))]

        def mark(label):
            MARKS.append((PE_N[0], label))

        def pe(instrs, reads, writes):
            instrs = list(instrs)
            def fn(eng):
                last = None
                for it in instrs:
                    if it[0] == "mm":
                        _, o, l, r, s0, s1 = it
                        last = eng.matmul(out=o, lhsT=l, rhs=r, start=s0, stop=s1)
                    else:
                        _, o, i_, idn = it
                        last = eng.transpose(out=o, in_=i_, identity=idn)
                return last
            rec.emit("pe", fn, reads, writes)
            PE_N[0] += len(instrs)

        def mm(out, pairs, split=False):
            n = len(pairs)
            if split:
                for i, (l, r) in enumerate(pairs):
                    pe([("mm", out.ap, l.ap, r.ap, i == 0, i == n - 1)], [l, r], [out])
                return
            instrs = [("mm", out.ap, l.ap, r.ap, i == 0, i == n - 1) for i, (l, r) in enumerate(pairs)]
            reads = [t for p in pairs for t in p]
            pe(instrs, reads, [out])

        def act(out, in_, func, bias=None, scale=None, accum=None, extra_reads=()):
            kw = {}
            if bias is not None:
                kw["bias"] = bias
            if scale is not None:
                kw["scale"] = scale
            if accum is not None:
                kw["accum_out"] = accum.ap
            rec.emit("act", lambda e: e.activation(out=out.ap, in_=in_.ap, func=func, **kw),
                     [in_] + list(extra_reads), [out] + ([accum] if accum is not None else []))

        def tt(out, a, b, op, eng="dve"):
            rec.emit(eng, lambda e: e.tensor_tensor(out=out.ap, in0=a.ap, in1=b.ap, op=op), [a, b], [out])

        def ts(out, a, s1, s2, op0, op1=None, extra_reads=(), eng="dve"):
            if op1 is None:
                rec.emit(eng, lambda e: e.tensor_scalar(out=out.ap, in0=a.ap, scalar1=s1, scalar2=None, op0=op0),
                         [a] + list(extra_reads), [out])
            else:
                rec.emit(eng, lambda e: e.tensor_scalar(out=out.ap, in0=a.ap, scalar1=s1, scalar2=s2, op0=op0, op1=op1),
                         [a] + list(extra_reads), [out])

        def stt(out, a, sc, b, op0, op1, extra_reads=(), eng="dve"):
            rec.emit(eng, lambda e: e.scalar_tensor_tensor(out=out.ap, in0=a.ap, scalar=sc, in1=b.ap, op0=op0, op1=op1),
                     [a, b] + list(extra_reads), [out])

        def recip(out, in_):
            rec.emit("dve", lambda e: e.reciprocal(out=out.ap, in_=in_.ap), [in_], [out])

        def cp(out, in_, eng="dve"):
            rec.emit(eng, lambda e: e.tensor_copy(out=out.ap, in_=in_.ap), [in_], [out])

        def mset(out, val, eng="dve"):
            rec.emit(eng, lambda e: e.memset(out.ap, val), [], [out])

        def dma(eng, stream, out, in_, reads=(), writes=()):
            rec.emit(eng, lambda e: e.dma_start(out=out, in_=in_), list(reads), list(writes), prod=stream)

        def dma_multi(eng, stream, pairs, reads=(), writes=()):
            def fn(e):
                return [e.dma_start(out=o, in_=i) for o, i in pairs]
            rec.emit(eng, fn, list(reads), list(writes), prod=stream, n=len(pairs))

        w_i = [0]

        def wload(src2d, kcs, cols):
            i = w_i[0]
            w_i[0] = (i + 1) % NW
            dst = wbuf[:, i, 0:kcs * cols].rearrange("p (k m) -> p k m", k=kcs)
            t = T(dst, [("w", i)])
            dma("pool", "w%d" % i, dst, src2d.rearrange("(k p) m -> p k m", p=128), writes=[t])
            return t

        def wload2(src2d):
            if w_i[0] % 2:
                w_i[0] = (w_i[0] + 1) % NW
            i = w_i[0]
            w_i[0] = (i + 2) % NW
            dst = wbuf[:, i:i + 2, :].rearrange("p a (k m) -> p (a k) m", m=512)
            t = T(dst, [("w", i), ("w", i + 1)])
            dma("pool", "w%d" % i, dst, src2d.rearrange("(k p) m -> p k m", p=128), writes=[t])
            return t

        GT = T(gT[:], [("gT",)])
        IDF = T(ident[:], [("ident",)])
        IDB = T(identb[:], [("identb",)])
        ONESB = T(onesb[:], [("onesb",)])
        MIXC = T(mixc[:], [("mixc",)])
        QDEC = T(qdec[:], [("qdec",)])
        epst = sb("epst", [128, 2], F32)
        EPSR = epst[:, 0:1]
        EPSL = epst[:, 1:2]
        EPST = T(epst[:], [("eps", 0), ("eps", 1)])

        vst = Rf(0, 1024)
        vst32 = T(vst.ap[0:32, :], vst.keys)
        mset(vst32, 0.0)
        wsn = Rf(2048, 1024)
        bsb = Rf(4096, 1024)
        tri = Rf(6144, 128)
        onesf = Rf(6656, 128)
        WTf = Rf(7168, 1024)
        Cs = Rf(9216, 2048)
        pre = [
            (ident[:], id_d[:, :]),
            (qdec[:], qdec_d[:, :]),
            (vst.ap[0:4, :], nmg_d[:, :]),
            (vst.ap[4:8, :], nfg_d[:, :]),
            (vst.ap[8:9, :], fng_d.rearrange("(o d) -> o d", o=1)),
            (vst.ap[9:15, :], cw_d.rearrange("j k d -> (j k) d")),
            (vst.ap[16:18, :], slb_d.rearrange("o (r d) -> (o r) d", r=2)),
            (wsn.ap.rearrange("p (g s) -> p g s", g=8), sws_d.rearrange("o g t s -> t (o g) s")),
            (bsb.ap, sbs_d.rearrange("o g t -> o (g t)").partition_broadcast(128)),
            (tri.ap, tri_d[:, :]),
        ]
        rec.emit("sp", lambda e: [e.dma_start(out=o, in_=i) for o, i in pre], [vst32],
                 [IDF, QDEC, vst, wsn, bsb, tri], prod="cst", n=len(pre))
        cp(IDB, IDF)
        mset(ONESB, 1.0 / 1024.0)
        mset(onesf, 1.0)
        mset(T(epst[:, 0:1], [("eps", 0)]), RMS_EPS)
        mset(T(epst[:, 1:2], [("eps", 1)]), LN_EPS)
        mset(T(S32[:], [("S32", i) for i in range(8)]), 0.0)
        mset(T(Sbf[:], [("Sbf", i) for i in range(8)]), 0.0)
        mset(T(halo[:], [("halo", 0), ("halo", 1)]), 0.0)
        bk = nb()
        pe([("tr", bk.ap[:, kc * 32:(kc + 1) * 32], vst.ap[0:32, kc * 128:(kc + 1) * 128], ident[0:32, 0:32])
            for kc in range(KC)], [vst, IDF], [bk])
        act(T(gT[:].rearrange("p k r -> p (k r)"), GT.keys), T(bk.ap[:, 0:256], bk.keys), AF.Copy)
        for half in range(2):
            bk = nb()
            pe([("tr", bk.ap[:, j * 128:(j + 1) * 128], wsn.ap[:, (half * 4 + j) * 128:(half * 4 + j + 1) * 128], ident[:])
                for j in range(4)], [wsn, IDF], [bk])
            for j in range(4):
                g_ = half * 4 + j
                tt(T(WTf.ap[:, g_ * 128:(g_ + 1) * 128], WTf.keys), T(bk.ap[:, j * 128:(j + 1) * 128], bk.keys), tri, ALU.mult)
        cp(T(WT[:].rearrange("p g t -> p (g t)"), [("WT",)]), WTf)
        WTt = T(WT[:], [("WT",)])
        for half in range(2):
            bk = nb()
            pe([("mm", bk.ap[:, j * 128:(j + 1) * 128], onesf.ap, WTf.ap[:, (half * 4 + j) * 128:(half * 4 + j + 1) * 128], True, True)
                for j in range(4)], [onesf, WTf], [bk])
            for j in range(4):
                g_ = half * 4 + j
                for i in range(2):
                    dc = g_ * 2 + i
                    r_, kc_ = dc // 8, dc % 8
                    stt(T(Cs.ap[:, dc * 128:(dc + 1) * 128], Cs.keys), T(bk.ap[:, j * 128:(j + 1) * 128], bk.keys),
                        gT[:, kc_, 16 + r_:17 + r_], T(bsb.ap[:, g_ * 128:(g_ + 1) * 128], bsb.keys),
                        ALU.mult, ALU.add, extra_reads=[GT])
        CD = T(C_dram, [("Cdram",)])
        dma("sp", "cst", C_dram[:, :], Cs.ap, reads=[Cs], writes=[CD])

        def rmsnorm(col, out_fn, out_dtype_bf16=True):
            mark("norm%d" % col)
            for s in CS[0]:
                PST = nb()
                for kc in range(KC):
                    q_ = nsq()
                    act(q_, xv(kc, s), AF.Square)
                    pe([("mm", PST.ap, ONESB.ap, q_.ap, kc == 0, kc == KC - 1)], [ONESB, q_], [PST])
                rs = T(rstd[:, s % 2, :], [("rstd", s % 2)])
                act(rs, PST, AF.Ln, bias=EPSR, extra_reads=[EPST])
                act(rs, rs, AF.Exp, scale=-0.5)
                for kc in range(KC):
                    stt(out_fn(kc, s), xv(kc, s), gT[:, kc, col:col + 1], rs, ALU.mult, ALU.mult, extra_reads=[GT])

        def resid_add(bk, mc, s):
            tt(xv(mc, s), bk, xv(mc, s), ALU.add)

        def proj_fm(w_t, mcs, rhs_fn, nk, evac, first=False):
            for mi in range(mcs):
                for s in CS[0]:
                    bk = nb()
                    mm(bk, [(T(w_t.ap[:, k, mi * 128:(mi + 1) * 128], w_t.keys), rhs_fn(k, s)) for k in range(nk)],
                       split=(first and mi == 0))
                    evac(bk, mi, s)

        def proj_acc(w_fn, parts, ncol_groups, rhs_fn, evac):
            for mg in range(ncol_groups):
                banks = [{s: nb() for s in CS[0]} for mi in range(2)]
                for pi, (kc0, nkc) in enumerate(parts):
                    wt = wload(w_fn(kc0, nkc, mg), nkc, 256)
                    for mi in range(2):
                        for s in CS[0]:
                            instrs = [("mm", banks[mi][s].ap, wt.ap[:, k, mi * 128:(mi + 1) * 128], rhs_fn(kc0 + k, s).ap,
                                       pi == 0 and k == 0, pi == len(parts) - 1 and k == nkc - 1) for k in range(nkc)]
                            pe(instrs, [wt] + [rhs_fn(kc0 + k, s) for k in range(nkc)], [banks[mi][s]])
                for mi in range(2):
                    for s in CS[0]:
                        evac(banks[mi][s], mg * 2 + mi, s)

        def ffn(l):
            mark("ffn_in")
            def actv(c, s):
                return Rb((c * SUB + s) * 512, 512)
            for mg in range(11):
                wg = wload(fwg_d[l, :, mg * 256:(mg + 1) * 256], KC, 256)
                wu = wload(fwu_d[l, :, mg * 256:(mg + 1) * 256], KC, 256)
                for mi in range(2):
                    for s in CS[0]:
                        ba = nb()
                        mm(ba, [(T(wg.ap[:, k, mi * 128:(mi + 1) * 128], wg.keys), hv(k, s)) for k in range(KC)],
                           split=(mg == 0 and mi == 0))
                        bu = nb()
                        mm(bu, [(T(wu.ap[:, k, mi * 128:(mi + 1) * 128], wu.keys), hv(k, s)) for k in range(KC)])
                        tm = ntmp()
                        act(tm, ba, AF.Silu)
                        tt(actv(mg * 2 + mi, s), tm, bu, ALU.mult)
            mark("ffn_down")
            proj_acc(lambda kc0, nkc, mg: fwd_d[l, kc0 * 128:(kc0 + nkc) * 128, mg * 256:(mg + 1) * 256],
                     [(0, 6), (6, 5), (11, 6), (17, 5)], 4, actv, resid_add)

        def out_proj(w_d2, nk, rhs_fn):
            mark("out_proj")
            proj_acc(lambda kc0, nkc, mg: w_d2[kc0 * 128:(kc0 + nkc) * 128, mg * 256:(mg + 1) * 256],
                     [(p * 8, 8) for p in range(nk // 8)], 4, rhs_fn, resid_add)

        def conv_mixer(j, tail_only=False):
            CZS = 2 * (G + 4)
            mark("conv_in")

            def czv(fc, c0, n):
                off = fc * CZS + 2 * c0
                return Rf(off, n)
            YO = KC * CZS

            def yv(kc, s):
                return Rb(YO + (kc * SUB + s) * 512, 512)
            HK = T(halo[:, j, :, :], [("halo", j)])
            for fc in range(KC):
                cp(czv(fc, 2, 2), T(halo[:, j, fc, :], HK.keys), eng="act" if False else "dve")
            for fg in range(4):
                wc = wload(cwin_d[j, :, D + fg * 256:D + (fg + 1) * 256], KC, 256)
                wz = wload(cwin_d[j, :, 2 * D + fg * 256:2 * D + (fg + 1) * 256], KC, 256)
                wb = None if tail_only else wload(cwin_d[j, :, fg * 256:(fg + 1) * 256], KC, 256)
                for mi in range(2):
                    fc = fg * 2 + mi
                    for s in CS[0]:
                        bc = nb()
                        mm(bc, [(T(wc.ap[:, k, mi * 128:(mi + 1) * 128], wc.keys), hv(k, s)) for k in range(KC)],
                           split=(fg == 0 and mi == 0))
                        bz = nb()
                        mm(bz, [(T(wz.ap[:, k, mi * 128:(mi + 1) * 128], wz.keys), hv(k, s)) for k in range(KC)])
                        tm = ntmp()
                        act(tm, bc, AF.Copy)
                        tt(czv(fc, 4 + s * 512, 512), tm, bz, ALU.mult)
                        if tail_only:
                            continue
                        bb = nb()
                        mm(bb, [(T(wb.ap[:, k, mi * 128:(mi + 1) * 128], wb.keys), hv(k, s)) for k in range(KC)])
                        t1 = ntmp()
                        c0 = 4 + s * 512
                        ts(t1, czv(fc, c0, 512), gT[:, fc, 9 + j * 3 + 2:9 + j * 3 + 3], None, ALU.mult, extra_reads=[GT])
                        stt(t1, czv(fc, c0 - 1, 512), gT[:, fc, 9 + j * 3 + 1:9 + j * 3 + 2], t1, ALU.mult, ALU.add, extra_reads=[GT])
                        stt(t1, czv(fc, c0 - 2, 512), gT[:, fc, 9 + j * 3:9 + j * 3 + 1], t1, ALU.mult, ALU.add, extra_reads=[GT])
                        tt(yv(fc, s), t1, bb, ALU.mult)
            for fc in range(KC):
                cp(T(halo[:, j, fc, :], HK.keys), czv(fc, 2 + G, 2))
            if not tail_only:
                out_proj(cwout_d[j], KC, yv)

        def sgu_mixer():
            def uv(c, s):
                return Rb((c * SUB + s) * 512, 512)
            NVS = 3 if SUB == 2 else 2
            VO = 16 * SUB * 512

            def vtok(slot):
                return Rf(VO + slot * 4096, 2048)
            dma_multi("sp", "mc", [(mixc[:, 0:2048], C_dram[:, :]),
                                   (mixc[:, 2048:4096], slg_d[0:1, :].partition_broadcast(128))],
                      reads=[CD], writes=[MIXC])
            mark("sgu_u")
            for mg in range(8):
                wt = wload(swin_d[0, :, mg * 256:(mg + 1) * 256], KC, 256)
                proj_fm(wt, 2, hv, KC, lambda bk, mi, s, mg=mg: act(uv(mg * 2 + mi, s), bk, AF.Gelu), first=(mg == 0))
            for q0 in range(0, NCH, NVS):
                mark("sgu_v%d" % q0)
                sums = {}
                for vt in range(4):
                    wt = wload2(swin_d[0, :, 2048 + vt * 512:2048 + (vt + 1) * 512])
                    for ci in range(min(NVS, NCH - q0)):
                        ch = q0 + ci
                        bk = nb()
                        mm(bk, [(hvc(k, ch), T(wt.ap[:, k, :], wt.keys)) for k in range(KC)])
                        vt_t = vtok(ci)
                        col = ci * 16 + vt
                        act(T(vt_t.ap[:, vt * 512:(vt + 1) * 512], vt_t.keys), bk, AF.Gelu,
                            accum=T(st[:, col:col + 1], [("st", col)]))
                for ci in range(min(NVS, NCH - q0)):
                    ch = q0 + ci
                    vt_t = vtok(ci)
                    b0 = ci * 16
                    STc = lambda a, n=1: T(st[:, b0 + a:b0 + a + n], [("st", b0 + a + i_) for i_ in range(n)])
                    vh = T(vhb[:, ch % 2, :], [("vhb", ch % 2)]) if SUB == 1 else Rb(28672 + (ch % 2) * 2048, 2048)
                    act(vh, vt_t, AF.Square, accum=STc(4))
                    rec.emit("dve", lambda e, a=STc(5), b=STc(0, 4): e.tensor_reduce(out=a.ap, in_=b.ap, axis=mybir.AxisListType.X, op=ALU.add),
                             [STc(0, 4)], [STc(5)])
                    ts(STc(5), STc(5), 1.0 / 2048.0, None, ALU.mult)
                    tt(STc(6), STc(5), STc(5), ALU.mult)
                    stt(STc(7), STc(4), 1.0 / 2048.0, STc(6), ALU.mult, ALU.subtract)
                    act(STc(7), STc(7), AF.Ln, bias=EPSL, extra_reads=[EPST])
                    act(STc(7), STc(7), AF.Exp, scale=-0.5)
                    stt(STc(8), STc(5), -1.0, STc(7), ALU.mult, ALU.mult)
                    act(vt_t, vt_t, AF.Identity, bias=st[:, b0 + 8:b0 + 9], scale=st[:, b0 + 7:b0 + 8], extra_reads=[STc(7), STc(8)])
                    tt(vh, vt_t, T(mixc[:, 2048:4096], MIXC.keys), ALU.mult)
                    s_ = ch // 4
                    cc = ch % 4
                    for b4 in range(4):
                        bk = nb()
                        pe([("mm", bk.ap[:, i_ * 128:(i_ + 1) * 128], vh.ap[:, (b4 * 4 + i_) * 128:(b4 * 4 + i_ + 1) * 128],
                             WT[:, (b4 * 4 + i_) // 2, :], True, True) for i_ in range(4)], [vh, WTt], [bk])
                        tm = ntmp()
                        tt(tm, bk, T(mixc[:, b4 * 512:(b4 + 1) * 512], MIXC.keys), ALU.add)
                        tm3 = T(tm.ap.rearrange("p (c t) -> p c t", c=4), tm.keys)
                        u0 = b4 * 4 * SUB * 512
                        u4 = T(R[:, u0:u0 + 4 * SUB * 512].rearrange("p (c s t) -> p c s t", c=4, s=SUB)[:, :, s_, cc * 128:(cc + 1) * 128],
                               rkeys(u0, 4 * SUB * 512))
                        tt(u4, tm3, u4, ALU.mult)
            out_proj(swout_d[0], 16, uv)

        def ret_mixer(g, state_only, fsubs=None):
            allsubs = list(range(SUB))
            fsubs = allsubs if fsubs is None else fsubs
            def qv(c, s):
                return Rb((c * SUB + s) * 512, 512)

            def kv(c, s):
                return Rb(KC * SUB * 512 + (c * SUB + s) * 512, 512)
            VO = 2 * KC * SUB * 512

            def vv(ch):
                return Rb(VO + ch * 2048, 2048)
            dma_multi("sp", "mc", [(mixc[:, 0:G], cos_d[:, g * G:(g + 1) * G]),
                                   (mixc[:, 1024:1024 + G], sin_d[:, g * G:(g + 1) * G]),
                                   (mixc[:, 2048:3072], kdec_d[:, :]),
                                   (mixc[:, 3072:3584], dT_d[:, :])], writes=[MIXC])

            def cosv(s):
                return T(mixc[:, s * 512:(s + 1) * 512], MIXC.keys)

            def sinv(s):
                return T(mixc[:, 1024 + s * 512:1024 + (s + 1) * 512], MIXC.keys)

            def rot_proj(col0, dst):
                for h in range(HEADS):
                    wt = wload(rwin_d[0, :, col0 + h * 256:col0 + (h + 1) * 256], KC, 256)
                    for hh in range(1):
                        for s in CS[0]:
                            ba = nb()
                            mm(ba, [(T(wt.ap[:, k, (hh * 2) * 128:(hh * 2 + 1) * 128], wt.keys), hv(k, s)) for k in range(KC)],
                               split=(h == 0 and col0 == D))
                            bb = nb()
                            mm(bb, [(T(wt.ap[:, k, (hh * 2 + 1) * 128:(hh * 2 + 2) * 128], wt.keys), hv(k, s)) for k in range(KC)])
                            t1, t2 = ntmp(), ntmp()
                            tt(t1, ba, cosv(s), ALU.mult)
                            tt(t2, bb, sinv(s), ALU.mult)
                            tt(dst(2 * h, s), t1, t2, ALU.subtract)
                            t3, t4 = ntmp(), ntmp()
                            tt(t3, bb, cosv(s), ALU.mult)
                            tt(t4, ba, sinv(s), ALU.mult)
                            tt(dst(2 * h + 1, s), t3, t4, ALU.add)
            mark("ret_k")
            CS[0] = allsubs
            rot_proj(D, kv)
            if not state_only:
                mark("ret_q")
                CS[0] = fsubs
                rot_proj(0, qv)
            CS[0] = allsubs
            mark("ret_v")
            for h in range(HEADS):
                wt = wload2(rwin_d[0, :, 2 * D + h * 512:2 * D + (h + 1) * 512])
                for ch in range(NCH):
                    bk = nb()
                    mm(bk, [(hvc(k, ch), T(wt.ap[:, k, :], wt.keys)) for k in range(KC)])
                    v_ = vv(ch)
                    act(T(v_.ap[:, h * 512:(h + 1) * 512], v_.keys), bk, AF.Copy)
            Skeys = lambda i: [("S32", i)]
            if not state_only:
                for i in range(8):
                    act(T(Sbf[:, i, :], [("Sbf", i)]), T(S32[:, i, :], Skeys(i)), AF.Copy)
            for ch in range(NCH):
                mark("ret_core%d" % ch)
                s_, cc = ch // 4, ch % 4
                v_ = vv(ch)
                tb = ntb()
                pe([("tr", tb.ap[:, c * 128:(c + 1) * 128], kv(c, s_).ap[:, cc * 128:(cc + 1) * 128], identb[:]) for c in range(KC)],
                   [kv(c, s_) for c in range(KC)] + [IDB], [tb])
                kdt = T(kd[:, ch % 2, :], [("kd", ch % 2)])
                tt(kdt, tb, T(mixc[:, 2048:3072], MIXC.keys), ALU.mult)
                full_ch = (not state_only) and (s_ in fsubs)
                if full_ch:
                    bs_ = nb()
                    instrs = []
                    for h in range(HEADS):
                        for i in range(2):
                            c = 2 * h + i
                            instrs.append(("mm", bs_.ap[:, h * 128:(h + 1) * 128], kv(c, s_).ap[:, cc * 128:(cc + 1) * 128],
                                           qv(c, s_).ap[:, cc * 128:(cc + 1) * 128], i == 0, i == 1))
                    pe(instrs, [kv(c, s_) for c in range(KC)] + [qv(c, s_) for c in range(KC)], [bs_])
                    sT = T(sTb[:, ch % 2, :], [("sTb", ch % 2)])
                    tt(sT, bs_, T(mixc[:, 3072:3584], MIXC.keys), ALU.mult)
                    obanks = []
                    for h in range(HEADS):
                        bo = nb()
                        instrs = [("mm", bo.ap, sT.ap[:, h * 128:(h + 1) * 128], v_.ap[:, h * 512:(h + 1) * 512], True, False)]
                        rd = [sT, v_]
                        for i in range(2):
                            c = 2 * h + i
                            instrs.append(("mm", bo.ap, qv(c, s_).ap[:, cc * 128:(cc + 1) * 128], Sbf[:, c, :], False, i == 1))
                            rd += [qv(c, s_), T(Sbf[:, c, :], [("Sbf", c)])]
                        pe(instrs, rd, [bo])
                        obanks.append(bo)
                if full_ch:
                    onb = []
                    for h in range(HEADS):
                        bo = obanks[h]
                        col = 32 + h * 4
                        ssq = T(st[:, col:col + 1], [("st", col)])
                        rr = T(st[:, col + 1:col + 2], [("st", col + 1)])
                        on = T(onbuf[:, h, :], [("on", h)])
                        act(on, bo, AF.Square, scale=qdec[:, h:h + 1], accum=ssq, extra_reads=[QDEC])
                        act(rr, ssq, AF.Ln, bias=EPSR, scale=1.0 / 512.0, extra_reads=[EPST])
                        act(rr, rr, AF.Exp, scale=-0.5)
                        tt(rr, rr, T(qdec[:, h:h + 1], QDEC.keys), ALU.mult)
                        act(on, bo, AF.Copy, scale=st[:, col + 1:col + 2], extra_reads=[rr])
                        onb.append(on)
                for c in range(8):
                    h = c // 2
                    bk = nb()
                    mm(bk, [(T(kdt.ap[:, c * 128:(c + 1) * 128], kdt.keys), T(v_.ap[:, h * 512:(h + 1) * 512], v_.keys))])
                    Sc = T(S32[:, c, :], Skeys(c))
                    stt(Sc, Sc, cdec[h], bk, ALU.mult, ALU.add)
                    if not state_only:
                        act(T(Sbf[:, c, :], [("Sbf", c)]), Sc, AF.Copy)
                if not full_ch:
                    continue
                for hp in range(2):
                    tb = ntb()
                    instrs = []
                    for hh in range(2):
                        for e4 in range(4):
                            instrs.append(("tr", tb.ap[:, (hh * 4 + e4) * 128:(hh * 4 + e4 + 1) * 128],
                                           onb[hp * 2 + hh].ap[:, e4 * 128:(e4 + 1) * 128], identb[:]))
                    pe(instrs, [onb[hp * 2], onb[hp * 2 + 1], IDB], [tb])
                    cp(T(v_.ap[:, hp * 1024:(hp + 1) * 1024], v_.keys), tb)
            if state_only:
                return
            mark("ret_gate")
            CS[0] = fsubs
            def ov(c, s):
                ap = R[:, VO + s * 4 * 2048:VO + (s + 1) * 4 * 2048].rearrange("p (ch c t) -> p ch c t", ch=4, c=16)[:, :, c, :]
                return T(ap, rkeys(VO + s * 4 * 2048, 4 * 2048))
            for mg in range(8):
                wt = wload(rwin_d[0, :, 4 * D + mg * 256:4 * D + (mg + 1) * 256], KC, 256)

                def evac(bk, mi, s, mg=mg):
                    tm = ntmp()
                    act(tm, bk, AF.Silu)
                    o_ = ov(mg * 2 + mi, s)
                    tt(o_, T(tm.ap.rearrange("p (ch t) -> p ch t", ch=4), tm.keys), o_, ALU.mult)
                proj_fm(wt, 2, hv, KC, evac)
            out_proj(rwout_d[0], 16, ov)
            CS[0] = allsubs

        XO = RN - 4096
        for g in range(NG):
            if g < npre - 1:
                mode = "pre"
            elif g == npre - 1:
                mode = "pre_last"
            else:
                mode = "full"
            mark("G%d_%s_load" % (g, mode))
            for ch in range(NCH):
                sl = ch % 2
                xs = Rf(XO + sl * 2048, 1024)
                tok0 = g * G + ch * 128
                dma("sp", "xs%d" % sl, xs.ap, x_d[tok0:tok0 + 128, :], writes=[xs])
                for half in range(2):
                    bk = nb()
                    pe([("tr", bk.ap[:, j * 128:(j + 1) * 128], xs.ap[:, (half * 4 + j) * 128:(half * 4 + j + 1) * 128], ident[:])
                        for j in range(4)], [xs, IDF], [bk])
                    s_ = ch // 4
                    dst = T(xT[:, half * 4:half * 4 + 4, ch * 128:(ch + 1) * 128], [("x", half * 4 + j, s_) for j in range(4)])
                    rec.emit("act" if half else "dve",
                             (lambda e, d=dst, b=bk: e.activation(out=d.ap, in_=b.ap.rearrange("p (c t) -> p c t", c=4), func=AF.Copy)) if half else
                             (lambda e, d=dst, b=bk: e.tensor_copy(out=d.ap, in_=b.ap.rearrange("p (c t) -> p c t", c=4))),
                             [bk], [dst])
            for l in range(DBG_NL):
                kind = l % 3
                if mode == "pre" and l == 3:
                    break
                rmsnorm(l, hv)
                if kind == 0:
                    conv_mixer(l // 3, tail_only=(mode == "pre_last" and l == 3))
                    if mode == "pre_last" and l == 3:
                        break
                elif kind == 1:
                    sgu_mixer()
                else:
                    ret_mixer(g, state_only=(mode == "pre"), fsubs=([SUB - 1] if mode == "pre_last" else None))
                    if mode == "pre":
                        break
                    if mode == "pre_last":
                        CS[0] = [SUB - 1]
                if DBG_FFN:
                    rmsnorm(4 + l, hv)
                    ffn(l)
            CS[0] = list(range(SUB))
            if mode != "full":
                continue
            def fv(kc, s):
                return Rf((kc * SUB + s) * 1024, 512)
            mark("final")
            rmsnorm(8, fv)
            for ch in range(NCH):
                s_, cc = ch // 4, ch % 4
                sl = ch % 2
                os_ = Rf(XO + sl * 2048, 1024)
                for half in range(2):
                    bk = nb()
                    pe([("tr", bk.ap[:, j * 128:(j + 1) * 128], fv(half * 4 + j, s_).ap[:, cc * 128:(cc + 1) * 128], ident[:])
                        for j in range(4)], [fv(half * 4 + j, s_) for j in range(4)] + [IDF], [bk])
                    dst = T(os_.ap[:, half * 512:(half + 1) * 512], os_.keys)
                    if half:
                        act(dst, bk, AF.Copy)
                    else:
                        cp(dst, bk)
                tok0 = (g - npre) * G + ch * 128
                dma("sp", "os%d" % sl, y_d[tok0:tok0 + 128, :], os_.ap, reads=[os_])
        rec.final_wait("sp", ["os0", "os1"])
        with nc.Block() as block:
            rec.run(block)
    return nc


def _tables(positions):
    half = 128
    inv_freq = (10000.0 ** (-(np.arange(half, dtype=np.float32) / np.float32(half)))).astype(np.float32)
    ang = (positions.astype(np.float32)[None, :] * inv_freq[:, None]).astype(np.float32)
    return np.cos(ang).astype(np.float32), np.sin(ang).astype(np.float32)


def _const_tables():
    gam = 1.0 - 2.0 ** (-5.0 - np.arange(HEADS, dtype=np.float64))
    lg = np.log(gam)
    idx = np.arange(128, dtype=np.float64)
    dT = np.zeros((128, HEADS, 128), np.float64)
    for h in range(HEADS):
        kf = np.exp(-(idx + 1.0) * lg[h]) / 16.0
        dT[:, h, :] = (idx[:, None] <= idx[None, :]) * kf[:, None]
    kdec = np.zeros((128, HEADS, 256), np.float64)
    for h in range(HEADS):
        kdec[:, h, :] = (np.exp((127.0 - idx) * lg[h]) / 16.0)[:, None]
    qdec = np.exp((idx[:, None] + 1.0) * lg[None, :])
    tri = (idx[:, None] <= idx[None, :]).astype(np.float32)
    return (dT.reshape(128, 512).astype(np.float32), kdec.reshape(128, 1024).astype(np.float32),
            qdec.astype(np.float32), np.eye(128, dtype=np.float32), tri)


_NC_CACHE = {}


def run_trunk(inputs, seq, npre, nseg, SUB, n_cores):
    G = SUB * 512
    half = seq // 2
    assert npre * G == half and nseg * G == half
    key = (npre, nseg, SUB, DBG_NL, DBG_FFN)
    if key not in _NC_CACHE:
        _NC_CACHE[key] = build(npre, nseg, SUB)
    nc = _NC_CACHE[key]
    x = np.ascontiguousarray(np.asarray(inputs["x"], dtype=np.float32))
    dT, kdec, qdec, ident, tri = _const_tables()
    shared = {k: np.ascontiguousarray(np.asarray(v, dtype=np.float32)) for k, v in inputs.items() if k != "x"}
    shared.update({"t_dT": dT, "t_kdec": kdec, "t_qdec": qdec, "t_ident": ident, "t_tri": tri})
    in_maps = []
    for c in range(n_cores):
        s, p = c // 2, c % 2
        if p == 0:
            xc = np.concatenate([np.zeros((half, D), np.float32), x[s, :half]], axis=0)
            pos = np.arange(-half, half)
        else:
            xc = x[s]
            pos = np.arange(0, seq)
        cos, sin = _tables(pos)
        m = dict(shared)
        m.update({"x": np.ascontiguousarray(xc), "t_cos": cos, "t_sin": sin})
        in_maps.append(m)
    res = run_bass_kernel_spmd(nc, in_maps, core_ids=list(range(n_cores)))
    out = np.zeros((n_cores // 2, seq, D), np.float32)
    for c in range(n_cores):
        s, p = c // 2, c % 2
        out[s, p * half:(p + 1) * half] = res.results[c]["y"]
    return out


SUB_CFG = 2
DBG_NL = 4
MARKS = []
DBG_FFN = True


def kernel(**inputs):
    G = SUB_CFG * 512
    n = (SEQ // 2) // G
    return run_trunk(inputs, SEQ, n, n, SUB_CFG, 8)
```

```python
import numpy as np
from contextlib import ExitStack
import concourse.bass as bass
import concourse.mybir as mybir
from concourse.bass_utils import run_bass_kernel_spmd

F32 = mybir.dt.float32
BF16 = mybir.dt.bfloat16
AF = mybir.ActivationFunctionType
ALU = mybir.AluOpType

D = 1024
KC = 8
DFF = 2816
FC = 22
RMS_EPS = 1e-6
LN_EPS = 1e-5
HEADS = 4
SEQ = 8192
BATCH = 4
COMPUTE = ("pe", "act", "dve")


class T:
    __slots__ = ("ap", "keys")

    def __init__(self, ap, keys):
        self.ap = ap
        self.keys = tuple(keys)


class Rec:
    def __init__(self, nc, es):
        self.nc = nc
        self.es = es
        self.ops = {e: [] for e in ("pe", "act", "dve", "pool", "sp")}
        self.sem = {}
        self.cnt = {}
        for e in COMPUTE:
            self.sem[e] = es.enter_context(nc.semaphore("s_" + e))
            self.cnt[e] = 0
        self.waited = {}
        self.lastw = {}
        self.readers = {}

    def stream(self, name):
        self.sem[name] = self.es.enter_context(self.nc.semaphore(name))
        self.cnt[name] = 0
        return name

    def _need(self, waits, eng, prod, val):
        if prod == eng and eng == "pe":
            return
        k = (eng, prod)
        if self.waited.get(k, 0) >= val:
            return
        self.waited[k] = val
        waits.append((self.sem[prod], val * (1 if prod in COMPUTE else 16)))

    def emit(self, eng, fn, reads, writes, prod=None, n=1):
        prod = prod or eng
        waits = []
        for t in reads:
            for k in t.keys:
                lw = self.lastw.get(k)
                if lw:
                    self._need(waits, eng, *lw)
        for t in writes:
            for k in t.keys:
                lw = self.lastw.get(k)
                if lw:
                    self._need(waits, eng, *lw)
                for p, v in self.readers.get(k, {}).items():
                    self._need(waits, eng, p, v)
        self.cnt[prod] += n
        val = self.cnt[prod]
        for t in writes:
            for k in t.keys:
                self.lastw[k] = (prod, val)
                self.readers[k] = {}
        for t in reads:
            for k in t.keys:
                self.readers.setdefault(k, {})[prod] = val
        self.ops[eng].append((waits, fn, self.sem[prod], 1 if prod in COMPUTE else 16))

    def final_wait(self, eng, prods):
        waits = [(self.sem[p], self.cnt[p] * (1 if p in COMPUTE else 16)) for p in prods if self.cnt[p]]
        self.ops[eng].append((waits, None, None, 0))

    def run(self, block):
        def mk(e):
            def body(eng):
                for waits, fn, sem, inc in self.ops[e]:
                    for sm, v in waits:
                        eng.wait_ge(sm, v)
                    if fn is None:
                        continue
                    r = fn(eng)
                    if isinstance(r, (list, tuple)):
                        for ins in r:
                            ins.then_inc(sem, inc)
                    else:
                        r.then_inc(sem, inc)
            return body
        block.tensor(mk("pe"))
        block.scalar(mk("act"))
        block.vector(mk("dve"))
        block.gpsimd(mk("pool"))
        block.sync(mk("sp"))


def build(npre, nseg, SUB):
    G = SUB * 512
    NCH = G // 128
    NG = npre + nseg
    NTOK = NG * G
    RN = 16384 * SUB
    nc = bass.Bass("TRN2", target_bir_lowering=False)

    def din(name, shape):
        return nc.dram_tensor(name, list(shape), F32, kind="ExternalInput").ap()

    x_d = din("x", [NTOK, D])
    nmg_d = din("norm_mix_g", [4, D])
    nfg_d = din("norm_ffn_g", [4, D])
    fng_d = din("final_norm_g", [D])
    cwin_d = din("conv_w_in", [2, D, 3 * D])
    cw_d = din("conv_w", [2, 3, D])
    cwout_d = din("conv_w_out", [2, D, D])
    swin_d = din("sgu_w_in", [1, D, 4096])
    slg_d = din("sgu_ln_g", [1, 2048])
    slb_d = din("sgu_ln_b", [1, 2048])
    sws_d = din("sgu_w_s", [1, 8, 128, 128])
    sbs_d = din("sgu_b_s", [1, 8, 128])
    swout_d = din("sgu_w_out", [1, 2048, D])
    rwin_d = din("ret_w_in", [1, D, 6 * D])
    rwout_d = din("ret_w_out", [1, 2048, D])
    fwg_d = din("ffn_w_gate", [4, D, DFF])
    fwu_d = din("ffn_w_up", [4, D, DFF])
    fwd_d = din("ffn_w_down", [4, DFF, D])
    cos_d = din("t_cos", [128, NTOK])
    sin_d = din("t_sin", [128, NTOK])
    dT_d = din("t_dT", [128, 512])
    kdec_d = din("t_kdec", [128, 1024])
    qdec_d = din("t_qdec", [128, 4])
    id_d = din("t_ident", [128, 128])
    tri_d = din("t_tri", [128, 128])
    y_d = nc.dram_tensor("y", [nseg * G, D], F32, kind="ExternalOutput").ap()
    C_dram = nc.dram_tensor("c_scr", [128, 2048], F32).ap()

    gam = [1.0 - 2.0 ** (-5.0 - h) for h in range(HEADS)]
    cdec = [float(np.exp(128.0 * np.log(g_))) for g_ in gam]

    with ExitStack() as es:
        def sb(name, shape, dt):
            return es.enter_context(nc.sbuf_tensor(name, list(shape), dt))

        def ps(name, shape, dt):
            return es.enter_context(nc.psum_tensor(name, list(shape), dt))

        xT = sb("xT", [128, KC, G], F32)
        hT = sb("hT", [128, KC, G], BF16)
        R = sb("R", [128, RN], BF16)
        S32 = sb("S32", [128, 8, 512], F32)
        Sbf = sb("Sbf", [128, 8, 512], BF16)
        NW = 6
        wbuf = sb("wbuf", [128, NW, 2048], BF16)
        mixc = sb("mixc", [128, 4096], F32)
        sq = sb("sq", [128, 4, 512], BF16)
        rstd = sb("rstd", [128, 2, 512], F32)
        NTMP = 4
        tmp = sb("tmp", [128, NTMP, 512], F32)
        kd = sb("kd", [128, 2, 1024], BF16)
        sTb = sb("sTb", [128, 2, 512], BF16)
        vhb = sb("vhb", [128, 2, 2048], BF16) if SUB == 1 else None
        WT = sb("WT", [128, 8, 128], BF16)
        ident = sb("ident", [128, 128], F32)
        identb = sb("identb", [128, 128], BF16)
        onesb = sb("onesb", [128, 128], BF16)
        gT = sb("gT", [128, KC, 32], F32)
        halo = sb("halo", [128, 2, KC, 2], F32)
        qdec = sb("qdec", [128, 4], F32)
        st = sb("st", [128, 64], F32)
        onbuf = sb("onbuf", [128, 4, 512], BF16)

        NB = 8
        pb = [ps("pb%d" % i, [128, 512], F32) for i in range(NB)]

        rec = Rec(nc, es)
        for nm in ["w0", "w1", "w2", "w3", "w4", "w5", "xs0", "xs1", "os0", "os1", "cst", "mc", "cs"]:
            rec.stream(nm)

        def rkeys(off, n):
            return [("R", i) for i in range(off // 512, (off + n - 1) // 512 + 1)]

        def Rb(off, n):
            return T(R[:, off:off + n], rkeys(off, n))

        def Rf(off, n):
            return T(R[:, off:off + 2 * n].bitcast(F32), rkeys(off, 2 * n))

        bank_i = [0]

        def nb():
            i = bank_i[0]
            bank_i[0] = (i + 1) % NB
            return T(pb[i][:], [("pb", i)])

        tb_i = [0]

        def ntb():
            b = nb()
            return T(b.ap.bitcast(BF16), b.keys)

        tmp_i = [0]

        def ntmp():
            i = tmp_i[0]
            tmp_i[0] = (i + 1) % NTMP
            return T(tmp[:, i, :], [("tmp", i)])

        sq_i = [0]

        def nsq():
            i = sq_i[0]
            sq_i[0] = (i + 1) % 4
            return T(sq[:, i, :], [("sq", i)])

        def V(ap, *keys):
            return T(ap, keys)

        def sub_of(t0):
            return t0 // 512

        def xv(kc, s):
            return T(xT[:, kc, s * 512:(s + 1) * 512], [("x", kc, s)])

        def hv(kc, s):
            return T(hT[:, kc, s * 512:(s + 1) * 512], [("h", kc, s)])

        def hvc(kc, ch):
            return T(hT[:, kc, ch * 128:(ch + 1) * 128], [("h", kc, ch // 4)])

        PE_N = [0]
        CS = [list(range(SUB))]

        def mark(label):
            MARKS.append((PE_N[0], label))

        def pe(instrs, reads, writes):
            instrs = list(instrs)
            def fn(eng):
                last = None
                for it in instrs:
                    if it[0] == "mm":
                        _, o, l, r, s0, s1 = it
                        last = eng.matmul(out=o, lhsT=l, rhs=r, start=s0, stop=s1)
                    else:
                        _, o, i_, idn = it
                        last = eng.transpose(out=o, in_=i_, identity=idn)
                return last
            rec.emit("pe", fn, reads, writes)
            PE_N[0] += len(instrs)

        def mm(out, pairs, split=False):
            n = len(pairs)
            if split:
                for i, (l, r) in enumerate(pairs):
                    pe([("mm", out.ap, l.ap, r.ap, i == 0, i == n - 1)], [l, r], [out])
                return
            instrs = [("mm", out.ap, l.ap, r.ap, i == 0, i == n - 1) for i, (l, r) in enumerate(pairs)]
            reads = [t for p in pairs for t in p]
            pe(instrs, reads, [out])

        def act(out, in_, func, bias=None, scale=None, accum=None, extra_reads=()):
            kw = {}
            if bias is not None:
                kw["bias"] = bias
            if scale is not None:
                kw["scale"] = scale
            if accum is not None:
                kw["accum_out"] = accum.ap
            rec.emit("act", lambda e: e.activation(out=out.ap, in_=in_.ap, func=func, **kw),
                     [in_] + list(extra_reads), [out] + ([accum] if accum is not None else []))

        def tt(out, a, b, op, eng="dve"):
            rec.emit(eng, lambda e: e.tensor_tensor(out=out.ap, in0=a.ap, in1=b.ap, op=op), [a, b], [out])

        def ts(out, a, s1, s2, op0, op1=None, extra_reads=(), eng="dve"):
            if op1 is None:
                rec.emit(eng, lambda e: e.tensor_scalar(out=out.ap, in0=a.ap, scalar1=s1, scalar2=None, op0=op0),
                         [a] + list(extra_reads), [out])
            else:
                rec.emit(eng, lambda e: e.tensor_scalar(out=out.ap, in0=a.ap, scalar1=s1, scalar2=s2, op0=op0, op1=op1),
                         [a] + list(extra_reads), [out])

        def stt(out, a, sc, b, op0, op1, extra_reads=(), eng="dve"):
            rec.emit(eng, lambda e: e.scalar_tensor_tensor(out=out.ap, in0=a.ap, scalar=sc, in1=b.ap, op0=op0, op1=op1),
                     [a, b] + list(extra_reads), [out])

        def recip(out, in_):
            rec.emit("dve", lambda e: e.reciprocal(out=out.ap, in_=in_.ap), [in_], [out])

        def cp(out, in_, eng="dve"):
            rec.emit(eng, lambda e: e.tensor_copy(out=out.ap, in_=in_.ap), [in_], [out])

        def mset(out, val, eng="dve"):
            rec.emit(eng, lambda e: e.memset(out.ap, val), [], [out])

        def dma(eng, stream, out, in_, reads=(), writes=()):
            rec.emit(eng, lambda e: e.dma_start(out=out, in_=in_), list(reads), list(writes), prod=stream)

        def dma_multi(eng, stream, pairs, reads=(), writes=()):
            def fn(e):
                return [e.dma_start(out=o, in_=i) for o, i in pairs]
            rec.emit(eng, fn, list(reads), list(writes), prod=stream, n=len(pairs))

        w_i = [0]

        def wload(src2d, kcs, cols):
            i = w_i[0]
            w_i[0] = (i + 1) % NW
            dst = wbuf[:, i, 0:kcs * cols].rearrange("p (k m) -> p k m", k=kcs)
            t = T(dst, [("w", i)])
            dma("pool", "w%d" % i, dst, src2d.rearrange("(k p) m -> p k m", p=128), writes=[t])
            return t

        def wload2(src2d):
            if w_i[0] % 2:
                w_i[0] = (w_i[0] + 1) % NW
            i = w_i[0]
            w_i[0] = (i + 2) % NW
            dst = wbuf[:, i:i + 2, :].rearrange("p a (k m) -> p (a k) m", m=512)
            t = T(dst, [("w", i), ("w", i + 1)])
            dma("pool", "w%d" % i, dst, src2d.rearrange("(k p) m -> p k m", p=128), writes=[t])
            return t

        GT = T(gT[:], [("gT",)])
        IDF = T(ident[:], [("ident",)])
        IDB = T(identb[:], [("identb",)])
        ONESB = T(onesb[:], [("onesb",)])
        MIXC = T(mixc[:], [("mixc",)])
        QDEC = T(qdec[:], [("qdec",)])
        epst = sb("epst", [128, 2], F32)
        EPSR = epst[:, 0:1]
        EPSL = epst[:, 1:2]
        EPST = T(epst[:], [("eps", 0), ("eps", 1)])

        vst = Rf(0, 1024)
        vst32 = T(vst.ap[0:32, :], vst.keys)
        mset(vst32, 0.0)
        wsn = Rf(2048, 1024)
        bsb = Rf(4096, 1024)
        tri = Rf(6144, 128)
        onesf = Rf(6656, 128)
        WTf = Rf(7168, 1024)
        Cs = Rf(9216, 2048)
        pre = [
            (ident[:], id_d[:, :]),
            (qdec[:], qdec_d[:, :]),
            (vst.ap[0:4, :], nmg_d[:, :]),
            (vst.ap[4:8, :], nfg_d[:, :]),
            (vst.ap[8:9, :], fng_d.rearrange("(o d) -> o d", o=1)),
            (vst.ap[9:15, :], cw_d.rearrange("j k d -> (j k) d")),
            (vst.ap[16:18, :], slb_d.rearrange("o (r d) -> (o r) d", r=2)),
            (wsn.ap.rearrange("p (g s) -> p g s", g=8), sws_d.rearrange("o g t s -> t (o g) s")),
            (bsb.ap, sbs_d.rearrange("o g t -> o (g t)").partition_broadcast(128)),
            (tri.ap, tri_d[:, :]),
        ]
        rec.emit("sp", lambda e: [e.dma_start(out=o, in_=i) for o, i in pre], [vst32],
                 [IDF, QDEC, vst, wsn, bsb, tri], prod="cst", n=len(pre))
        cp(IDB, IDF)
        mset(ONESB, 1.0 / 1024.0)
        mset(onesf, 1.0)
        mset(T(epst[:, 0:1], [("eps", 0)]), RMS_EPS)
        mset(T(epst[:, 1:2], [("eps", 1)]), LN_EPS)
        mset(T(S32[:], [("S32", i) for i in range(8)]), 0.0)
        mset(T(Sbf[:], [("Sbf", i) for i in range(8)]), 0.0)
        mset(T(halo[:], [("halo", 0), ("halo", 1)]), 0.0)
        bk = nb()
        pe([("tr", bk.ap[:, kc * 32:(kc + 1) * 32], vst.ap[0:32, kc * 128:(kc + 1) * 128], ident[0:32, 0:32])
            for kc in range(KC)], [vst, IDF], [bk])
        act(T(gT[:].rearrange("p k r -> p (k r)"), GT.keys), T(bk.ap[:, 0:256], bk.keys), AF.Copy)
        for half in range(2):
            bk = nb()
            pe([("tr", bk.ap[:, j * 128:(j + 1) * 128], wsn.ap[:, (half * 4 + j) * 128:(half * 4 + j + 1) * 128], ident[:])
                for j in range(4)], [wsn, IDF], [bk])
            for j in range(4):
                g_ = half * 4 + j
                tt(T(WTf.ap[:, g_ * 128:(g_ + 1) * 128], WTf.keys), T(bk.ap[:, j * 128:(j + 1) * 128], bk.keys), tri, ALU.mult)
        cp(T(WT[:].rearrange("p g t -> p (g t)"), [("WT",)]), WTf)
        WTt = T(WT[:], [("WT",)])
        for half in range(2):
            bk = nb()
            pe([("mm", bk.ap[:, j * 128:(j + 1) * 128], onesf.ap, WTf.ap[:, (half * 4 + j) * 128:(half * 4 + j + 1) * 128], True, True)
                for j in range(4)], [onesf, WTf], [bk])
            for j in range(4):
                g_ = half * 4 + j
                for i in range(2):
                    dc = g_ * 2 + i
                    r_, kc_ = dc // 8, dc % 8
                    stt(T(Cs.ap[:, dc * 128:(dc + 1) * 128], Cs.keys), T(bk.ap[:, j * 128:(j + 1) * 128], bk.keys),
                        gT[:, kc_, 16 + r_:17 + r_], T(bsb.ap[:, g_ * 128:(g_ + 1) * 128], bsb.keys),
                        ALU.mult, ALU.add, extra_reads=[GT])
        CD = T(C_dram, [("Cdram",)])
        dma("sp", "cst", C_dram[:, :], Cs.ap, reads=[Cs], writes=[CD])

        def rmsnorm(col, out_fn, out_dtype_bf16=True):
            mark("norm%d" % col)
            for s in CS[0]:
                PST = nb()
                for kc in range(KC):
                    q_ = nsq()
                    act(q_, xv(kc, s), AF.Square)
                    pe([("mm", PST.ap, ONESB.ap, q_.ap, kc == 0, kc == KC - 1)], [ONESB, q_], [PST])
                rs = T(rstd[:, s % 2, :], [("rstd", s % 2)])
                act(rs, PST, AF.Ln, bias=EPSR, extra_reads=[EPST])
                act(rs, rs, AF.Exp, scale=-0.5)
                for kc in range(KC):
                    stt(out_fn(kc, s), xv(kc, s), gT[:, kc, col:col + 1], rs, ALU.mult, ALU.mult, extra_reads=[GT])

        def resid_add(bk, mc, s):
            tt(xv(mc, s), bk, xv(mc, s), ALU.add)

        def proj_fm(w_t, mcs, rhs_fn, nk, evac, first=False):
            for s in CS[0]:
                for mi in range(mcs):
                    bk = nb()
                    mm(bk, [(T(w_t.ap[:, k, mi * 128:(mi + 1) * 128], w_t.keys), rhs_fn(k, s)) for k in range(nk)],
                       split=(first and mi == 0))
                    evac(bk, mi, s)

        def proj_acc(w_fn, parts, ncol_groups, rhs_fn, evac):
            for mg in range(ncol_groups):
                banks = [{s: nb() for s in CS[0]} for mi in range(2)]
                for pi, (kc0, nkc) in enumerate(parts):
                    wt = wload(w_fn(kc0, nkc, mg), nkc, 256)
                    for mi in range(2):
                        for s in CS[0]:
                            instrs = [("mm", banks[mi][s].ap, wt.ap[:, k, mi * 128:(mi + 1) * 128], rhs_fn(kc0 + k, s).ap,
                                       pi == 0 and k == 0, pi == len(parts) - 1 and k == nkc - 1) for k in range(nkc)]
                            pe(instrs, [wt] + [rhs_fn(kc0 + k, s) for k in range(nkc)], [banks[mi][s]])
                for mi in range(2):
                    for s in CS[0]:
                        evac(banks[mi][s], mg * 2 + mi, s)

        def ffn(l):
            mark("ffn_in")
            def actv(c, s):
                return Rb((c * SUB + s) * 512, 512)
            for g0 in range(0, 11, 2):
              grp = list(range(g0, min(g0 + 2, 11)))
              wts = {mg: (wload(fwg_d[l, :, mg * 256:(mg + 1) * 256], KC, 256),
                          wload(fwu_d[l, :, mg * 256:(mg + 1) * 256], KC, 256)) for mg in grp}
              for s in CS[0]:
                for mg in grp:
                    wg, wu = wts[mg]
                    for mi in range(2):
                        ba = nb()
                        mm(ba, [(T(wg.ap[:, k, mi * 128:(mi + 1) * 128], wg.keys), hv(k, s)) for k in range(KC)],
                           split=(mg == 0 and mi == 0))
                        bu = nb()
                        mm(bu, [(T(wu.ap[:, k, mi * 128:(mi + 1) * 128], wu.keys), hv(k, s)) for k in range(KC)])
                        tm = ntmp()
                        act(tm, ba, AF.Silu)
                        tt(actv(mg * 2 + mi, s), tm, bu, ALU.mult)
            mark("ffn_down")
            proj_acc(lambda kc0, nkc, mg: fwd_d[l, kc0 * 128:(kc0 + nkc) * 128, mg * 256:(mg + 1) * 256],
                     [(0, 6), (6, 5), (11, 6), (17, 5)], 4, actv, resid_add)

        def out_proj(w_d2, nk, rhs_fn):
            mark("out_proj")
            proj_acc(lambda kc0, nkc, mg: w_d2[kc0 * 128:(kc0 + nkc) * 128, mg * 256:(mg + 1) * 256],
                     [(p * 8, 8) for p in range(nk // 8)], 4, rhs_fn, resid_add)

        def conv_mixer(j, tail_only=False):
            CZS = 2 * (G + 4)
            mark("conv_in")

            def czv(fc, c0, n):
                off = fc * CZS + 2 * c0
                return Rf(off, n)
            YO = KC * CZS

            def yv(kc, s):
                return Rb(YO + (kc * SUB + s) * 512, 512)
            HK = T(halo[:, j, :, :], [("halo", j)])
            for fc in range(KC):
                cp(czv(fc, 2, 2), T(halo[:, j, fc, :], HK.keys), eng="act" if False else "dve")
            for fg in range(4):
                wc = wload(cwin_d[j, :, D + fg * 256:D + (fg + 1) * 256], KC, 256)
                wz = wload(cwin_d[j, :, 2 * D + fg * 256:2 * D + (fg + 1) * 256], KC, 256)
                wb = None if tail_only else wload(cwin_d[j, :, fg * 256:(fg + 1) * 256], KC, 256)
                for s in CS[0]:
                    for mi in range(2):
                        fc = fg * 2 + mi
                        bc = nb()
                        mm(bc, [(T(wc.ap[:, k, mi * 128:(mi + 1) * 128], wc.keys), hv(k, s)) for k in range(KC)],
                           split=(fg == 0 and mi == 0))
                        bz = nb()
                        mm(bz, [(T(wz.ap[:, k, mi * 128:(mi + 1) * 128], wz.keys), hv(k, s)) for k in range(KC)])
                        tm = ntmp()
                        act(tm, bc, AF.Copy)
                        tt(czv(fc, 4 + s * 512, 512), tm, bz, ALU.mult)
                        if tail_only:
                            continue
                        bb = nb()
                        mm(bb, [(T(wb.ap[:, k, mi * 128:(mi + 1) * 128], wb.keys), hv(k, s)) for k in range(KC)])
                        t1 = ntmp()
                        c0 = 4 + s * 512
                        ts(t1, czv(fc, c0, 512), gT[:, fc, 9 + j * 3 + 2:9 + j * 3 + 3], None, ALU.mult, extra_reads=[GT])
                        stt(t1, czv(fc, c0 - 1, 512), gT[:, fc, 9 + j * 3 + 1:9 + j * 3 + 2], t1, ALU.mult, ALU.add, extra_reads=[GT])
                        stt(t1, czv(fc, c0 - 2, 512), gT[:, fc, 9 + j * 3:9 + j * 3 + 1], t1, ALU.mult, ALU.add, extra_reads=[GT])
                        tt(yv(fc, s), t1, bb, ALU.mult)
            for fc in range(KC):
                cp(T(halo[:, j, fc, :], HK.keys), czv(fc, 2 + G, 2))
            if not tail_only:
                out_proj(cwout_d[j], KC, yv)

        def sgu_mixer():
            def uv(c, s):
                return Rb((c * SUB + s) * 512, 512)
            NVS = 2
            VO = 16 * SUB * 512

            def vtok(slot):
                return Rf(VO + slot * 4096, 2048)
            dma_multi("sp", "mc", [(mixc[:, 0:2048], C_dram[:, :]),
                                   (mixc[:, 2048:4096], slg_d[0:1, :].partition_broadcast(128))],
                      reads=[CD], writes=[MIXC])
            mark("sgu_u")
            for mg in range(8):
                wt = wload(swin_d[0, :, mg * 256:(mg + 1) * 256], KC, 256)
                proj_fm(wt, 2, hv, KC, lambda bk, mi, s, mg=mg: act(uv(mg * 2 + mi, s), bk, AF.Gelu), first=(mg == 0))
            for q0 in range(0, NCH, NVS):
                mark("sgu_v%d" % q0)
                sums = {}
                for vt in range(4):
                    wt = wload2(swin_d[0, :, 2048 + vt * 512:2048 + (vt + 1) * 512])
                    for ci in range(NVS):
                        ch = q0 + ci
                        bk = nb()
                        mm(bk, [(hvc(k, ch), T(wt.ap[:, k, :], wt.keys)) for k in range(KC)])
                        vt_t = vtok(ci)
                        col = ci * 16 + vt
                        act(T(vt_t.ap[:, vt * 512:(vt + 1) * 512], vt_t.keys), bk, AF.Gelu,
                            accum=T(st[:, col:col + 1], [("st", col)]))
                for ci in range(NVS):
                    ch = q0 + ci
                    vt_t = vtok(ci)
                    b0 = ci * 16
                    STc = lambda a, n=1: T(st[:, b0 + a:b0 + a + n], [("st", b0 + a + i_) for i_ in range(n)])
                    vh = T(vhb[:, ch % 2, :], [("vhb", ch % 2)]) if SUB == 1 else Rb(24576 + (ch % 2) * 2048, 2048)
                    act(vh, vt_t, AF.Square, accum=STc(4))
                    rec.emit("dve", lambda e, a=STc(5), b=STc(0, 4): e.tensor_reduce(out=a.ap, in_=b.ap, axis=mybir.AxisListType.X, op=ALU.add),
                             [STc(0, 4)], [STc(5)])
                    ts(STc(5), STc(5), 1.0 / 2048.0, None, ALU.mult)
                    tt(STc(6), STc(5), STc(5), ALU.mult)
                    stt(STc(7), STc(4), 1.0 / 2048.0, STc(6), ALU.mult, ALU.subtract)
                    act(STc(7), STc(7), AF.Ln, bias=EPSL, extra_reads=[EPST])
                    act(STc(7), STc(7), AF.Exp, scale=-0.5)
                    stt(STc(8), STc(5), -1.0, STc(7), ALU.mult, ALU.mult)
                    act(vt_t, vt_t, AF.Identity, bias=st[:, b0 + 8:b0 + 9], scale=st[:, b0 + 7:b0 + 8], extra_reads=[STc(7), STc(8)])
                    tt(vh, vt_t, T(mixc[:, 2048:4096], MIXC.keys), ALU.mult)
                    s_ = ch // 4
                    cc = ch % 4
                    for b4 in range(4):
                        bk = nb()
                        pe([("mm", bk.ap[:, i_ * 128:(i_ + 1) * 128], vh.ap[:, (b4 * 4 + i_) * 128:(b4 * 4 + i_ + 1) * 128],
                             WT[:, (b4 * 4 + i_) // 2, :], True, True) for i_ in range(4)], [vh, WTt], [bk])
                        tm = ntmp()
                        tt(tm, bk, T(mixc[:, b4 * 512:(b4 + 1) * 512], MIXC.keys), ALU.add)
                        tm3 = T(tm.ap.rearrange("p (c t) -> p c t", c=4), tm.keys)
                        u0 = b4 * 4 * SUB * 512
                        u4 = T(R[:, u0:u0 + 4 * SUB * 512].rearrange("p (c s t) -> p c s t", c=4, s=SUB)[:, :, s_, cc * 128:(cc + 1) * 128],
                               rkeys(u0, 4 * SUB * 512))
                        tt(u4, tm3, u4, ALU.mult)
            out_proj(swout_d[0], 16, uv)

        def ret_mixer(g, state_only, fsubs=None):
            allsubs = list(range(SUB))
            fsubs = allsubs if fsubs is None else fsubs
            def qv(c, s):
                return Rb((c * SUB + s) * 512, 512)

            def kv(c, s):
                return Rb(KC * SUB * 512 + (c * SUB + s) * 512, 512)
            VO = 2 * KC * SUB * 512

            def vv(ch):
                return Rb(VO + ch * 2048, 2048)
            dma_multi("sp", "mc", [(mixc[:, 0:G], cos_d[:, g * G:(g + 1) * G]),
                                   (mixc[:, 1024:1024 + G], sin_d[:, g * G:(g + 1) * G]),
                                   (mixc[:, 2048:3072], kdec_d[:, :]),
                                   (mixc[:, 3072:3584], dT_d[:, :])], writes=[MIXC])

            def cosv(s):
                return T(mixc[:, s * 512:(s + 1) * 512], MIXC.keys)

            def sinv(s):
                return T(mixc[:, 1024 + s * 512:1024 + (s + 1) * 512], MIXC.keys)

            def rot_proj(col0, dst):
                for h in range(HEADS):
                    wt = wload(rwin_d[0, :, col0 + h * 256:col0 + (h + 1) * 256], KC, 256)
                    for hh in range(1):
                        for s in CS[0]:
                            ba = nb()
                            mm(ba, [(T(wt.ap[:, k, (hh * 2) * 128:(hh * 2 + 1) * 128], wt.keys), hv(k, s)) for k in range(KC)],
                               split=(h == 0 and col0 == D))
                            bb = nb()
                            mm(bb, [(T(wt.ap[:, k, (hh * 2 + 1) * 128:(hh * 2 + 2) * 128], wt.keys), hv(k, s)) for k in range(KC)])
                            t1, t2 = ntmp(), ntmp()
                            tt(t1, ba, cosv(s), ALU.mult)
                            tt(t2, bb, sinv(s), ALU.mult)
                            tt(dst(2 * h, s), t1, t2, ALU.subtract)
                            t3, t4 = ntmp(), ntmp()
                            tt(t3, bb, cosv(s), ALU.mult)
                            tt(t4, ba, sinv(s), ALU.mult)
                            tt(dst(2 * h + 1, s), t3, t4, ALU.add)
            mark("ret_k")
            CS[0] = allsubs
            rot_proj(D, kv)
            if not state_only:
                mark("ret_q")
                CS[0] = fsubs
                rot_proj(0, qv)
            CS[0] = allsubs
            mark("ret_v")
            for h in range(HEADS):
                wt = wload2(rwin_d[0, :, 2 * D + h * 512:2 * D + (h + 1) * 512])
                for ch in range(NCH):
                    bk = nb()
                    mm(bk, [(hvc(k, ch), T(wt.ap[:, k, :], wt.keys)) for k in range(KC)])
                    v_ = vv(ch)
                    act(T(v_.ap[:, h * 512:(h + 1) * 512], v_.keys), bk, AF.Copy)
            Skeys = lambda i: [("S32", i)]
            if not state_only:
                for i in range(8):
                    act(T(Sbf[:, i, :], [("Sbf", i)]), T(S32[:, i, :], Skeys(i)), AF.Copy)
            for ch in range(NCH):
                mark("ret_core%d" % ch)
                s_, cc = ch // 4, ch % 4
                v_ = vv(ch)
                tb = ntb()
                pe([("tr", tb.ap[:, c * 128:(c + 1) * 128], kv(c, s_).ap[:, cc * 128:(cc + 1) * 128], identb[:]) for c in range(KC)],
                   [kv(c, s_) for c in range(KC)] + [IDB], [tb])
                kdt = T(kd[:, ch % 2, :], [("kd", ch % 2)])
                tt(kdt, tb, T(mixc[:, 2048:3072], MIXC.keys), ALU.mult)
                full_ch = (not state_only) and (s_ in fsubs)
                if full_ch:
                    bs_ = nb()
                    instrs = []
                    for h in range(HEADS):
                        for i in range(2):
                            c = 2 * h + i
                            instrs.append(("mm", bs_.ap[:, h * 128:(h + 1) * 128], kv(c, s_).ap[:, cc * 128:(cc + 1) * 128],
                                           qv(c, s_).ap[:, cc * 128:(cc + 1) * 128], i == 0, i == 1))
                    pe(instrs, [kv(c, s_) for c in range(KC)] + [qv(c, s_) for c in range(KC)], [bs_])
                    sT = T(sTb[:, ch % 2, :], [("sTb", ch % 2)])
                    tt(sT, bs_, T(mixc[:, 3072:3584], MIXC.keys), ALU.mult)
                    obanks = []
                    for h in range(HEADS):
                        bo = nb()
                        instrs = [("mm", bo.ap, sT.ap[:, h * 128:(h + 1) * 128], v_.ap[:, h * 512:(h + 1) * 512], True, False)]
                        rd = [sT, v_]
                        for i in range(2):
                            c = 2 * h + i
                            instrs.append(("mm", bo.ap, qv(c, s_).ap[:, cc * 128:(cc + 1) * 128], Sbf[:, c, :], False, i == 1))
                            rd += [qv(c, s_), T(Sbf[:, c, :], [("Sbf", c)])]
                        pe(instrs, rd, [bo])
                        obanks.append(bo)
                if full_ch:
                    onb = []
                    for h in range(HEADS):
                        bo = obanks[h]
                        col = 32 + h * 4
                        ssq = T(st[:, col:col + 1], [("st", col)])
                        rr = T(st[:, col + 1:col + 2], [("st", col + 1)])
                        on = T(onbuf[:, h, :], [("on", h)])
                        act(on, bo, AF.Square, scale=qdec[:, h:h + 1], accum=ssq, extra_reads=[QDEC])
                        act(rr, ssq, AF.Ln, bias=EPSR, scale=1.0 / 512.0, extra_reads=[EPST])
                        act(rr, rr, AF.Exp, scale=-0.5)
                        tt(rr, rr, T(qdec[:, h:h + 1], QDEC.keys), ALU.mult)
                        act(on, bo, AF.Copy, scale=st[:, col + 1:col + 2], extra_reads=[rr])
                        onb.append(on)
                for c in range(8):
                    h = c // 2
                    bk = nb()
                    mm(bk, [(T(kdt.ap[:, c * 128:(c + 1) * 128], kdt.keys), T(v_.ap[:, h * 512:(h + 1) * 512], v_.keys))])
                    Sc = T(S32[:, c, :], Skeys(c))
                    stt(Sc, Sc, cdec[h], bk, ALU.mult, ALU.add)
                    if not state_only:
                        act(T(Sbf[:, c, :], [("Sbf", c)]), Sc, AF.Copy)
                if not full_ch:
                    continue
                for hp in range(2):
                    tb = ntb()
                    instrs = []
                    for hh in range(2):
                        for e4 in range(4):
                            instrs.append(("tr", tb.ap[:, (hh * 4 + e4) * 128:(hh * 4 + e4 + 1) * 128],
                                           onb[hp * 2 + hh].ap[:, e4 * 128:(e4 + 1) * 128], identb[:]))
                    pe(instrs, [onb[hp * 2], onb[hp * 2 + 1], IDB], [tb])
                    cp(T(v_.ap[:, hp * 1024:(hp + 1) * 1024], v_.keys), tb)
            if state_only:
                return
            mark("ret_gate")
            CS[0] = fsubs
            def ov(c, s):
                ap = R[:, VO + s * 4 * 2048:VO + (s + 1) * 4 * 2048].rearrange("p (ch c t) -> p ch c t", ch=4, c=16)[:, :, c, :]
                return T(ap, rkeys(VO + s * 4 * 2048, 4 * 2048))
            for mg in range(8):
                wt = wload(rwin_d[0, :, 4 * D + mg * 256:4 * D + (mg + 1) * 256], KC, 256)

                def evac(bk, mi, s, mg=mg):
                    tm = ntmp()
                    act(tm, bk, AF.Silu)
                    o_ = ov(mg * 2 + mi, s)
                    tt(o_, T(tm.ap.rearrange("p (ch t) -> p ch t", ch=4), tm.keys), o_, ALU.mult)
                proj_fm(wt, 2, hv, KC, evac)
            out_proj(rwout_d[0], 16, ov)
            CS[0] = allsubs

        XO = RN - 4096
        for g in range(NG):
            if g < npre - 1:
                mode = "pre"
            elif g == npre - 1:
                mode = "pre_last"
            else:
                mode = "full"
            mark("G%d_%s_load" % (g, mode))
            for ch in range(NCH):
                sl = ch % 2
                xs = Rf(XO + sl * 2048, 1024)
                tok0 = g * G + ch * 128
                dma("sp", "xs%d" % sl, xs.ap, x_d[tok0:tok0 + 128, :], writes=[xs])
                for half in range(2):
                    bk = nb()
                    pe([("tr", bk.ap[:, j * 128:(j + 1) * 128], xs.ap[:, (half * 4 + j) * 128:(half * 4 + j + 1) * 128], ident[:])
                        for j in range(4)], [xs, IDF], [bk])
                    s_ = ch // 4
                    dst = T(xT[:, half * 4:half * 4 + 4, ch * 128:(ch + 1) * 128], [("x", half * 4 + j, s_) for j in range(4)])
                    rec.emit("act" if half else "dve",
                             (lambda e, d=dst, b=bk: e.activation(out=d.ap, in_=b.ap.rearrange("p (c t) -> p c t", c=4), func=AF.Copy)) if half else
                             (lambda e, d=dst, b=bk: e.tensor_copy(out=d.ap, in_=b.ap.rearrange("p (c t) -> p c t", c=4))),
                             [bk], [dst])
            for l in range(DBG_NL):
                kind = l % 3
                if mode == "pre" and l == 3:
                    break
                rmsnorm(l, hv)
                if kind == 0:
                    conv_mixer(l // 3, tail_only=(mode == "pre_last" and l == 3))
                    if mode == "pre_last" and l == 3:
                        break
                elif kind == 1:
                    sgu_mixer()
                else:
                    ret_mixer(g, state_only=(mode == "pre"), fsubs=([SUB - 1] if mode == "pre_last" else None))
                    if mode == "pre":
                        break
                    if mode == "pre_last":
                        CS[0] = [SUB - 1]
                if DBG_FFN:
                    rmsnorm(4 + l, hv)
                    ffn(l)
            CS[0] = list(range(SUB))
            if mode != "full":
                continue
            def fv(kc, s):
                return Rf((kc * SUB + s) * 1024, 512)
            mark("final")
            rmsnorm(8, fv)
            for ch in range(NCH):
                s_, cc = ch // 4, ch % 4
                sl = ch % 2
                os_ = Rf(XO + sl * 2048, 1024)
                for half in range(2):
                    bk = nb()
                    pe([("tr", bk.ap[:, j * 128:(j + 1) * 128], fv(half * 4 + j, s_).ap[:, cc * 128:(cc + 1) * 128], ident[:])
                        for j in range(4)], [fv(half * 4 + j, s_) for j in range(4)] + [IDF], [bk])
                    dst = T(os_.ap[:, half * 512:(half + 1) * 512], os_.keys)
                    if half:
                        act(dst, bk, AF.Copy)
                    else:
                        cp(dst, bk)
                tok0 = (g - npre) * G + ch * 128
                dma("sp", "os%d" % sl, y_d[tok0:tok0 + 128, :], os_.ap, reads=[os_])
        rec.final_wait("sp", ["os0", "os1"])
        with nc.Block() as block:
            rec.run(block)
    return nc


def _tables(positions):
    half = 128
    inv_freq = (10000.0 ** (-(np.arange(half, dtype=np.float32) / np.float32(half)))).astype(np.float32)
    ang = (positions.astype(np.float32)[None, :] * inv_freq[:, None]).astype(np.float32)
    return np.cos(ang).astype(np.float32), np.sin(ang).astype(np.float32)


def _const_tables():
    gam = 1.0 - 2.0 ** (-5.0 - np.arange(HEADS, dtype=np.float64))
    lg = np.log(gam)
    idx = np.arange(128, dtype=np.float64)
    dT = np.zeros((128, HEADS, 128), np.float64)
    for h in range(HEADS):
        kf = np.exp(-(idx + 1.0) * lg[h]) / 16.0
        dT[:, h, :] = (idx[:, None] <= idx[None, :]) * kf[:, None]
    kdec = np.zeros((128, HEADS, 256), np.float64)
    for h in range(HEADS):
        kdec[:, h, :] = (np.exp((127.0 - idx) * lg[h]) / 16.0)[:, None]
    qdec = np.exp((idx[:, None] + 1.0) * lg[None, :])
    tri = (idx[:, None] <= idx[None, :]).astype(np.float32)
    return (dT.reshape(128, 512).astype(np.float32), kdec.reshape(128, 1024).astype(np.float32),
            qdec.astype(np.float32), np.eye(128, dtype=np.float32), tri)


_NC_CACHE = {}


def run_trunk(inputs, seq, npre, nseg, SUB, n_cores):
    G = SUB * 512
    half = seq // 2
    assert npre * G == half and nseg * G == half
    key = (npre, nseg, SUB, DBG_NL, DBG_FFN)
    if key not in _NC_CACHE:
        _NC_CACHE[key] = build(npre, nseg, SUB)
    nc = _NC_CACHE[key]
    x = np.ascontiguousarray(np.asarray(inputs["x"], dtype=np.float32))
    dT, kdec, qdec, ident, tri = _const_tables()
    shared = {k: np.ascontiguousarray(np.asarray(v, dtype=np.float32)) for k, v in inputs.items() if k != "x"}
    shared.update({"t_dT": dT, "t_kdec": kdec, "t_qdec": qdec, "t_ident": ident, "t_tri": tri})
    in_maps = []
    for c in range(n_cores):
        s, p = c // 2, c % 2
        if p == 0:
            xc = np.concatenate([np.zeros((half, D), np.float32), x[s, :half]], axis=0)
            pos = np.arange(-half, half)
        else:
            xc = x[s]
            pos = np.arange(0, seq)
        cos, sin = _tables(pos)
        m = dict(shared)
        m.update({"x": np.ascontiguousarray(xc), "t_cos": cos, "t_sin": sin})
        in_maps.append(m)
    res = run_bass_kernel_spmd(nc, in_maps, core_ids=list(range(n_cores)))
    out = np.zeros((n_cores // 2, seq, D), np.float32)
    for c in range(n_cores):
        s, p = c // 2, c % 2
        out[s, p * half:(p + 1) * half] = res.results[c]["y"]
    return out


SUB_CFG = 2
DBG_NL = 4
MARKS = []
DBG_FFN = True


def kernel(**inputs):
    G = SUB_CFG * 512
    n = (SEQ // 2) // G
    return run_trunk(inputs, SEQ, n, n, SUB_CFG, 8)
```

```python
import numpy as np
from contextlib import ExitStack
import concourse.bass as bass
import concourse.mybir as mybir
from concourse.bass_utils import run_bass_kernel_spmd

F32 = mybir.dt.float32
BF16 = mybir.dt.bfloat16
AF = mybir.ActivationFunctionType
ALU = mybir.AluOpType

D = 1024
KC = 8
DFF = 2816
FC = 22
RMS_EPS = 1e-6
LN_EPS = 1e-5
HEADS = 4
SEQ = 8192
BATCH = 4
COMPUTE = ("pe", "act", "dve")


class T:
    __slots__ = ("ap", "keys")

    def __init__(self, ap, keys):
        self.ap = ap
        self.keys = tuple(keys)


class Rec:
    def __init__(self, nc, es):
        self.nc = nc
        self.es = es
        self.ops = {e: [] for e in ("pe", "act", "dve", "pool", "sp")}
        self.sem = {}
        self.cnt = {}
        for e in COMPUTE:
            self.sem[e] = es.enter_context(nc.semaphore("s_" + e))
            self.cnt[e] = 0
        self.waited = {}
        self.lastw = {}
        self.readers = {}

    def stream(self, name):
        self.sem[name] = self.es.enter_context(self.nc.semaphore(name))
        self.cnt[name] = 0
        return name

    def _need(self, waits, eng, prod, val):
        if prod == eng and eng == "pe":
            return
        k = (eng, prod)
        if self.waited.get(k, 0) >= val:
            return
        self.waited[k] = val
        waits.append((self.sem[prod], val * (1 if prod in COMPUTE else 16)))

    def emit(self, eng, fn, reads, writes, prod=None, n=1):
        prod = prod or eng
        waits = []
        for t in reads:
            for k in t.keys:
                lw = self.lastw.get(k)
                if lw:
                    self._need(waits, eng, *lw)
        for t in writes:
            for k in t.keys:
                lw = self.lastw.get(k)
                if lw:
                    self._need(waits, eng, *lw)
                for p, v in self.readers.get(k, {}).items():
                    self._need(waits, eng, p, v)
        self.cnt[prod] += n
        val = self.cnt[prod]
        for t in writes:
            for k in t.keys:
                self.lastw[k] = (prod, val)
                self.readers[k] = {}
        for t in reads:
            for k in t.keys:
                self.readers.setdefault(k, {})[prod] = val
        self.ops[eng].append((waits, fn, self.sem[prod], 1 if prod in COMPUTE else 16))

    def final_wait(self, eng, prods):
        waits = [(self.sem[p], self.cnt[p] * (1 if p in COMPUTE else 16)) for p in prods if self.cnt[p]]
        self.ops[eng].append((waits, None, None, 0))

    def run(self, block):
        def mk(e):
            def body(eng):
                for waits, fn, sem, inc in self.ops[e]:
                    for sm, v in waits:
                        eng.wait_ge(sm, v)
                    if fn is None:
                        continue
                    r = fn(eng)
                    if isinstance(r, (list, tuple)):
                        for ins in r:
                            ins.then_inc(sem, inc)
                    else:
                        r.then_inc(sem, inc)
            return body
        block.tensor(mk("pe"))
        block.scalar(mk("act"))
        block.vector(mk("dve"))
        block.gpsimd(mk("pool"))
        block.sync(mk("sp"))


def build(npre, nseg, SUB):
    G = SUB * 512
    NCH = G // 128
    NG = npre + nseg
    NTOK = NG * G
    RN = 16384 * SUB
    nc = bass.Bass("TRN2", target_bir_lowering=False)

    def din(name, shape):
        return nc.dram_tensor(name, list(shape), F32, kind="ExternalInput").ap()

    x_d = din("x", [NTOK, D])
    nmg_d = din("norm_mix_g", [4, D])
    nfg_d = din("norm_ffn_g", [4, D])
    fng_d = din("final_norm_g", [D])
    cwin_d = din("conv_w_in", [2, D, 3 * D])
    cw_d = din("conv_w", [2, 3, D])
    cwout_d = din("conv_w_out", [2, D, D])
    swin_d = din("sgu_w_in", [1, D, 4096])
    slg_d = din("sgu_ln_g", [1, 2048])
    slb_d = din("sgu_ln_b", [1, 2048])
    sws_d = din("sgu_w_s", [1, 8, 128, 128])
    sbs_d = din("sgu_b_s", [1, 8, 128])
    swout_d = din("sgu_w_out", [1, 2048, D])
    rwin_d = din("ret_w_in", [1, D, 6 * D])
    rwout_d = din("ret_w_out", [1, 2048, D])
    fwg_d = din("ffn_w_gate", [4, D, DFF])
    fwu_d = din("ffn_w_up", [4, D, DFF])
    fwd_d = din("ffn_w_down", [4, DFF, D])
    cos_d = din("t_cos", [128, NTOK])
    sin_d = din("t_sin", [128, NTOK])
    dT_d = din("t_dT", [128, 512])
    kdec_d = din("t_kdec", [128, 1024])
    qdec_d = din("t_qdec", [128, 4])
    id_d = din("t_ident", [128, 128])
    tri_d = din("t_tri", [128, 128])
    y_d = nc.dram_tensor("y", [nseg * G, D], F32, kind="ExternalOutput").ap()
    C_dram = nc.dram_tensor("c_scr", [128, 2048], F32).ap()

    gam = [1.0 - 2.0 ** (-5.0 - h) for h in range(HEADS)]
    cdec = [float(np.exp(128.0 * np.log(g_))) for g_ in gam]

    with ExitStack() as es:
        def sb(name, shape, dt):
            return es.enter_context(nc.sbuf_tensor(name, list(shape), dt))

        def ps(name, shape, dt):
            return es.enter_context(nc.psum_tensor(name, list(shape), dt))

        xT = sb("xT", [128, KC, G], F32)
        hT = sb("hT", [128, KC, G], BF16)
        R = sb("R", [128, RN], BF16)
        S32 = sb("S32", [128, 8, 512], F32)
        Sbf = sb("Sbf", [128, 8, 512], BF16)
        NW = 6
        wbuf = sb("wbuf", [128, NW, 2048], BF16)
        mixc = sb("mixc", [128, 4096], F32)
        sq = sb("sq", [128, 4, 512], BF16)
        rstd = sb("rstd", [128, 2, 512], F32)
        NTMP = 4
        tmp = sb("tmp", [128, NTMP, 512], F32)
        kd = sb("kd", [128, 2, 1024], BF16)
        sTb = sb("sTb", [128, 2, 512], BF16)
        vhb = sb("vhb", [128, 2, 2048], BF16) if SUB == 1 else None
        WT = sb("WT", [128, 8, 128], BF16)
        ident = sb("ident", [128, 128], F32)
        identb = sb("identb", [128, 128], BF16)
        onesb = sb("onesb", [128, 128], BF16)
        gT = sb("gT", [128, KC, 32], F32)
        halo = sb("halo", [128, 2, KC, 2], F32)
        qdec = sb("qdec", [128, 4], F32)
        st = sb("st", [128, 64], F32)
        onbuf = sb("onbuf", [128, 4, 512], BF16)

        NB = 8
        pb = [ps("pb%d" % i, [128, 512], F32) for i in range(NB)]

        rec = Rec(nc, es)
        for nm in ["w0", "w1", "w2", "w3", "w4", "w5", "xs0", "xs1", "os0", "os1", "cst", "mc", "cs"]:
            rec.stream(nm)

        def rkeys(off, n):
            return [("R", i) for i in range(off // 512, (off + n - 1) // 512 + 1)]

        def Rb(off, n):
            return T(R[:, off:off + n], rkeys(off, n))

        def Rf(off, n):
            return T(R[:, off:off + 2 * n].bitcast(F32), rkeys(off, 2 * n))

        bank_i = [0]

        def nb():
            i = bank_i[0]
            bank_i[0] = (i + 1) % NB
            return T(pb[i][:], [("pb", i)])

        tb_i = [0]

        def ntb():
            b = nb()
            return T(b.ap.bitcast(BF16), b.keys)

        tmp_i = [0]

        def ntmp():
            i = tmp_i[0]
            tmp_i[0] = (i + 1) % NTMP
            return T(tmp[:, i, :], [("tmp", i)])

        sq_i = [0]

        def nsq():
            i = sq_i[0]
            sq_i[0] = (i + 1) % 4
            return T(sq[:, i, :], [("sq", i)])

        def V(ap, *keys):
            return T(ap, keys)

        def sub_of(t0):
            return t0 // 512

        def xv(kc, s):
            return T(xT[:, kc, s * 512:(s + 1) * 512], [("x", kc, s)])

        def hv(kc, s):
            return T(hT[:, kc, s * 512:(s + 1) * 512], [("h", kc, s)])

        def hvc(kc, ch):
            return T(hT[:, kc, ch * 128:(ch + 1) * 128], [("h", kc, ch // 4)])

        PE_N = [0]
        CS = [list(range(SUB))]

        def mark(label):
            MARKS.append((PE_N[0], label))

        def pe(instrs, reads, writes):
            instrs = list(instrs)
            def fn(eng):
                last = None
                for it in instrs:
                    if it[0] == "mm":
                        _, o, l, r, s0, s1 = it
                        last = eng.matmul(out=o, lhsT=l, rhs=r, start=s0, stop=s1)
                    else:
                        _, o, i_, idn = it
                        last = eng.transpose(out=o, in_=i_, identity=idn)
                return last
            rec.emit("pe", fn, reads, writes)
            PE_N[0] += len(instrs)

        def mm(out, pairs, split=False):
            n = len(pairs)
            if split:
                for i, (l, r) in enumerate(pairs):
                    pe([("mm", out.ap, l.ap, r.ap, i == 0, i == n - 1)], [l, r], [out])
                return
            instrs = [("mm", out.ap, l.ap, r.ap, i == 0, i == n - 1) for i, (l, r) in enumerate(pairs)]
            reads = [t for p in pairs for t in p]
            pe(instrs, reads, [out])

        def act(out, in_, func, bias=None, scale=None, accum=None, extra_reads=()):
            kw = {}
            if bias is not None:
                kw["bias"] = bias
            if scale is not None:
                kw["scale"] = scale
            if accum is not None:
                kw["accum_out"] = accum.ap
            rec.emit("act", lambda e: e.activation(out=out.ap, in_=in_.ap, func=func, **kw),
                     [in_] + list(extra_reads), [out] + ([accum] if accum is not None else []))

        def tt(out, a, b, op, eng="dve"):
            rec.emit(eng, lambda e: e.tensor_tensor(out=out.ap, in0=a.ap, in1=b.ap, op=op), [a, b], [out])

        def ts(out, a, s1, s2, op0, op1=None, extra_reads=(), eng="dve"):
            if op1 is None:
                rec.emit(eng, lambda e: e.tensor_scalar(out=out.ap, in0=a.ap, scalar1=s1, scalar2=None, op0=op0),
                         [a] + list(extra_reads), [out])
            else:
                rec.emit(eng, lambda e: e.tensor_scalar(out=out.ap, in0=a.ap, scalar1=s1, scalar2=s2, op0=op0, op1=op1),
                         [a] + list(extra_reads), [out])

        def stt(out, a, sc, b, op0, op1, extra_reads=(), eng="dve"):
            rec.emit(eng, lambda e: e.scalar_tensor_tensor(out=out.ap, in0=a.ap, scalar=sc, in1=b.ap, op0=op0, op1=op1),
                     [a, b] + list(extra_reads), [out])

        def recip(out, in_):
            rec.emit("dve", lambda e: e.reciprocal(out=out.ap, in_=in_.ap), [in_], [out])

        def cp(out, in_, eng="dve"):
            rec.emit(eng, lambda e: e.tensor_copy(out=out.ap, in_=in_.ap), [in_], [out])

        def mset(out, val, eng="dve"):
            rec.emit(eng, lambda e: e.memset(out.ap, val), [], [out])

        def dma(eng, stream, out, in_, reads=(), writes=()):
            rec.emit(eng, lambda e: e.dma_start(out=out, in_=in_), list(reads), list(writes), prod=stream)

        def dma_multi(eng, stream, pairs, reads=(), writes=()):
            def fn(e):
                return [e.dma_start(out=o, in_=i) for o, i in pairs]
            rec.emit(eng, fn, list(reads), list(writes), prod=stream, n=len(pairs))

        w_i = [0]

        def wload(src2d, kcs, cols):
            i = w_i[0]
            w_i[0] = (i + 1) % NW
            dst = wbuf[:, i, 0:kcs * cols].rearrange("p (k m) -> p k m", k=kcs)
            t = T(dst, [("w", i)])
            dma("pool", "w%d" % i, dst, src2d.rearrange("(k p) m -> p k m", p=128), writes=[t])
            return t

        def wload2(src2d):
            if w_i[0] % 2:
                w_i[0] = (w_i[0] + 1) % NW
            i = w_i[0]
            w_i[0] = (i + 2) % NW
            dst = wbuf[:, i:i + 2, :].rearrange("p a (k m) -> p (a k) m", m=512)
            t = T(dst, [("w", i), ("w", i + 1)])
            dma("pool", "w%d" % i, dst, src2d.rearrange("(k p) m -> p k m", p=128), writes=[t])
            return t

        GT = T(gT[:], [("gT",)])
        IDF = T(ident[:], [("ident",)])
        IDB = T(identb[:], [("identb",)])
        ONESB = T(onesb[:], [("onesb",)])
        MIXC = T(mixc[:], [("mixc",)])
        QDEC = T(qdec[:], [("qdec",)])
        epst = sb("epst", [128, 2], F32)
        EPSR = epst[:, 0:1]
        EPSL = epst[:, 1:2]
        EPST = T(epst[:], [("eps", 0), ("eps", 1)])

        vst = Rf(0, 1024)
        vst32 = T(vst.ap[0:32, :], vst.keys)
        mset(vst32, 0.0)
        wsn = Rf(2048, 1024)
        bsb = Rf(4096, 1024)
        tri = Rf(6144, 128)
        onesf = Rf(6656, 128)
        WTf = Rf(7168, 1024)
        Cs = Rf(9216, 2048)
        pre = [
            (ident[:], id_d[:, :]),
            (qdec[:], qdec_d[:, :]),
            (vst.ap[0:4, :], nmg_d[:, :]),
            (vst.ap[4:8, :], nfg_d[:, :]),
            (vst.ap[8:9, :], fng_d.rearrange("(o d) -> o d", o=1)),
            (vst.ap[9:15, :], cw_d.rearrange("j k d -> (j k) d")),
            (vst.ap[16:18, :], slb_d.rearrange("o (r d) -> (o r) d", r=2)),
            (wsn.ap.rearrange("p (g s) -> p g s", g=8), sws_d.rearrange("o g t s -> t (o g) s")),
            (bsb.ap, sbs_d.rearrange("o g t -> o (g t)").partition_broadcast(128)),
            (tri.ap, tri_d[:, :]),
        ]
        rec.emit("sp", lambda e: [e.dma_start(out=o, in_=i) for o, i in pre], [vst32],
                 [IDF, QDEC, vst, wsn, bsb, tri], prod="cst", n=len(pre))
        cp(IDB, IDF)
        mset(ONESB, 1.0 / 1024.0)
        mset(onesf, 1.0)
        mset(T(epst[:, 0:1], [("eps", 0)]), RMS_EPS)
        mset(T(epst[:, 1:2], [("eps", 1)]), LN_EPS)
        mset(T(S32[:], [("S32", i) for i in range(8)]), 0.0)
        mset(T(Sbf[:], [("Sbf", i) for i in range(8)]), 0.0)
        mset(T(halo[:], [("halo", 0), ("halo", 1)]), 0.0)
        bk = nb()
        pe([("tr", bk.ap[:, kc * 32:(kc + 1) * 32], vst.ap[0:32, kc * 128:(kc + 1) * 128], ident[0:32, 0:32])
            for kc in range(KC)], [vst, IDF], [bk])
        act(T(gT[:].rearrange("p k r -> p (k r)"), GT.keys), T(bk.ap[:, 0:256], bk.keys), AF.Copy)
        for half in range(2):
            bk = nb()
            pe([("tr", bk.ap[:, j * 128:(j + 1) * 128], wsn.ap[:, (half * 4 + j) * 128:(half * 4 + j + 1) * 128], ident[:])
                for j in range(4)], [wsn, IDF], [bk])
            for j in range(4):
                g_ = half * 4 + j
                tt(T(WTf.ap[:, g_ * 128:(g_ + 1) * 128], WTf.keys), T(bk.ap[:, j * 128:(j + 1) * 128], bk.keys), tri, ALU.mult)
        cp(T(WT[:].rearrange("p g t -> p (g t)"), [("WT",)]), WTf)
        WTt = T(WT[:], [("WT",)])
        for half in range(2):
            bk = nb()
            pe([("mm", bk.ap[:, j * 128:(j + 1) * 128], onesf.ap, WTf.ap[:, (half * 4 + j) * 128:(half * 4 + j + 1) * 128], True, True)
                for j in range(4)], [onesf, WTf], [bk])
            for j in range(4):
                g_ = half * 4 + j
                for i in range(2):
                    dc = g_ * 2 + i
                    r_, kc_ = dc // 8, dc % 8
                    stt(T(Cs.ap[:, dc * 128:(dc + 1) * 128], Cs.keys), T(bk.ap[:, j * 128:(j + 1) * 128], bk.keys),
                        gT[:, kc_, 16 + r_:17 + r_], T(bsb.ap[:, g_ * 128:(g_ + 1) * 128], bsb.keys),
                        ALU.mult, ALU.add, extra_reads=[GT])
        CD = T(C_dram, [("Cdram",)])
        dma("sp", "cst", C_dram[:, :], Cs.ap, reads=[Cs], writes=[CD])

        def rmsnorm(col, out_fn, out_dtype_bf16=True):
            mark("norm%d" % col)
            for s in CS[0]:
                PST = nb()
                for kc in range(KC):
                    q_ = nsq()
                    act(q_, xv(kc, s), AF.Square)
                    pe([("mm", PST.ap, ONESB.ap, q_.ap, kc == 0, kc == KC - 1)], [ONESB, q_], [PST])
                rs = T(rstd[:, s % 2, :], [("rstd", s % 2)])
                act(rs, PST, AF.Ln, bias=EPSR, extra_reads=[EPST])
                act(rs, rs, AF.Exp, scale=-0.5)
                for kc in range(KC):
                    stt(out_fn(kc, s), xv(kc, s), gT[:, kc, col:col + 1], rs, ALU.mult, ALU.mult, extra_reads=[GT])

        def resid_add(bk, mc, s):
            tt(xv(mc, s), bk, xv(mc, s), ALU.add)

        def proj_fm(w_t, mcs, rhs_fn, nk, evac, first=False):
            for s in CS[0]:
                for mi in range(mcs):
                    bk = nb()
                    mm(bk, [(T(w_t.ap[:, k, mi * 128:(mi + 1) * 128], w_t.keys), rhs_fn(k, s)) for k in range(nk)],
                       split=(first and mi == 0))
                    evac(bk, mi, s)

        def proj_acc(w_fn, parts, ncol_groups, rhs_fn, evac):
            for mg in range(ncol_groups):
                banks = [{s: nb() for s in CS[0]} for mi in range(2)]
                for pi, (kc0, nkc) in enumerate(parts):
                    wt = wload(w_fn(kc0, nkc, mg), nkc, 256)
                    for mi in range(2):
                        for s in CS[0]:
                            instrs = [("mm", banks[mi][s].ap, wt.ap[:, k, mi * 128:(mi + 1) * 128], rhs_fn(kc0 + k, s).ap,
                                       pi == 0 and k == 0, pi == len(parts) - 1 and k == nkc - 1) for k in range(nkc)]
                            pe(instrs, [wt] + [rhs_fn(kc0 + k, s) for k in range(nkc)], [banks[mi][s]])
                for mi in range(2):
                    for s in CS[0]:
                        evac(banks[mi][s], mg * 2 + mi, s)

        def ffn(l):
            mark("ffn_in")
            def actv(c, s):
                return Rb((c * SUB + s) * 512, 512)
            for g0 in range(0, 11, 2):
              grp = list(range(g0, min(g0 + 2, 11)))
              wts = {mg: (wload(fwg_d[l, :, mg * 256:(mg + 1) * 256], KC, 256),
                          wload(fwu_d[l, :, mg * 256:(mg + 1) * 256], KC, 256)) for mg in grp}
              for s in CS[0]:
                for mg in grp:
                    wg, wu = wts[mg]
                    for mi in range(2):
                        ba = nb()
                        mm(ba, [(T(wg.ap[:, k, mi * 128:(mi + 1) * 128], wg.keys), hv(k, s)) for k in range(KC)],
                           split=(mg == 0 and mi == 0))
                        bu = nb()
                        mm(bu, [(T(wu.ap[:, k, mi * 128:(mi + 1) * 128], wu.keys), hv(k, s)) for k in range(KC)])
                        tm = ntmp()
                        act(tm, ba, AF.Silu)
                        tt(actv(mg * 2 + mi, s), tm, bu, ALU.mult)
            mark("ffn_down")
            proj_acc(lambda kc0, nkc, mg: fwd_d[l, kc0 * 128:(kc0 + nkc) * 128, mg * 256:(mg + 1) * 256],
                     [(0, 6), (6, 5), (11, 6), (17, 5)], 4, actv, resid_add)

        def out_proj(w_d2, nk, rhs_fn):
            mark("out_proj")
            proj_acc(lambda kc0, nkc, mg: w_d2[kc0 * 128:(kc0 + nkc) * 128, mg * 256:(mg + 1) * 256],
                     [(p * 8, 8) for p in range(nk // 8)], 4, rhs_fn, resid_add)

        def conv_mixer(j, tail_only=False):
            CZS = 2 * (G + 4)
            mark("conv_in")

            def czv(fc, c0, n):
                off = fc * CZS + 2 * c0
                return Rf(off, n)
            YO = KC * CZS

            def yv(kc, s):
                return Rb(YO + (kc * SUB + s) * 512, 512)
            HK = T(halo[:, j, :, :], [("halo", j)])
            for fc in range(KC):
                cp(czv(fc, 2, 2), T(halo[:, j, fc, :], HK.keys), eng="act" if False else "dve")
            for fg in range(4):
                wc = wload(cwin_d[j, :, D + fg * 256:D + (fg + 1) * 256], KC, 256)
                wz = wload(cwin_d[j, :, 2 * D + fg * 256:2 * D + (fg + 1) * 256], KC, 256)
                wb = None if tail_only else wload(cwin_d[j, :, fg * 256:(fg + 1) * 256], KC, 256)
                for s in CS[0]:
                    for mi in range(2):
                        fc = fg * 2 + mi
                        bc = nb()
                        mm(bc, [(T(wc.ap[:, k, mi * 128:(mi + 1) * 128], wc.keys), hv(k, s)) for k in range(KC)],
                           split=(fg == 0 and mi == 0))
                        bz = nb()
                        mm(bz, [(T(wz.ap[:, k, mi * 128:(mi + 1) * 128], wz.keys), hv(k, s)) for k in range(KC)])
                        tm = ntmp()
                        act(tm, bc, AF.Copy)
                        tt(czv(fc, 4 + s * 512, 512), tm, bz, ALU.mult)
                        if tail_only:
                            continue
                        bb = nb()
                        mm(bb, [(T(wb.ap[:, k, mi * 128:(mi + 1) * 128], wb.keys), hv(k, s)) for k in range(KC)])
                        t1 = ntmp()
                        c0 = 4 + s * 512
                        ts(t1, czv(fc, c0, 512), gT[:, fc, 9 + j * 3 + 2:9 + j * 3 + 3], None, ALU.mult, extra_reads=[GT])
                        stt(t1, czv(fc, c0 - 1, 512), gT[:, fc, 9 + j * 3 + 1:9 + j * 3 + 2], t1, ALU.mult, ALU.add, extra_reads=[GT])
                        stt(t1, czv(fc, c0 - 2, 512), gT[:, fc, 9 + j * 3:9 + j * 3 + 1], t1, ALU.mult, ALU.add, extra_reads=[GT])
                        tt(yv(fc, s), t1, bb, ALU.mult)
            for fc in range(KC):
                cp(T(halo[:, j, fc, :], HK.keys), czv(fc, 2 + G, 2))
            if not tail_only:
                out_proj(cwout_d[j], KC, yv)

        def sgu_mixer():
            def uv(c, s):
                return Rb((c * SUB + s) * 512, 512)
            NVS = 2
            VO = 16 * SUB * 512

            def vtok(slot):
                return Rf(VO + slot * 4096, 2048)
            dma_multi("sp", "mc", [(mixc[:, 0:2048], C_dram[:, :]),
                                   (mixc[:, 2048:4096], slg_d[0:1, :].partition_broadcast(128))],
                      reads=[CD], writes=[MIXC])
            mark("sgu_u")
            for mg in range(8):
                wt = wload(swin_d[0, :, mg * 256:(mg + 1) * 256], KC, 256)
                proj_fm(wt, 2, hv, KC, lambda bk, mi, s, mg=mg: act(uv(mg * 2 + mi, s), bk, AF.Gelu), first=(mg == 0))
            def vh_of(ch):
                return T(vhb[:, ch % 2, :], [("vhb", ch % 2)]) if SUB == 1 else Rb(24576 + (ch % 2) * 2048, 2048)

            def vproj(q0):
                mark("sgu_v%d" % q0)
                for vt in range(4):
                    wt = wload2(swin_d[0, :, 2048 + vt * 512:2048 + (vt + 1) * 512])
                    for ci in range(NVS):
                        ch = q0 + ci
                        bk = nb()
                        mm(bk, [(hvc(k, ch), T(wt.ap[:, k, :], wt.keys)) for k in range(KC)])
                        vt_t = vtok(ci)
                        col = ci * 16 + vt
                        act(T(vt_t.ap[:, vt * 512:(vt + 1) * 512], vt_t.keys), bk, AF.Gelu,
                            accum=T(st[:, col:col + 1], [("st", col)]))

            def chain(q0):
                for ci in range(NVS):
                    ch = q0 + ci
                    vt_t = vtok(ci)
                    b0 = ci * 16
                    STc = lambda a, n=1, b0=b0: T(st[:, b0 + a:b0 + a + n], [("st", b0 + a + i_) for i_ in range(n)])
                    vh = vh_of(ch)
                    act(vh, vt_t, AF.Square, accum=STc(4))
                    rec.emit("dve", lambda e, a=STc(5), b=STc(0, 4): e.tensor_reduce(out=a.ap, in_=b.ap, axis=mybir.AxisListType.X, op=ALU.add),
                             [STc(0, 4)], [STc(5)])
                    ts(STc(5), STc(5), 1.0 / 2048.0, None, ALU.mult)
                    tt(STc(6), STc(5), STc(5), ALU.mult)
                    stt(STc(7), STc(4), 1.0 / 2048.0, STc(6), ALU.mult, ALU.subtract)
                    act(STc(7), STc(7), AF.Ln, bias=EPSL, extra_reads=[EPST])
                    act(STc(7), STc(7), AF.Exp, scale=-0.5)
                    stt(STc(8), STc(5), -1.0, STc(7), ALU.mult, ALU.mult)
                    act(vt_t, vt_t, AF.Identity, bias=st[:, b0 + 8:b0 + 9], scale=st[:, b0 + 7:b0 + 8], extra_reads=[STc(7), STc(8)])
                    tt(vh, vt_t, T(mixc[:, 2048:4096], MIXC.keys), ALU.mult)

            def spatial(q0):
                for ci in range(NVS):
                    ch = q0 + ci
                    vh = vh_of(ch)
                    s_ = ch // 4
                    cc = ch % 4
                    for b4 in range(4):
                        bk = nb()
                        pe([("mm", bk.ap[:, i_ * 128:(i_ + 1) * 128], vh.ap[:, (b4 * 4 + i_) * 128:(b4 * 4 + i_ + 1) * 128],
                             WT[:, (b4 * 4 + i_) // 2, :], True, True) for i_ in range(4)], [vh, WTt], [bk])
                        tm = ntmp()
                        tt(tm, bk, T(mixc[:, b4 * 512:(b4 + 1) * 512], MIXC.keys), ALU.add)
                        tm3 = T(tm.ap.rearrange("p (c t) -> p c t", c=4), tm.keys)
                        u0 = b4 * 4 * SUB * 512
                        u4 = T(R[:, u0:u0 + 4 * SUB * 512].rearrange("p (c s t) -> p c s t", c=4, s=SUB)[:, :, s_, cc * 128:(cc + 1) * 128],
                               rkeys(u0, 4 * SUB * 512))
                        tt(u4, tm3, u4, ALU.mult)

            qs = list(range(0, NCH, NVS))
            vproj(qs[0])
            chain(qs[0])
            for qi in range(1, len(qs)):
                vproj(qs[qi])
                spatial(qs[qi - 1])
                chain(qs[qi])
            spatial(qs[-1])
            out_proj(swout_d[0], 16, uv)

        def ret_mixer(g, state_only, fsubs=None):
            allsubs = list(range(SUB))
            fsubs = allsubs if fsubs is None else fsubs
            def qv(c, s):
                return Rb((c * SUB + s) * 512, 512)

            def kv(c, s):
                return Rb(KC * SUB * 512 + (c * SUB + s) * 512, 512)
            VO = 2 * KC * SUB * 512

            def vv(ch):
                return Rb(VO + ch * 2048, 2048)
            dma_multi("sp", "mc", [(mixc[:, 0:G], cos_d[:, g * G:(g + 1) * G]),
                                   (mixc[:, 1024:1024 + G], sin_d[:, g * G:(g + 1) * G]),
                                   (mixc[:, 2048:3072], kdec_d[:, :]),
                                   (mixc[:, 3072:3584], dT_d[:, :])], writes=[MIXC])

            def cosv(s):
                return T(mixc[:, s * 512:(s + 1) * 512], MIXC.keys)

            def sinv(s):
                return T(mixc[:, 1024 + s * 512:1024 + (s + 1) * 512], MIXC.keys)

            def rot_proj(col0, dst):
                for h in range(HEADS):
                    wt = wload(rwin_d[0, :, col0 + h * 256:col0 + (h + 1) * 256], KC, 256)
                    for hh in range(1):
                        for s in CS[0]:
                            ba = nb()
                            mm(ba, [(T(wt.ap[:, k, (hh * 2) * 128:(hh * 2 + 1) * 128], wt.keys), hv(k, s)) for k in range(KC)],
                               split=(h == 0 and col0 == D))
                            bb = nb()
                            mm(bb, [(T(wt.ap[:, k, (hh * 2 + 1) * 128:(hh * 2 + 2) * 128], wt.keys), hv(k, s)) for k in range(KC)])
                            t1, t2 = ntmp(), ntmp()
                            tt(t1, ba, cosv(s), ALU.mult)
                            tt(t2, bb, sinv(s), ALU.mult)
                            tt(dst(2 * h, s), t1, t2, ALU.subtract)
                            t3, t4 = ntmp(), ntmp()
                            tt(t3, bb, cosv(s), ALU.mult)
                            tt(t4, ba, sinv(s), ALU.mult)
                            tt(dst(2 * h + 1, s), t3, t4, ALU.add)
            mark("ret_k")
            CS[0] = allsubs
            rot_proj(D, kv)
            if not state_only:
                mark("ret_q")
                CS[0] = fsubs
                rot_proj(0, qv)
            CS[0] = allsubs
            mark("ret_v")
            for h in range(HEADS):
                wt = wload2(rwin_d[0, :, 2 * D + h * 512:2 * D + (h + 1) * 512])
                for ch in range(NCH):
                    bk = nb()
                    mm(bk, [(hvc(k, ch), T(wt.ap[:, k, :], wt.keys)) for k in range(KC)])
                    v_ = vv(ch)
                    act(T(v_.ap[:, h * 512:(h + 1) * 512], v_.keys), bk, AF.Copy)
            Skeys = lambda i: [("S32", i)]
            if not state_only:
                for i in range(8):
                    act(T(Sbf[:, i, :], [("Sbf", i)]), T(S32[:, i, :], Skeys(i)), AF.Copy)
            for ch in range(NCH):
                mark("ret_core%d" % ch)
                s_, cc = ch // 4, ch % 4
                v_ = vv(ch)
                tb = ntb()
                pe([("tr", tb.ap[:, c * 128:(c + 1) * 128], kv(c, s_).ap[:, cc * 128:(cc + 1) * 128], identb[:]) for c in range(KC)],
                   [kv(c, s_) for c in range(KC)] + [IDB], [tb])
                kdt = T(kd[:, ch % 2, :], [("kd", ch % 2)])
                tt(kdt, tb, T(mixc[:, 2048:3072], MIXC.keys), ALU.mult)
                full_ch = (not state_only) and (s_ in fsubs)
                if full_ch:
                    bs_ = nb()
                    instrs = []
                    for h in range(HEADS):
                        for i in range(2):
                            c = 2 * h + i
                            instrs.append(("mm", bs_.ap[:, h * 128:(h + 1) * 128], kv(c, s_).ap[:, cc * 128:(cc + 1) * 128],
                                           qv(c, s_).ap[:, cc * 128:(cc + 1) * 128], i == 0, i == 1))
                    pe(instrs, [kv(c, s_) for c in range(KC)] + [qv(c, s_) for c in range(KC)], [bs_])
                    sT = T(sTb[:, ch % 2, :], [("sTb", ch % 2)])
                    tt(sT, bs_, T(mixc[:, 3072:3584], MIXC.keys), ALU.mult)
                    obanks = []
                    for h in range(HEADS):
                        bo = nb()
                        instrs = [("mm", bo.ap, sT.ap[:, h * 128:(h + 1) * 128], v_.ap[:, h * 512:(h + 1) * 512], True, False)]
                        rd = [sT, v_]
                        for i in range(2):
                            c = 2 * h + i
                            instrs.append(("mm", bo.ap, qv(c, s_).ap[:, cc * 128:(cc + 1) * 128], Sbf[:, c, :], False, i == 1))
                            rd += [qv(c, s_), T(Sbf[:, c, :], [("Sbf", c)])]
                        pe(instrs, rd, [bo])
                        obanks.append(bo)
                if full_ch:
                    onb = []
                    for h in range(HEADS):
                        bo = obanks[h]
                        col = 32 + h * 4
                        ssq = T(st[:, col:col + 1], [("st", col)])
                        rr = T(st[:, col + 1:col + 2], [("st", col + 1)])
                        on = T(onbuf[:, h, :], [("on", h)])
                        act(on, bo, AF.Square, scale=qdec[:, h:h + 1], accum=ssq, extra_reads=[QDEC])
                        act(rr, ssq, AF.Ln, bias=EPSR, scale=1.0 / 512.0, extra_reads=[EPST])
                        act(rr, rr, AF.Exp, scale=-0.5)
                        tt(rr, rr, T(qdec[:, h:h + 1], QDEC.keys), ALU.mult)
                        act(on, bo, AF.Copy, scale=st[:, col + 1:col + 2], extra_reads=[rr])
                        onb.append(on)
                for c in range(8):
                    h = c // 2
                    bk = nb()
                    mm(bk, [(T(kdt.ap[:, c * 128:(c + 1) * 128], kdt.keys), T(v_.ap[:, h * 512:(h + 1) * 512], v_.keys))])
                    Sc = T(S32[:, c, :], Skeys(c))
                    stt(Sc, Sc, cdec[h], bk, ALU.mult, ALU.add)
                    if not state_only:
                        act(T(Sbf[:, c, :], [("Sbf", c)]), Sc, AF.Copy)
                if not full_ch:
                    continue
                for hp in range(2):
                    tb = ntb()
                    instrs = []
                    for hh in range(2):
                        for e4 in range(4):
                            instrs.append(("tr", tb.ap[:, (hh * 4 + e4) * 128:(hh * 4 + e4 + 1) * 128],
                                           onb[hp * 2 + hh].ap[:, e4 * 128:(e4 + 1) * 128], identb[:]))
                    pe(instrs, [onb[hp * 2], onb[hp * 2 + 1], IDB], [tb])
                    cp(T(v_.ap[:, hp * 1024:(hp + 1) * 1024], v_.keys), tb)
            if state_only:
                return
            mark("ret_gate")
            CS[0] = fsubs
            def ov(c, s):
                ap = R[:, VO + s * 4 * 2048:VO + (s + 1) * 4 * 2048].rearrange("p (ch c t) -> p ch c t", ch=4, c=16)[:, :, c, :]
                return T(ap, rkeys(VO + s * 4 * 2048, 4 * 2048))
            for mg in range(8):
                wt = wload(rwin_d[0, :, 4 * D + mg * 256:4 * D + (mg + 1) * 256], KC, 256)

                def evac(bk, mi, s, mg=mg):
                    tm = ntmp()
                    act(tm, bk, AF.Silu)
                    o_ = ov(mg * 2 + mi, s)
                    tt(o_, T(tm.ap.rearrange("p (ch t) -> p ch t", ch=4), tm.keys), o_, ALU.mult)
                proj_fm(wt, 2, hv, KC, evac)
            out_proj(rwout_d[0], 16, ov)
            CS[0] = allsubs

        XO = RN - 4096
        for g in range(NG):
            if g < npre - 1:
                mode = "pre"
            elif g == npre - 1:
                mode = "pre_last"
            else:
                mode = "full"
            mark("G%d_%s_load" % (g, mode))
            for ch in range(NCH):
                sl = ch % 2
                xs = Rf(XO + sl * 2048, 1024)
                tok0 = g * G + ch * 128
                dma("sp", "xs%d" % sl, xs.ap, x_d[tok0:tok0 + 128, :], writes=[xs])
                for half in range(2):
                    bk = nb()
                    pe([("tr", bk.ap[:, j * 128:(j + 1) * 128], xs.ap[:, (half * 4 + j) * 128:(half * 4 + j + 1) * 128], ident[:])
                        for j in range(4)], [xs, IDF], [bk])
                    s_ = ch // 4
                    dst = T(xT[:, half * 4:half * 4 + 4, ch * 128:(ch + 1) * 128], [("x", half * 4 + j, s_) for j in range(4)])
                    rec.emit("act" if half else "dve",
                             (lambda e, d=dst, b=bk: e.activation(out=d.ap, in_=b.ap.rearrange("p (c t) -> p c t", c=4), func=AF.Copy)) if half else
                             (lambda e, d=dst, b=bk: e.tensor_copy(out=d.ap, in_=b.ap.rearrange("p (c t) -> p c t", c=4))),
                             [bk], [dst])
            for l in range(DBG_NL):
                kind = l % 3
                if mode == "pre" and l == 3:
                    break
                rmsnorm(l, hv)
                if kind == 0:
                    conv_mixer(l // 3, tail_only=(mode == "pre_last" and l == 3))
                    if mode == "pre_last" and l == 3:
                        break
                elif kind == 1:
                    sgu_mixer()
                else:
                    ret_mixer(g, state_only=(mode == "pre"), fsubs=([SUB - 1] if mode == "pre_last" else None))
                    if mode == "pre":
                        break
                    if mode == "pre_last":
                        CS[0] = [SUB - 1]
                if DBG_FFN:
                    rmsnorm(4 + l, hv)
                    ffn(l)
            CS[0] = list(range(SUB))
            if mode != "full":
                continue
            def fv(kc, s):
                return Rf((kc * SUB + s) * 1024, 512)
            mark("final")
            rmsnorm(8, fv)
            for ch in range(NCH):
                s_, cc = ch // 4, ch % 4
                sl = ch % 2
                os_ = Rf(XO + sl * 2048, 1024)
                for half in range(2):
                    bk = nb()
                    pe([("tr", bk.ap[:, j * 128:(j + 1) * 128], fv(half * 4 + j, s_).ap[:, cc * 128:(cc + 1) * 128], ident[:])
                        for j in range(4)], [fv(half * 4 + j, s_) for j in range(4)] + [IDF], [bk])
                    dst = T(os_.ap[:, half * 512:(half + 1) * 512], os_.keys)
                    if half:
                        act(dst, bk, AF.Copy)
                    else:
                        cp(dst, bk)
                tok0 = (g - npre) * G + ch * 128
                dma("sp", "os%d" % sl, y_d[tok0:tok0 + 128, :], os_.ap, reads=[os_])
        rec.final_wait("sp", ["os0", "os1"])
        with nc.Block() as block:
            rec.run(block)
    return nc


def _tables(positions):
    half = 128
    inv_freq = (10000.0 ** (-(np.arange(half, dtype=np.float32) / np.float32(half)))).astype(np.float32)
    ang = (positions.astype(np.float32)[None, :] * inv_freq[:, None]).astype(np.float32)
    return np.cos(ang).astype(np.float32), np.sin(ang).astype(np.float32)


def _const_tables():
    gam = 1.0 - 2.0 ** (-5.0 - np.arange(HEADS, dtype=np.float64))
    lg = np.log(gam)
    idx = np.arange(128, dtype=np.float64)
    dT = np.zeros((128, HEADS, 128), np.float64)
    for h in range(HEADS):
        kf = np.exp(-(idx + 1.0) * lg[h]) / 16.0
        dT[:, h, :] = (idx[:, None] <= idx[None, :]) * kf[:, None]
    kdec = np.zeros((128, HEADS, 256), np.float64)
    for h in range(HEADS):
        kdec[:, h, :] = (np.exp((127.0 - idx) * lg[h]) / 16.0)[:, None]
    qdec = np.exp((idx[:, None] + 1.0) * lg[None, :])
    tri = (idx[:, None] <= idx[None, :]).astype(np.float32)
    return (dT.reshape(128, 512).astype(np.float32), kdec.reshape(128, 1024).astype(np.float32),
            qdec.astype(np.float32), np.eye(128, dtype=np.float32), tri)


_NC_CACHE = {}


def run_trunk(inputs, seq, npre, nseg, SUB, n_cores):
    G = SUB * 512
    half = seq // 2
    assert npre * G == half and nseg * G == half
    key = (npre, nseg, SUB, DBG_NL, DBG_FFN)
    if key not in _NC_CACHE:
        _NC_CACHE[key] = build(npre, nseg, SUB)
    nc = _NC_CACHE[key]
    x = np.ascontiguousarray(np.asarray(inputs["x"], dtype=np.float32))
    dT, kdec, qdec, ident, tri = _const_tables()
    shared = {k: np.ascontiguousarray(np.asarray(v, dtype=np.float32)) for k, v in inputs.items() if k != "x"}
    shared.update({"t_dT": dT, "t_kdec": kdec, "t_qdec": qdec, "t_ident": ident, "t_tri": tri})
    in_maps = []
    for c in range(n_cores):
        s, p = c // 2, c % 2
        if p == 0:
            xc = np.concatenate([np.zeros((half, D), np.float32), x[s, :half]], axis=0)
            pos = np.arange(-half, half)
        else:
            xc = x[s]
            pos = np.arange(0, seq)
        cos, sin = _tables(pos)
        m = dict(shared)
        m.update({"x": np.ascontiguousarray(xc), "t_cos": cos, "t_sin": sin})
        in_maps.append(m)
    res = run_bass_kernel_spmd(nc, in_maps, core_ids=list(range(n_cores)))
    out = np.zeros((n_cores // 2, seq, D), np.float32)
    for c in range(n_cores):
        s, p = c // 2, c % 2
        out[s, p * half:(p + 1) * half] = res.results[c]["y"]
    return out


SUB_CFG = 2
DBG_NL = 4
MARKS = []
DBG_FFN = True


def kernel(**inputs):
    G = SUB_CFG * 512
    n = (SEQ // 2) // G
    return run_trunk(inputs, SEQ, n, n, SUB_CFG, 8)
```

```python
import numpy as np
from contextlib import ExitStack
import concourse.bass as bass
import concourse.mybir as mybir
from concourse.bass_utils import run_bass_kernel_spmd

F32 = mybir.dt.float32
BF16 = mybir.dt.bfloat16
AF = mybir.ActivationFunctionType
ALU = mybir.AluOpType

D = 1024
KC = 8
DFF = 2816
FC = 22
RMS_EPS = 1e-6
LN_EPS = 1e-5
HEADS = 4
SEQ = 8192
BATCH = 4
COMPUTE = ("pe", "act", "dve")


class T:
    __slots__ = ("ap", "keys")

    def __init__(self, ap, keys):
        self.ap = ap
        self.keys = tuple(keys)


class Rec:
    def __init__(self, nc, es):
        self.nc = nc
        self.es = es
        self.ops = {e: [] for e in ("pe", "act", "dve", "pool", "sp")}
        self.sem = {}
        self.cnt = {}
        for e in COMPUTE:
            self.sem[e] = es.enter_context(nc.semaphore("s_" + e))
            self.cnt[e] = 0
        self.waited = {}
        self.lastw = {}
        self.readers = {}

    def stream(self, name):
        self.sem[name] = self.es.enter_context(self.nc.semaphore(name))
        self.cnt[name] = 0
        return name

    def _need(self, waits, eng, prod, val):
        if prod == eng and eng == "pe":
            return
        k = (eng, prod)
        if self.waited.get(k, 0) >= val:
            return
        self.waited[k] = val
        waits.append((self.sem[prod], val * (1 if prod in COMPUTE else 16)))

    def emit(self, eng, fn, reads, writes, prod=None, n=1):
        prod = prod or eng
        waits = []
        for t in reads:
            for k in t.keys:
                lw = self.lastw.get(k)
                if lw:
                    self._need(waits, eng, *lw)
        for t in writes:
            for k in t.keys:
                lw = self.lastw.get(k)
                if lw:
                    self._need(waits, eng, *lw)
                for p, v in self.readers.get(k, {}).items():
                    self._need(waits, eng, p, v)
        self.cnt[prod] += n
        val = self.cnt[prod]
        for t in writes:
            for k in t.keys:
                self.lastw[k] = (prod, val)
                self.readers[k] = {}
        for t in reads:
            for k in t.keys:
                self.readers.setdefault(k, {})[prod] = val
        self.ops[eng].append((waits, fn, self.sem[prod], 1 if prod in COMPUTE else 16))

    def final_wait(self, eng, prods):
        waits = [(self.sem[p], self.cnt[p] * (1 if p in COMPUTE else 16)) for p in prods if self.cnt[p]]
        self.ops[eng].append((waits, None, None, 0))

    def run(self, block):
        def mk(e):
            def body(eng):
                for waits, fn, sem, inc in self.ops[e]:
                    for sm, v in waits:
                        eng.wait_ge(sm, v)
                    if fn is None:
                        continue
                    r = fn(eng)
                    if isinstance(r, (list, tuple)):
                        for ins in r:
                            ins.then_inc(sem, inc)
                    else:
                        r.then_inc(sem, inc)
            return body
        block.tensor(mk("pe"))
        block.scalar(mk("act"))
        block.vector(mk("dve"))
        block.gpsimd(mk("pool"))
        block.sync(mk("sp"))


def build(npre, nseg, SUB):
    G = SUB * 512
    NCH = G // 128
    NG = npre + nseg
    NTOK = NG * G
    RN = 16384 * SUB
    nc = bass.Bass("TRN2", target_bir_lowering=False)

    def din(name, shape):
        return nc.dram_tensor(name, list(shape), F32, kind="ExternalInput").ap()

    x_d = din("x", [NTOK, D])
    nmg_d = din("norm_mix_g", [4, D])
    nfg_d = din("norm_ffn_g", [4, D])
    fng_d = din("final_norm_g", [D])
    cwin_d = din("conv_w_in", [2, D, 3 * D])
    cw_d = din("conv_w", [2, 3, D])
    cwout_d = din("conv_w_out", [2, D, D])
    swin_d = din("sgu_w_in", [1, D, 4096])
    slg_d = din("sgu_ln_g", [1, 2048])
    slb_d = din("sgu_ln_b", [1, 2048])
    sws_d = din("sgu_w_s", [1, 8, 128, 128])
    sbs_d = din("sgu_b_s", [1, 8, 128])
    swout_d = din("sgu_w_out", [1, 2048, D])
    rwin_d = din("ret_w_in", [1, D, 6 * D])
    rwout_d = din("ret_w_out", [1, 2048, D])
    fwg_d = din("ffn_w_gate", [4, D, DFF])
    fwu_d = din("ffn_w_up", [4, D, DFF])
    fwd_d = din("ffn_w_down", [4, DFF, D])
    cos_d = din("t_cos", [128, NTOK])
    sin_d = din("t_sin", [128, NTOK])
    dT_d = din("t_dT", [128, 512])
    kdec_d = din("t_kdec", [128, 1024])
    qdec_d = din("t_qdec", [128, 4])
    id_d = din("t_ident", [128, 128])
    tri_d = din("t_tri", [128, 128])
    y_d = nc.dram_tensor("y", [nseg * G, D], F32, kind="ExternalOutput").ap()
    C_dram = nc.dram_tensor("c_scr", [128, 2048], F32).ap()

    gam = [1.0 - 2.0 ** (-5.0 - h) for h in range(HEADS)]
    cdec = [float(np.exp(128.0 * np.log(g_))) for g_ in gam]

    with ExitStack() as es:
        def sb(name, shape, dt):
            return es.enter_context(nc.sbuf_tensor(name, list(shape), dt))

        def ps(name, shape, dt):
            return es.enter_context(nc.psum_tensor(name, list(shape), dt))

        xT = sb("xT", [128, KC, G], F32)
        hT = sb("hT", [128, KC, G], BF16)
        R = sb("R", [128, RN], BF16)
        S32 = sb("S32", [128, 8, 512], F32)
        Sbf = sb("Sbf", [128, 8, 512], BF16)
        NW = 6
        wbuf = sb("wbuf", [128, NW, 2048], BF16)
        mixc = sb("mixc", [128, 4096], F32)
        sq = sb("sq", [128, 3, 512], BF16)
        rstd = sb("rstd", [128, 2, 512], F32)
        NTMP = 4
        tmp = sb("tmp", [128, NTMP, 512], F32)
        kd = sb("kd", [128, 1, 1024], BF16)
        sTb = sb("sTb", [128, 2, 512], BF16)
        vhb = sb("vhb", [128, 2, 2048], BF16) if SUB == 1 else None
        WT = sb("WT", [128, 8, 128], BF16)
        ident = sb("ident", [128, 128], F32)
        identb = sb("identb", [128, 128], BF16)
        onesb = sb("onesb", [128, 128], BF16)
        gT = sb("gT", [128, KC, 32], F32)
        halo = sb("halo", [128, 2, KC, 2], F32)
        qdec = sb("qdec", [128, 4], F32)
        st = sb("st", [128, 64], F32)
        onbuf = sb("onbuf", [128, 8, 512], BF16)

        NB = 8
        pb = [ps("pb%d" % i, [128, 512], F32) for i in range(NB)]

        rec = Rec(nc, es)
        for nm in ["w0", "w1", "w2", "w3", "w4", "w5", "xs0", "xs1", "os0", "os1", "cst", "mc", "cs"]:
            rec.stream(nm)

        def rkeys(off, n):
            return [("R", i) for i in range(off // 512, (off + n - 1) // 512 + 1)]

        def Rb(off, n):
            return T(R[:, off:off + n], rkeys(off, n))

        def Rf(off, n):
            return T(R[:, off:off + 2 * n].bitcast(F32), rkeys(off, 2 * n))

        bank_i = [0]

        def nb():
            i = bank_i[0]
            bank_i[0] = (i + 1) % NB
            return T(pb[i][:], [("pb", i)])

        tb_i = [0]

        def ntb():
            b = nb()
            return T(b.ap.bitcast(BF16), b.keys)

        tmp_i = [0]

        def ntmp():
            i = tmp_i[0]
            tmp_i[0] = (i + 1) % NTMP
            return T(tmp[:, i, :], [("tmp", i)])

        sq_i = [0]

        def nsq():
            i = sq_i[0]
            sq_i[0] = (i + 1) % 3
            return T(sq[:, i, :], [("sq", i)])

        def V(ap, *keys):
            return T(ap, keys)

        def sub_of(t0):
            return t0 // 512

        def xv(kc, s):
            return T(xT[:, kc, s * 512:(s + 1) * 512], [("x", kc, s)])

        def hv(kc, s):
            return T(hT[:, kc, s * 512:(s + 1) * 512], [("h", kc, s)])

        def hvc(kc, ch):
            return T(hT[:, kc, ch * 128:(ch + 1) * 128], [("h", kc, ch // 4)])

        PE_N = [0]
        CS = [list(range(SUB))]

        def mark(label):
            MARKS.append((PE_N[0], label))

        def pe(instrs, reads, writes):
            instrs = list(instrs)
            def fn(eng):
                last = None
                for it in instrs:
                    if it[0] == "mm":
                        _, o, l, r, s0, s1 = it
                        last = eng.matmul(out=o, lhsT=l, rhs=r, start=s0, stop=s1)
                    else:
                        _, o, i_, idn = it
                        last = eng.transpose(out=o, in_=i_, identity=idn)
                return last
            rec.emit("pe", fn, reads, writes)
            PE_N[0] += len(instrs)

        def mm(out, pairs, split=False):
            n = len(pairs)
            if split:
                for i, (l, r) in enumerate(pairs):
                    pe([("mm", out.ap, l.ap, r.ap, i == 0, i == n - 1)], [l, r], [out])
                return
            instrs = [("mm", out.ap, l.ap, r.ap, i == 0, i == n - 1) for i, (l, r) in enumerate(pairs)]
            reads = [t for p in pairs for t in p]
            pe(instrs, reads, [out])

        def act(out, in_, func, bias=None, scale=None, accum=None, extra_reads=()):
            kw = {}
            if bias is not None:
                kw["bias"] = bias
            if scale is not None:
                kw["scale"] = scale
            if accum is not None:
                kw["accum_out"] = accum.ap
            rec.emit("act", lambda e: e.activation(out=out.ap, in_=in_.ap, func=func, **kw),
                     [in_] + list(extra_reads), [out] + ([accum] if accum is not None else []))

        def tt(out, a, b, op, eng="dve"):
            rec.emit(eng, lambda e: e.tensor_tensor(out=out.ap, in0=a.ap, in1=b.ap, op=op), [a, b], [out])

        def ts(out, a, s1, s2, op0, op1=None, extra_reads=(), eng="dve"):
            if op1 is None:
                rec.emit(eng, lambda e: e.tensor_scalar(out=out.ap, in0=a.ap, scalar1=s1, scalar2=None, op0=op0),
                         [a] + list(extra_reads), [out])
            else:
                rec.emit(eng, lambda e: e.tensor_scalar(out=out.ap, in0=a.ap, scalar1=s1, scalar2=s2, op0=op0, op1=op1),
                         [a] + list(extra_reads), [out])

        def stt(out, a, sc, b, op0, op1, extra_reads=(), eng="dve"):
            rec.emit(eng, lambda e: e.scalar_tensor_tensor(out=out.ap, in0=a.ap, scalar=sc, in1=b.ap, op0=op0, op1=op1),
                     [a, b] + list(extra_reads), [out])

        def recip(out, in_):
            rec.emit("dve", lambda e: e.reciprocal(out=out.ap, in_=in_.ap), [in_], [out])

        def cp(out, in_, eng="dve"):
            rec.emit(eng, lambda e: e.tensor_copy(out=out.ap, in_=in_.ap), [in_], [out])

        def mset(out, val, eng="dve"):
            rec.emit(eng, lambda e: e.memset(out.ap, val), [], [out])

        def dma(eng, stream, out, in_, reads=(), writes=()):
            rec.emit(eng, lambda e: e.dma_start(out=out, in_=in_), list(reads), list(writes), prod=stream)

        def dma_multi(eng, stream, pairs, reads=(), writes=()):
            def fn(e):
                return [e.dma_start(out=o, in_=i) for o, i in pairs]
            rec.emit(eng, fn, list(reads), list(writes), prod=stream, n=len(pairs))

        w_i = [0]

        def wload(src2d, kcs, cols):
            i = w_i[0]
            w_i[0] = (i + 1) % NW
            dst = wbuf[:, i, 0:kcs * cols].rearrange("p (k m) -> p k m", k=kcs)
            t = T(dst, [("w", i)])
            dma("pool", "w%d" % i, dst, src2d.rearrange("(k p) m -> p k m", p=128), writes=[t])
            return t

        def wload2(src2d):
            if w_i[0] % 2:
                w_i[0] = (w_i[0] + 1) % NW
            i = w_i[0]
            w_i[0] = (i + 2) % NW
            dst = wbuf[:, i:i + 2, :].rearrange("p a (k m) -> p (a k) m", m=512)
            t = T(dst, [("w", i), ("w", i + 1)])
            dma("pool", "w%d" % i, dst, src2d.rearrange("(k p) m -> p k m", p=128), writes=[t])
            return t

        GT = T(gT[:], [("gT",)])
        IDF = T(ident[:], [("ident",)])
        IDB = T(identb[:], [("identb",)])
        ONESB = T(onesb[:], [("onesb",)])
        MIXC = T(mixc[:], [("mixc",)])
        QDEC = T(qdec[:], [("qdec",)])
        epst = sb("epst", [128, 2], F32)
        EPSR = epst[:, 0:1]
        EPSL = epst[:, 1:2]
        EPST = T(epst[:], [("eps", 0), ("eps", 1)])

        vst = Rf(0, 1024)
        vst32 = T(vst.ap[0:32, :], vst.keys)
        mset(vst32, 0.0)
        wsn = Rf(2048, 1024)
        bsb = Rf(4096, 1024)
        tri = Rf(6144, 128)
        onesf = Rf(6656, 128)
        WTf = Rf(7168, 1024)
        Cs = Rf(9216, 2048)
        pre = [
            (ident[:], id_d[:, :]),
            (qdec[:], qdec_d[:, :]),
            (vst.ap[0:4, :], nmg_d[:, :]),
            (vst.ap[4:8, :], nfg_d[:, :]),
            (vst.ap[8:9, :], fng_d.rearrange("(o d) -> o d", o=1)),
            (vst.ap[9:15, :], cw_d.rearrange("j k d -> (j k) d")),
            (vst.ap[16:18, :], slb_d.rearrange("o (r d) -> (o r) d", r=2)),
            (wsn.ap.rearrange("p (g s) -> p g s", g=8), sws_d.rearrange("o g t s -> t (o g) s")),
            (bsb.ap, sbs_d.rearrange("o g t -> o (g t)").partition_broadcast(128)),
            (tri.ap, tri_d[:, :]),
        ]
        rec.emit("sp", lambda e: [e.dma_start(out=o, in_=i) for o, i in pre], [vst32],
                 [IDF, QDEC, vst, wsn, bsb, tri], prod="cst", n=len(pre))
        cp(IDB, IDF)
        mset(ONESB, 1.0 / 1024.0)
        mset(onesf, 1.0)
        mset(T(epst[:, 0:1], [("eps", 0)]), RMS_EPS)
        mset(T(epst[:, 1:2], [("eps", 1)]), LN_EPS)
        mset(T(S32[:], [("S32", i) for i in range(8)]), 0.0)
        mset(T(Sbf[:], [("Sbf", i) for i in range(8)]), 0.0)
        mset(T(halo[:], [("halo", 0), ("halo", 1)]), 0.0)
        bk = nb()
        pe([("tr", bk.ap[:, kc * 32:(kc + 1) * 32], vst.ap[0:32, kc * 128:(kc + 1) * 128], ident[0:32, 0:32])
            for kc in range(KC)], [vst, IDF], [bk])
        act(T(gT[:].rearrange("p k r -> p (k r)"), GT.keys), T(bk.ap[:, 0:256], bk.keys), AF.Copy)
        for half in range(2):
            bk = nb()
            pe([("tr", bk.ap[:, j * 128:(j + 1) * 128], wsn.ap[:, (half * 4 + j) * 128:(half * 4 + j + 1) * 128], ident[:])
                for j in range(4)], [wsn, IDF], [bk])
            for j in range(4):
                g_ = half * 4 + j
                tt(T(WTf.ap[:, g_ * 128:(g_ + 1) * 128], WTf.keys), T(bk.ap[:, j * 128:(j + 1) * 128], bk.keys), tri, ALU.mult)
        cp(T(WT[:].rearrange("p g t -> p (g t)"), [("WT",)]), WTf)
        WTt = T(WT[:], [("WT",)])
        for half in range(2):
            bk = nb()
            pe([("mm", bk.ap[:, j * 128:(j + 1) * 128], onesf.ap, WTf.ap[:, (half * 4 + j) * 128:(half * 4 + j + 1) * 128], True, True)
                for j in range(4)], [onesf, WTf], [bk])
            for j in range(4):
                g_ = half * 4 + j
                for i in range(2):
                    dc = g_ * 2 + i
                    r_, kc_ = dc // 8, dc % 8
                    stt(T(Cs.ap[:, dc * 128:(dc + 1) * 128], Cs.keys), T(bk.ap[:, j * 128:(j + 1) * 128], bk.keys),
                        gT[:, kc_, 16 + r_:17 + r_], T(bsb.ap[:, g_ * 128:(g_ + 1) * 128], bsb.keys),
                        ALU.mult, ALU.add, extra_reads=[GT])
        CD = T(C_dram, [("Cdram",)])
        dma("sp", "cst", C_dram[:, :], Cs.ap, reads=[Cs], writes=[CD])

        def rmsnorm(col, out_fn, out_dtype_bf16=True):
            mark("norm%d" % col)
            for s in CS[0]:
                PST = nb()
                for kc in range(KC):
                    q_ = nsq()
                    act(q_, xv(kc, s), AF.Square)
                    pe([("mm", PST.ap, ONESB.ap, q_.ap, kc == 0, kc == KC - 1)], [ONESB, q_], [PST])
                rs = T(rstd[:, s % 2, :], [("rstd", s % 2)])
                act(rs, PST, AF.Ln, bias=EPSR, extra_reads=[EPST])
                act(rs, rs, AF.Exp, scale=-0.5)
                for kc in range(KC):
                    stt(out_fn(kc, s), xv(kc, s), gT[:, kc, col:col + 1], rs, ALU.mult, ALU.mult, extra_reads=[GT])

        def resid_add(bk, mc, s):
            tt(xv(mc, s), bk, xv(mc, s), ALU.add)

        def proj_fm(w_t, mcs, rhs_fn, nk, evac, first=False):
            for s in CS[0]:
                for mi in range(mcs):
                    bk = nb()
                    mm(bk, [(T(w_t.ap[:, k, mi * 128:(mi + 1) * 128], w_t.keys), rhs_fn(k, s)) for k in range(nk)],
                       split=(first and mi == 0))
                    evac(bk, mi, s)

        def proj_acc(w_fn, parts, ncol_groups, rhs_fn, evac):
            for mg in range(ncol_groups):
                banks = [{s: nb() for s in CS[0]} for mi in range(2)]
                for pi, (kc0, nkc) in enumerate(parts):
                    wt = wload(w_fn(kc0, nkc, mg), nkc, 256)
                    for mi in range(2):
                        for s in CS[0]:
                            instrs = [("mm", banks[mi][s].ap, wt.ap[:, k, mi * 128:(mi + 1) * 128], rhs_fn(kc0 + k, s).ap,
                                       pi == 0 and k == 0, pi == len(parts) - 1 and k == nkc - 1) for k in range(nkc)]
                            pe(instrs, [wt] + [rhs_fn(kc0 + k, s) for k in range(nkc)], [banks[mi][s]])
                for mi in range(2):
                    for s in CS[0]:
                        evac(banks[mi][s], mg * 2 + mi, s)

        def ffn(l):
            mark("ffn_in")
            def actv(c, s):
                return Rb((c * SUB + s) * 512, 512)
            for g0 in range(0, 11, 2):
              grp = list(range(g0, min(g0 + 2, 11)))
              wts = {mg: (wload(fwg_d[l, :, mg * 256:(mg + 1) * 256], KC, 256),
                          wload(fwu_d[l, :, mg * 256:(mg + 1) * 256], KC, 256)) for mg in grp}
              for s in CS[0]:
                for mg in grp:
                    wg, wu = wts[mg]
                    for mi in range(2):
                        ba = nb()
                        mm(ba, [(T(wg.ap[:, k, mi * 128:(mi + 1) * 128], wg.keys), hv(k, s)) for k in range(KC)],
                           split=(mg == 0 and mi == 0))
                        bu = nb()
                        mm(bu, [(T(wu.ap[:, k, mi * 128:(mi + 1) * 128], wu.keys), hv(k, s)) for k in range(KC)])
                        tm = ntmp()
                        act(tm, ba, AF.Silu)
                        tt(actv(mg * 2 + mi, s), tm, bu, ALU.mult)
            mark("ffn_down")
            proj_acc(lambda kc0, nkc, mg: fwd_d[l, kc0 * 128:(kc0 + nkc) * 128, mg * 256:(mg + 1) * 256],
                     [(0, 6), (6, 5), (11, 6), (17, 5)], 4, actv, resid_add)

        def out_proj(w_d2, nk, rhs_fn):
            mark("out_proj")
            proj_acc(lambda kc0, nkc, mg: w_d2[kc0 * 128:(kc0 + nkc) * 128, mg * 256:(mg + 1) * 256],
                     [(p * 8, 8) for p in range(nk // 8)], 4, rhs_fn, resid_add)

        def conv_mixer(j, tail_only=False):
            CZS = 2 * (G + 4)
            mark("conv_in")

            def czv(fc, c0, n):
                off = fc * CZS + 2 * c0
                return Rf(off, n)
            YO = KC * CZS

            def yv(kc, s):
                return Rb(YO + (kc * SUB + s) * 512, 512)
            HK = T(halo[:, j, :, :], [("halo", j)])
            for fc in range(KC):
                cp(czv(fc, 2, 2), T(halo[:, j, fc, :], HK.keys), eng="act" if False else "dve")
            for fg in range(4):
                wc = wload(cwin_d[j, :, D + fg * 256:D + (fg + 1) * 256], KC, 256)
                wz = wload(cwin_d[j, :, 2 * D + fg * 256:2 * D + (fg + 1) * 256], KC, 256)
                wb = None if tail_only else wload(cwin_d[j, :, fg * 256:(fg + 1) * 256], KC, 256)
                for s in CS[0]:
                    for mi in range(2):
                        fc = fg * 2 + mi
                        bc = nb()
                        mm(bc, [(T(wc.ap[:, k, mi * 128:(mi + 1) * 128], wc.keys), hv(k, s)) for k in range(KC)],
                           split=(fg == 0 and mi == 0))
                        bz = nb()
                        mm(bz, [(T(wz.ap[:, k, mi * 128:(mi + 1) * 128], wz.keys), hv(k, s)) for k in range(KC)])
                        tm = ntmp()
                        act(tm, bc, AF.Copy)
                        tt(czv(fc, 4 + s * 512, 512), tm, bz, ALU.mult)
                        if tail_only:
                            continue
                        bb = nb()
                        mm(bb, [(T(wb.ap[:, k, mi * 128:(mi + 1) * 128], wb.keys), hv(k, s)) for k in range(KC)])
                        t1 = ntmp()
                        c0 = 4 + s * 512
                        ts(t1, czv(fc, c0, 512), gT[:, fc, 9 + j * 3 + 2:9 + j * 3 + 3], None, ALU.mult, extra_reads=[GT])
                        stt(t1, czv(fc, c0 - 1, 512), gT[:, fc, 9 + j * 3 + 1:9 + j * 3 + 2], t1, ALU.mult, ALU.add, extra_reads=[GT])
                        stt(t1, czv(fc, c0 - 2, 512), gT[:, fc, 9 + j * 3:9 + j * 3 + 1], t1, ALU.mult, ALU.add, extra_reads=[GT])
                        tt(yv(fc, s), t1, bb, ALU.mult)
            for fc in range(KC):
                cp(T(halo[:, j, fc, :], HK.keys), czv(fc, 2 + G, 2))
            if not tail_only:
                out_proj(cwout_d[j], KC, yv)

        def sgu_mixer():
            def uv(c, s):
                return Rb((c * SUB + s) * 512, 512)
            NVS = 2
            VO = 16 * SUB * 512

            def vtok(slot):
                return Rf(VO + slot * 4096, 2048)
            dma_multi("sp", "mc", [(mixc[:, 0:2048], C_dram[:, :]),
                                   (mixc[:, 2048:4096], slg_d[0:1, :].partition_broadcast(128))],
                      reads=[CD], writes=[MIXC])
            mark("sgu_u")
            for mg in range(8):
                wt = wload(swin_d[0, :, mg * 256:(mg + 1) * 256], KC, 256)
                proj_fm(wt, 2, hv, KC, lambda bk, mi, s, mg=mg: act(uv(mg * 2 + mi, s), bk, AF.Gelu), first=(mg == 0))
            def vh_of(ch):
                return T(vhb[:, ch % 2, :], [("vhb", ch % 2)]) if SUB == 1 else Rb(24576 + (ch % 2) * 2048, 2048)

            def vproj(q0):
                mark("sgu_v%d" % q0)
                for vt in range(4):
                    wt = wload2(swin_d[0, :, 2048 + vt * 512:2048 + (vt + 1) * 512])
                    for ci in range(NVS):
                        ch = q0 + ci
                        bk = nb()
                        mm(bk, [(hvc(k, ch), T(wt.ap[:, k, :], wt.keys)) for k in range(KC)])
                        vt_t = vtok(ci)
                        col = ci * 16 + vt
                        act(T(vt_t.ap[:, vt * 512:(vt + 1) * 512], vt_t.keys), bk, AF.Gelu,
                            accum=T(st[:, col:col + 1], [("st", col)]))

            def chain(q0):
                for ci in range(NVS):
                    ch = q0 + ci
                    vt_t = vtok(ci)
                    b0 = ci * 16
                    STc = lambda a, n=1, b0=b0: T(st[:, b0 + a:b0 + a + n], [("st", b0 + a + i_) for i_ in range(n)])
                    vh = vh_of(ch)
                    act(vh, vt_t, AF.Square, accum=STc(4))
                    rec.emit("dve", lambda e, a=STc(5), b=STc(0, 4): e.tensor_reduce(out=a.ap, in_=b.ap, axis=mybir.AxisListType.X, op=ALU.add),
                             [STc(0, 4)], [STc(5)])
                    ts(STc(5), STc(5), 1.0 / 2048.0, None, ALU.mult)
                    tt(STc(6), STc(5), STc(5), ALU.mult)
                    stt(STc(7), STc(4), 1.0 / 2048.0, STc(6), ALU.mult, ALU.subtract)
                    act(STc(7), STc(7), AF.Ln, bias=EPSL, extra_reads=[EPST])
                    act(STc(7), STc(7), AF.Exp, scale=-0.5)
                    stt(STc(8), STc(5), -1.0, STc(7), ALU.mult, ALU.mult)
                    act(vt_t, vt_t, AF.Identity, bias=st[:, b0 + 8:b0 + 9], scale=st[:, b0 + 7:b0 + 8], extra_reads=[STc(7), STc(8)])
                    tt(vh, vt_t, T(mixc[:, 2048:4096], MIXC.keys), ALU.mult)

            def spatial(q0):
                for ci in range(NVS):
                    ch = q0 + ci
                    vh = vh_of(ch)
                    s_ = ch // 4
                    cc = ch % 4
                    for b4 in range(4):
                        bk = nb()
                        pe([("mm", bk.ap[:, i_ * 128:(i_ + 1) * 128], vh.ap[:, (b4 * 4 + i_) * 128:(b4 * 4 + i_ + 1) * 128],
                             WT[:, (b4 * 4 + i_) // 2, :], True, True) for i_ in range(4)], [vh, WTt], [bk])
                        tm = ntmp()
                        tt(tm, bk, T(mixc[:, b4 * 512:(b4 + 1) * 512], MIXC.keys), ALU.add)
                        tm3 = T(tm.ap.rearrange("p (c t) -> p c t", c=4), tm.keys)
                        u0 = b4 * 4 * SUB * 512
                        u4 = T(R[:, u0:u0 + 4 * SUB * 512].rearrange("p (c s t) -> p c s t", c=4, s=SUB)[:, :, s_, cc * 128:(cc + 1) * 128],
                               rkeys(u0, 4 * SUB * 512))
                        tt(u4, tm3, u4, ALU.mult)

            qs = list(range(0, NCH, NVS))
            vproj(qs[0])
            chain(qs[0])
            for qi in range(1, len(qs)):
                vproj(qs[qi])
                spatial(qs[qi - 1])
                chain(qs[qi])
            spatial(qs[-1])
            out_proj(swout_d[0], 16, uv)

        def ret_mixer(g, state_only, fsubs=None):
            allsubs = list(range(SUB))
            fsubs = allsubs if fsubs is None else fsubs
            def qv(c, s):
                return Rb((c * SUB + s) * 512, 512)

            def kv(c, s):
                return Rb(KC * SUB * 512 + (c * SUB + s) * 512, 512)
            VO = 2 * KC * SUB * 512

            def vv(ch):
                return Rb(VO + ch * 2048, 2048)
            dma_multi("sp", "mc", [(mixc[:, 0:G], cos_d[:, g * G:(g + 1) * G]),
                                   (mixc[:, 1024:1024 + G], sin_d[:, g * G:(g + 1) * G]),
                                   (mixc[:, 2048:3072], kdec_d[:, :]),
                                   (mixc[:, 3072:3584], dT_d[:, :])], writes=[MIXC])

            def cosv(s):
                return T(mixc[:, s * 512:(s + 1) * 512], MIXC.keys)

            def sinv(s):
                return T(mixc[:, 1024 + s * 512:1024 + (s + 1) * 512], MIXC.keys)

            def rot_proj(col0, dst):
                for h in range(HEADS):
                    wt = wload(rwin_d[0, :, col0 + h * 256:col0 + (h + 1) * 256], KC, 256)
                    for hh in range(1):
                        for s in CS[0]:
                            ba = nb()
                            mm(ba, [(T(wt.ap[:, k, (hh * 2) * 128:(hh * 2 + 1) * 128], wt.keys), hv(k, s)) for k in range(KC)],
                               split=(h == 0 and col0 == D))
                            bb = nb()
                            mm(bb, [(T(wt.ap[:, k, (hh * 2 + 1) * 128:(hh * 2 + 2) * 128], wt.keys), hv(k, s)) for k in range(KC)])
                            t1, t2 = ntmp(), ntmp()
                            tt(t1, ba, cosv(s), ALU.mult)
                            tt(t2, bb, sinv(s), ALU.mult)
                            tt(dst(2 * h, s), t1, t2, ALU.subtract)
                            t3, t4 = ntmp(), ntmp()
                            tt(t3, bb, cosv(s), ALU.mult)
                            tt(t4, ba, sinv(s), ALU.mult)
                            tt(dst(2 * h + 1, s), t3, t4, ALU.add)
            mark("ret_k")
            CS[0] = allsubs
            rot_proj(D, kv)
            if not state_only:
                mark("ret_q")
                CS[0] = fsubs
                rot_proj(0, qv)
            CS[0] = allsubs
            mark("ret_v")
            for h in range(HEADS):
                wt = wload2(rwin_d[0, :, 2 * D + h * 512:2 * D + (h + 1) * 512])
                for ch in range(NCH):
                    bk = nb()
                    mm(bk, [(hvc(k, ch), T(wt.ap[:, k, :], wt.keys)) for k in range(KC)])
                    v_ = vv(ch)
                    act(T(v_.ap[:, h * 512:(h + 1) * 512], v_.keys), bk, AF.Copy)
            Skeys = lambda i: [("S32", i)]
            if not state_only:
                for i in range(8):
                    act(T(Sbf[:, i, :], [("Sbf", i)]), T(S32[:, i, :], Skeys(i)), AF.Copy)
            pending = [None]
            for ch in range(NCH):
                mark("ret_core%d" % ch)
                s_, cc = ch // 4, ch % 4
                v_ = vv(ch)
                tb = ntb()
                pe([("tr", tb.ap[:, c * 128:(c + 1) * 128], kv(c, s_).ap[:, cc * 128:(cc + 1) * 128], identb[:]) for c in range(KC)],
                   [kv(c, s_) for c in range(KC)] + [IDB], [tb])
                kdt = T(kd[:, 0, :], [("kd", 0)])
                tt(kdt, tb, T(mixc[:, 2048:3072], MIXC.keys), ALU.mult)
                full_ch = (not state_only) and (s_ in fsubs)
                if full_ch:
                    bs_ = nb()
                    instrs = []
                    for h in range(HEADS):
                        for i in range(2):
                            c = 2 * h + i
                            instrs.append(("mm", bs_.ap[:, h * 128:(h + 1) * 128], kv(c, s_).ap[:, cc * 128:(cc + 1) * 128],
                                           qv(c, s_).ap[:, cc * 128:(cc + 1) * 128], i == 0, i == 1))
                    pe(instrs, [kv(c, s_) for c in range(KC)] + [qv(c, s_) for c in range(KC)], [bs_])
                    sT = T(sTb[:, ch % 2, :], [("sTb", ch % 2)])
                    tt(sT, bs_, T(mixc[:, 3072:3584], MIXC.keys), ALU.mult)
                    obanks = []
                    for h in range(HEADS):
                        bo = nb()
                        instrs = [("mm", bo.ap, sT.ap[:, h * 128:(h + 1) * 128], v_.ap[:, h * 512:(h + 1) * 512], True, False)]
                        rd = [sT, v_]
                        for i in range(2):
                            c = 2 * h + i
                            instrs.append(("mm", bo.ap, qv(c, s_).ap[:, cc * 128:(cc + 1) * 128], Sbf[:, c, :], False, i == 1))
                            rd += [qv(c, s_), T(Sbf[:, c, :], [("Sbf", c)])]
                        pe(instrs, rd, [bo])
                        obanks.append(bo)
                if full_ch:
                    onb = []
                    for h in range(HEADS):
                        bo = obanks[h]
                        col = 32 + h * 4
                        ssq = T(st[:, col:col + 1], [("st", col)])
                        rr = T(st[:, col + 1:col + 2], [("st", col + 1)])
                        on = T(onbuf[:, (ch % 2) * 4 + h, :], [("on", (ch % 2) * 4 + h)])
                        act(on, bo, AF.Square, scale=qdec[:, h:h + 1], accum=ssq, extra_reads=[QDEC])
                        act(rr, ssq, AF.Ln, bias=EPSR, scale=1.0 / 512.0, extra_reads=[EPST])
                        act(rr, rr, AF.Exp, scale=-0.5)
                        tt(rr, rr, T(qdec[:, h:h + 1], QDEC.keys), ALU.mult)
                        act(on, bo, AF.Copy, scale=st[:, col + 1:col + 2], extra_reads=[rr])
                        onb.append(on)
                for c in range(8):
                    h = c // 2
                    bk = nb()
                    mm(bk, [(T(kdt.ap[:, c * 128:(c + 1) * 128], kdt.keys), T(v_.ap[:, h * 512:(h + 1) * 512], v_.keys))])
                    Sc = T(S32[:, c, :], Skeys(c))
                    stt(Sc, Sc, cdec[h], bk, ALU.mult, ALU.add)
                    if not state_only:
                        act(T(Sbf[:, c, :], [("Sbf", c)]), Sc, AF.Copy)
                if pending[0] is not None:
                    pending[0]()
                    pending[0] = None
                if not full_ch:
                    continue

                def do_tr(onb=onb, v_=v_):
                    for hp in range(2):
                        tb = ntb()
                        instrs = []
                        for hh in range(2):
                            for e4 in range(4):
                                instrs.append(("tr", tb.ap[:, (hh * 4 + e4) * 128:(hh * 4 + e4 + 1) * 128],
                                               onb[hp * 2 + hh].ap[:, e4 * 128:(e4 + 1) * 128], identb[:]))
                        pe(instrs, [onb[hp * 2], onb[hp * 2 + 1], IDB], [tb])
                        cp(T(v_.ap[:, hp * 1024:(hp + 1) * 1024], v_.keys), tb)
                pending[0] = do_tr
            if pending[0] is not None:
                pending[0]()
                pending[0] = None
            if state_only:
                return
            mark("ret_gate")
            CS[0] = fsubs
            def ov(c, s):
                ap = R[:, VO + s * 4 * 2048:VO + (s + 1) * 4 * 2048].rearrange("p (ch c t) -> p ch c t", ch=4, c=16)[:, :, c, :]
                return T(ap, rkeys(VO + s * 4 * 2048, 4 * 2048))
            for mg in range(8):
                wt = wload(rwin_d[0, :, 4 * D + mg * 256:4 * D + (mg + 1) * 256], KC, 256)

                def evac(bk, mi, s, mg=mg):
                    tm = ntmp()
                    act(tm, bk, AF.Silu)
                    o_ = ov(mg * 2 + mi, s)
                    tt(o_, T(tm.ap.rearrange("p (ch t) -> p ch t", ch=4), tm.keys), o_, ALU.mult)
                proj_fm(wt, 2, hv, KC, evac)
            out_proj(rwout_d[0], 16, ov)
            CS[0] = allsubs

        XO = RN - 4096
        for g in range(NG):
            if g < npre - 1:
                mode = "pre"
            elif g == npre - 1:
                mode = "pre_last"
            else:
                mode = "full"
            mark("G%d_%s_load" % (g, mode))
            for ch in range(NCH):
                sl = ch % 2
                xs = Rf(XO + sl * 2048, 1024)
                tok0 = g * G + ch * 128
                dma("sp", "xs%d" % sl, xs.ap, x_d[tok0:tok0 + 128, :], writes=[xs])
                for half in range(2):
                    bk = nb()
                    pe([("tr", bk.ap[:, j * 128:(j + 1) * 128], xs.ap[:, (half * 4 + j) * 128:(half * 4 + j + 1) * 128], ident[:])
                        for j in range(4)], [xs, IDF], [bk])
                    s_ = ch // 4
                    dst = T(xT[:, half * 4:half * 4 + 4, ch * 128:(ch + 1) * 128], [("x", half * 4 + j, s_) for j in range(4)])
                    rec.emit("act" if half else "dve",
                             (lambda e, d=dst, b=bk: e.activation(out=d.ap, in_=b.ap.rearrange("p (c t) -> p c t", c=4), func=AF.Copy)) if half else
                             (lambda e, d=dst, b=bk: e.tensor_copy(out=d.ap, in_=b.ap.rearrange("p (c t) -> p c t", c=4))),
                             [bk], [dst])
            for l in range(DBG_NL):
                kind = l % 3
                if mode == "pre" and l == 3:
                    break
                rmsnorm(l, hv)
                if kind == 0:
                    conv_mixer(l // 3, tail_only=(mode == "pre_last" and l == 3))
                    if mode == "pre_last" and l == 3:
                        break
                elif kind == 1:
                    sgu_mixer()
                else:
                    ret_mixer(g, state_only=(mode == "pre"), fsubs=([SUB - 1] if mode == "pre_last" else None))
                    if mode == "pre":
                        break
                    if mode == "pre_last":
                        CS[0] = [SUB - 1]
                if DBG_FFN:
                    rmsnorm(4 + l, hv)
                    ffn(l)
            CS[0] = list(range(SUB))
            if mode != "full":
                continue
            def fv(kc, s):
                return Rf((kc * SUB + s) * 1024, 512)
            mark("final")
            rmsnorm(8, fv)
            for ch in range(NCH):
                s_, cc = ch // 4, ch % 4
                sl = ch % 2
                os_ = Rf(XO + sl * 2048, 1024)
                for half in range(2):
                    bk = nb()
                    pe([("tr", bk.ap[:, j * 128:(j + 1) * 128], fv(half * 4 + j, s_).ap[:, cc * 128:(cc + 1) * 128], ident[:])
                        for j in range(4)], [fv(half * 4 + j, s_) for j in range(4)] + [IDF], [bk])
                    dst = T(os_.ap[:, half * 512:(half + 1) * 512], os_.keys)
                    if half:
                        act(dst, bk, AF.Copy)
                    else:
                        cp(dst, bk)
                tok0 = (g - npre) * G + ch * 128
                dma("sp", "os%d" % sl, y_d[tok0:tok0 + 128, :], os_.ap, reads=[os_])
        rec.final_wait("sp", ["os0", "os1"])
        with nc.Block() as block:
            rec.run(block)
    return nc


def _tables(positions):
    half = 128
    inv_freq = (10000.0 ** (-(np.arange(half, dtype=np.float32) / np.float32(half)))).astype(np.float32)
    ang = (positions.astype(np.float32)[None, :] * inv_freq[:, None]).astype(np.float32)
    return np.cos(ang).astype(np.float32), np.sin(ang).astype(np.float32)


def _const_tables():
    gam = 1.0 - 2.0 ** (-5.0 - np.arange(HEADS, dtype=np.float64))
    lg = np.log(gam)
    idx = np.arange(128, dtype=np.float64)
    dT = np.zeros((128, HEADS, 128), np.float64)
    for h in range(HEADS):
        kf = np.exp(-(idx + 1.0) * lg[h]) / 16.0
        dT[:, h, :] = (idx[:, None] <= idx[None, :]) * kf[:, None]
    kdec = np.zeros((128, HEADS, 256), np.float64)
    for h in range(HEADS):
        kdec[:, h, :] = (np.exp((127.0 - idx) * lg[h]) / 16.0)[:, None]
    qdec = np.exp((idx[:, None] + 1.0) * lg[None, :])
    tri = (idx[:, None] <= idx[None, :]).astype(np.float32)
    return (dT.reshape(128, 512).astype(np.float32), kdec.reshape(128, 1024).astype(np.float32),
            qdec.astype(np.float32), np.eye(128, dtype=np.float32), tri)


_NC_CACHE = {}


def run_trunk(inputs, seq, npre, nseg, SUB, n_cores):
    G = SUB * 512
    half = seq // 2
    assert npre * G == half and nseg * G == half
    key = (npre, nseg, SUB, DBG_NL, DBG_FFN)
    if key not in _NC_CACHE:
        _NC_CACHE[key] = build(npre, nseg, SUB)
    nc = _NC_CACHE[key]
    x = np.ascontiguousarray(np.asarray(inputs["x"], dtype=np.float32))
    dT, kdec, qdec, ident, tri = _const_tables()
    shared = {k: np.ascontiguousarray(np.asarray(v, dtype=np.float32)) for k, v in inputs.items() if k != "x"}
    shared.update({"t_dT": dT, "t_kdec": kdec, "t_qdec": qdec, "t_ident": ident, "t_tri": tri})
    in_maps = []
    for c in range(n_cores):
        s, p = c // 2, c % 2
        if p == 0:
            xc = np.concatenate([np.zeros((half, D), np.float32), x[s, :half]], axis=0)
            pos = np.arange(-half, half)
        else:
            xc = x[s]
            pos = np.arange(0, seq)
        cos, sin = _tables(pos)
        m = dict(shared)
        m.update({"x": np.ascontiguousarray(xc), "t_cos": cos, "t_sin": sin})
        in_maps.append(m)
    res = run_bass_kernel_spmd(nc, in_maps, core_ids=list(range(n_cores)))
    out = np.zeros((n_cores // 2, seq, D), np.float32)
    for c in range(n_cores):
        s, p = c // 2, c % 2
        out[s, p * half:(p + 1) * half] = res.results[c]["y"]
    return out


SUB_CFG = 2
DBG_NL = 4
MARKS = []
DBG_FFN = True


def kernel(**inputs):
    G = SUB_CFG * 512
    n = (SEQ // 2) // G
    return run_trunk(inputs, SEQ, n, n, SUB_CFG, 8)
```

```python
import numpy as np
from contextlib import ExitStack
import concourse.bass as bass
import concourse.mybir as mybir
from concourse.bass_utils import run_bass_kernel_spmd

F32 = mybir.dt.float32
BF16 = mybir.dt.bfloat16
AF = mybir.ActivationFunctionType
ALU = mybir.AluOpType

D = 1024
KC = 8
DFF = 2816
FC = 22
RMS_EPS = 1e-6
LN_EPS = 1e-5
HEADS = 4
SEQ = 8192
BATCH = 4
COMPUTE = ("pe", "act", "dve")


class T:
    __slots__ = ("ap", "keys")

    def __init__(self, ap, keys):
        self.ap = ap
        self.keys = tuple(keys)


class Rec:
    def __init__(self, nc, es):
        self.nc = nc
        self.es = es
        self.ops = {e: [] for e in ("pe", "act", "dve", "pool", "sp")}
        self.sem = {}
        self.cnt = {}
        for e in COMPUTE:
            self.sem[e] = es.enter_context(nc.semaphore("s_" + e))
            self.cnt[e] = 0
        self.waited = {}
        self.lastw = {}
        self.readers = {}

    def stream(self, name):
        self.sem[name] = self.es.enter_context(self.nc.semaphore(name))
        self.cnt[name] = 0
        return name

    def _need(self, waits, eng, prod, val):
        if prod == eng and eng == "pe":
            return
        k = (eng, prod)
        if self.waited.get(k, 0) >= val:
            return
        self.waited[k] = val
        waits.append((self.sem[prod], val * (1 if prod in COMPUTE else 16)))

    def emit(self, eng, fn, reads, writes, prod=None, n=1):
        prod = prod or eng
        waits = []
        for t in reads:
            for k in t.keys:
                lw = self.lastw.get(k)
                if lw:
                    self._need(waits, eng, *lw)
        for t in writes:
            for k in t.keys:
                lw = self.lastw.get(k)
                if lw:
                    self._need(waits, eng, *lw)
                for p, v in self.readers.get(k, {}).items():
                    self._need(waits, eng, p, v)
        self.cnt[prod] += n
        val = self.cnt[prod]
        for t in writes:
            for k in t.keys:
                self.lastw[k] = (prod, val)
                self.readers[k] = {}
        for t in reads:
            for k in t.keys:
                self.readers.setdefault(k, {})[prod] = val
        self.ops[eng].append((waits, fn, self.sem[prod], 1 if prod in COMPUTE else 16))

    def final_wait(self, eng, prods):
        waits = [(self.sem[p], self.cnt[p] * (1 if p in COMPUTE else 16)) for p in prods if self.cnt[p]]
        self.ops[eng].append((waits, None, None, 0))

    def run(self, block):
        def mk(e):
            def body(eng):
                for waits, fn, sem, inc in self.ops[e]:
                    for sm, v in waits:
                        eng.wait_ge(sm, v)
                    if fn is None:
                        continue
                    r = fn(eng)
                    if isinstance(r, (list, tuple)):
                        for ins in r:
                            ins.then_inc(sem, inc)
                    else:
                        r.then_inc(sem, inc)
            return body
        block.tensor(mk("pe"))
        block.scalar(mk("act"))
        block.vector(mk("dve"))
        block.gpsimd(mk("pool"))
        block.sync(mk("sp"))


def build(npre, nseg, SUB):
    G = SUB * 512
    NCH = G // 128
    NG = npre + nseg
    NTOK = NG * G
    RN = 16384 * SUB
    nc = bass.Bass("TRN2", target_bir_lowering=False)

    def din(name, shape):
        return nc.dram_tensor(name, list(shape), F32, kind="ExternalInput").ap()

    x_d = din("x", [NTOK, D])
    nmg_d = din("norm_mix_g", [4, D])
    nfg_d = din("norm_ffn_g", [4, D])
    fng_d = din("final_norm_g", [D])
    cwin_d = din("conv_w_in", [2, D, 3 * D])
    cw_d = din("conv_w", [2, 3, D])
    cwout_d = din("conv_w_out", [2, D, D])
    swin_d = din("sgu_w_in", [1, D, 4096])
    slg_d = din("sgu_ln_g", [1, 2048])
    slb_d = din("sgu_ln_b", [1, 2048])
    sws_d = din("sgu_w_s", [1, 8, 128, 128])
    sbs_d = din("sgu_b_s", [1, 8, 128])
    swout_d = din("sgu_w_out", [1, 2048, D])
    rwin_d = din("ret_w_in", [1, D, 6 * D])
    rwout_d = din("ret_w_out", [1, 2048, D])
    fwg_d = din("ffn_w_gate", [4, D, DFF])
    fwu_d = din("ffn_w_up", [4, D, DFF])
    fwd_d = din("ffn_w_down", [4, DFF, D])
    cos_d = din("t_cos", [128, NTOK])
    sin_d = din("t_sin", [128, NTOK])
    dT_d = din("t_dT", [128, 512])
    kdec_d = din("t_kdec", [128, 1024])
    qdec_d = din("t_qdec", [128, 4])
    id_d = din("t_ident", [128, 128])
    tri_d = din("t_tri", [128, 128])
    y_d = nc.dram_tensor("y", [nseg * G, D], F32, kind="ExternalOutput").ap()
    C_dram = nc.dram_tensor("c_scr", [128, 2048], F32).ap()

    gam = [1.0 - 2.0 ** (-5.0 - h) for h in range(HEADS)]
    cdec = [float(np.exp(128.0 * np.log(g_))) for g_ in gam]

    with ExitStack() as es:
        def sb(name, shape, dt):
            return es.enter_context(nc.sbuf_tensor(name, list(shape), dt))

        def ps(name, shape, dt):
            return es.enter_context(nc.psum_tensor(name, list(shape), dt))

        xT = sb("xT", [128, KC, G], F32)
        hT = sb("hT", [128, KC, G], BF16)
        R = sb("R", [128, RN], BF16)
        S32 = sb("S32", [128, 8, 512], F32)
        Sbf = sb("Sbf", [128, 8, 512], BF16)
        NW = 6
        wbuf = sb("wbuf", [128, NW, 2048], BF16)
        mixc = sb("mixc", [128, 4096], F32)
        sq = sb("sq", [128, 4, 512], BF16)
        rstd = sb("rstd", [128, 2, 512], F32)
        NTMP = 3
        tmp = sb("tmp", [128, NTMP, 512], F32)
        kd = sb("kd", [128, 2, 1024], BF16)
        sTb = sb("sTb", [128, 1, 512], BF16)
        vhb = sb("vhb", [128, 2, 2048], BF16) if SUB == 1 else None
        WT = sb("WT", [128, 8, 128], BF16)
        ident = sb("ident", [128, 128], F32)
        identb = sb("identb", [128, 128], BF16)
        onesb = sb("onesb", [128, 128], BF16)
        gT = sb("gT", [128, KC, 32], F32)
        halo = sb("halo", [128, 2, KC, 2], F32)
        qdec = sb("qdec", [128, 4], F32)
        st = sb("st", [128, 64], F32)
        onbuf = sb("onbuf", [128, 8, 512], BF16)

        NB = 8
        pb = [ps("pb%d" % i, [128, 512], F32) for i in range(NB)]

        rec = Rec(nc, es)
        for nm in ["w0", "w1", "w2", "w3", "w4", "w5", "xs0", "xs1", "os0", "os1", "cst", "mc", "cs"]:
            rec.stream(nm)

        def rkeys(off, n):
            return [("R", i) for i in range(off // 512, (off + n - 1) // 512 + 1)]

        def Rb(off, n):
            return T(R[:, off:off + n], rkeys(off, n))

        def Rf(off, n):
            return T(R[:, off:off + 2 * n].bitcast(F32), rkeys(off, 2 * n))

        bank_i = [0]

        def nb():
            i = bank_i[0]
            bank_i[0] = (i + 1) % NB
            return T(pb[i][:], [("pb", i)])

        tb_i = [0]

        def ntb():
            b = nb()
            return T(b.ap.bitcast(BF16), b.keys)

        tmp_i = [0]

        def ntmp():
            i = tmp_i[0]
            tmp_i[0] = (i + 1) % NTMP
            return T(tmp[:, i, :], [("tmp", i)])

        sq_i = [0]

        def nsq():
            i = sq_i[0]
            sq_i[0] = (i + 1) % 4
            return T(sq[:, i, :], [("sq", i)])

        def V(ap, *keys):
            return T(ap, keys)

        def sub_of(t0):
            return t0 // 512

        def xv(kc, s):
            return T(xT[:, kc, s * 512:(s + 1) * 512], [("x", kc, s)])

        def hv(kc, s):
            return T(hT[:, kc, s * 512:(s + 1) * 512], [("h", kc, s)])

        def hvc(kc, ch):
            return T(hT[:, kc, ch * 128:(ch + 1) * 128], [("h", kc, ch // 4)])

        PE_N = [0]
        CS = [list(range(SUB))]

        def mark(label):
            MARKS.append((PE_N[0], label))

        def pe(instrs, reads, writes):
            instrs = list(instrs)
            def fn(eng):
                last = None
                for it in instrs:
                    if it[0] == "mm":
                        _, o, l, r, s0, s1 = it
                        last = eng.matmul(out=o, lhsT=l, rhs=r, start=s0, stop=s1)
                    else:
                        _, o, i_, idn = it
                        last = eng.transpose(out=o, in_=i_, identity=idn)
                return last
            rec.emit("pe", fn, reads, writes)
            PE_N[0] += len(instrs)

        def mm(out, pairs, split=False):
            n = len(pairs)
            if split:
                for i, (l, r) in enumerate(pairs):
                    pe([("mm", out.ap, l.ap, r.ap, i == 0, i == n - 1)], [l, r], [out])
                return
            instrs = [("mm", out.ap, l.ap, r.ap, i == 0, i == n - 1) for i, (l, r) in enumerate(pairs)]
            reads = [t for p in pairs for t in p]
            pe(instrs, reads, [out])

        def act(out, in_, func, bias=None, scale=None, accum=None, extra_reads=()):
            kw = {}
            if bias is not None:
                kw["bias"] = bias
            if scale is not None:
                kw["scale"] = scale
            if accum is not None:
                kw["accum_out"] = accum.ap
            rec.emit("act", lambda e: e.activation(out=out.ap, in_=in_.ap, func=func, **kw),
                     [in_] + list(extra_reads), [out] + ([accum] if accum is not None else []))

        def tt(out, a, b, op, eng="dve"):
            rec.emit(eng, lambda e: e.tensor_tensor(out=out.ap, in0=a.ap, in1=b.ap, op=op), [a, b], [out])

        def ts(out, a, s1, s2, op0, op1=None, extra_reads=(), eng="dve"):
            if op1 is None:
                rec.emit(eng, lambda e: e.tensor_scalar(out=out.ap, in0=a.ap, scalar1=s1, scalar2=None, op0=op0),
                         [a] + list(extra_reads), [out])
            else:
                rec.emit(eng, lambda e: e.tensor_scalar(out=out.ap, in0=a.ap, scalar1=s1, scalar2=s2, op0=op0, op1=op1),
                         [a] + list(extra_reads), [out])

        def stt(out, a, sc, b, op0, op1, extra_reads=(), eng="dve"):
            rec.emit(eng, lambda e: e.scalar_tensor_tensor(out=out.ap, in0=a.ap, scalar=sc, in1=b.ap, op0=op0, op1=op1),
                     [a, b] + list(extra_reads), [out])

        def recip(out, in_):
            rec.emit("dve", lambda e: e.reciprocal(out=out.ap, in_=in_.ap), [in_], [out])

        def cp(out, in_, eng="dve"):
            rec.emit(eng, lambda e: e.tensor_copy(out=out.ap, in_=in_.ap), [in_], [out])

        def mset(out, val, eng="dve"):
            rec.emit(eng, lambda e: e.memset(out.ap, val), [], [out])

        def dma(eng, stream, out, in_, reads=(), writes=()):
            rec.emit(eng, lambda e: e.dma_start(out=out, in_=in_), list(reads), list(writes), prod=stream)

        def dma_multi(eng, stream, pairs, reads=(), writes=()):
            def fn(e):
                return [e.dma_start(out=o, in_=i) for o, i in pairs]
            rec.emit(eng, fn, list(reads), list(writes), prod=stream, n=len(pairs))

        w_i = [0]

        def wload(src2d, kcs, cols):
            i = w_i[0]
            w_i[0] = (i + 1) % NW
            dst = wbuf[:, i, 0:kcs * cols].rearrange("p (k m) -> p k m", k=kcs)
            t = T(dst, [("w", i)])
            dma("pool", "w%d" % i, dst, src2d.rearrange("(k p) m -> p k m", p=128), writes=[t])
            return t

        def wload2(src2d):
            if w_i[0] % 2:
                w_i[0] = (w_i[0] + 1) % NW
            i = w_i[0]
            w_i[0] = (i + 2) % NW
            dst = wbuf[:, i:i + 2, :].rearrange("p a (k m) -> p (a k) m", m=512)
            t = T(dst, [("w", i), ("w", i + 1)])
            dma("pool", "w%d" % i, dst, src2d.rearrange("(k p) m -> p k m", p=128), writes=[t])
            return t

        GT = T(gT[:], [("gT",)])
        IDF = T(ident[:], [("ident",)])
        IDB = T(identb[:], [("identb",)])
        ONESB = T(onesb[:], [("onesb",)])
        MIXC = T(mixc[:], [("mixc",)])
        QDEC = T(qdec[:], [("qdec",)])
        epst = sb("epst", [128, 2], F32)
        EPSR = epst[:, 0:1]
        EPSL = epst[:, 1:2]
        EPST = T(epst[:], [("eps", 0), ("eps", 1)])

        vst = Rf(0, 1024)
        vst32 = T(vst.ap[0:32, :], vst.keys)
        mset(vst32, 0.0)
        wsn = Rf(2048, 1024)
        bsb = Rf(4096, 1024)
        tri = Rf(6144, 128)
        onesf = Rf(6656, 128)
        WTf = Rf(7168, 1024)
        Cs = Rf(9216, 2048)
        pre = [
            (ident[:], id_d[:, :]),
            (qdec[:], qdec_d[:, :]),
            (vst.ap[0:4, :], nmg_d[:, :]),
            (vst.ap[4:8, :], nfg_d[:, :]),
            (vst.ap[8:9, :], fng_d.rearrange("(o d) -> o d", o=1)),
            (vst.ap[9:15, :], cw_d.rearrange("j k d -> (j k) d")),
            (vst.ap[16:18, :], slb_d.rearrange("o (r d) -> (o r) d", r=2)),
            (wsn.ap.rearrange("p (g s) -> p g s", g=8), sws_d.rearrange("o g t s -> t (o g) s")),
            (bsb.ap, sbs_d.rearrange("o g t -> o (g t)").partition_broadcast(128)),
            (tri.ap, tri_d[:, :]),
        ]
        rec.emit("sp", lambda e: [e.dma_start(out=o, in_=i) for o, i in pre], [vst32],
                 [IDF, QDEC, vst, wsn, bsb, tri], prod="cst", n=len(pre))
        cp(IDB, IDF)
        mset(ONESB, 1.0 / 1024.0)
        mset(onesf, 1.0)
        mset(T(epst[:, 0:1], [("eps", 0)]), RMS_EPS)
        mset(T(epst[:, 1:2], [("eps", 1)]), LN_EPS)
        mset(T(S32[:], [("S32", i) for i in range(8)]), 0.0)
        mset(T(Sbf[:], [("Sbf", i) for i in range(8)]), 0.0)
        mset(T(halo[:], [("halo", 0), ("halo", 1)]), 0.0)
        bk = nb()
        pe([("tr", bk.ap[:, kc * 32:(kc + 1) * 32], vst.ap[0:32, kc * 128:(kc + 1) * 128], ident[0:32, 0:32])
            for kc in range(KC)], [vst, IDF], [bk])
        act(T(gT[:].rearrange("p k r -> p (k r)"), GT.keys), T(bk.ap[:, 0:256], bk.keys), AF.Copy)
        for half in range(2):
            bk = nb()
            pe([("tr", bk.ap[:, j * 128:(j + 1) * 128], wsn.ap[:, (half * 4 + j) * 128:(half * 4 + j + 1) * 128], ident[:])
                for j in range(4)], [wsn, IDF], [bk])
            for j in range(4):
                g_ = half * 4 + j
                tt(T(WTf.ap[:, g_ * 128:(g_ + 1) * 128], WTf.keys), T(bk.ap[:, j * 128:(j + 1) * 128], bk.keys), tri, ALU.mult)
        cp(T(WT[:].rearrange("p g t -> p (g t)"), [("WT",)]), WTf)
        WTt = T(WT[:], [("WT",)])
        for half in range(2):
            bk = nb()
            pe([("mm", bk.ap[:, j * 128:(j + 1) * 128], onesf.ap, WTf.ap[:, (half * 4 + j) * 128:(half * 4 + j + 1) * 128], True, True)
                for j in range(4)], [onesf, WTf], [bk])
            for j in range(4):
                g_ = half * 4 + j
                for i in range(2):
                    dc = g_ * 2 + i
                    r_, kc_ = dc // 8, dc % 8
                    stt(T(Cs.ap[:, dc * 128:(dc + 1) * 128], Cs.keys), T(bk.ap[:, j * 128:(j + 1) * 128], bk.keys),
                        gT[:, kc_, 16 + r_:17 + r_], T(bsb.ap[:, g_ * 128:(g_ + 1) * 128], bsb.keys),
                        ALU.mult, ALU.add, extra_reads=[GT])
        CD = T(C_dram, [("Cdram",)])
        dma("sp", "cst", C_dram[:, :], Cs.ap, reads=[Cs], writes=[CD])

        def rmsnorm(col, out_fn, out_dtype_bf16=True):
            mark("norm%d" % col)
            for s in CS[0]:
                PST = nb()
                for kc in range(KC):
                    q_ = nsq()
                    act(q_, xv(kc, s), AF.Square)
                    pe([("mm", PST.ap, ONESB.ap, q_.ap, kc == 0, kc == KC - 1)], [ONESB, q_], [PST])
                rs = T(rstd[:, s % 2, :], [("rstd", s % 2)])
                act(rs, PST, AF.Ln, bias=EPSR, extra_reads=[EPST])
                act(rs, rs, AF.Exp, scale=-0.5)
                for kc in range(KC):
                    stt(out_fn(kc, s), xv(kc, s), gT[:, kc, col:col + 1], rs, ALU.mult, ALU.mult, extra_reads=[GT])

        def resid_add(bk, mc, s):
            tt(xv(mc, s), bk, xv(mc, s), ALU.add)

        def proj_fm(w_t, mcs, rhs_fn, nk, evac, first=False):
            for s in CS[0]:
                for mi in range(mcs):
                    bk = nb()
                    mm(bk, [(T(w_t.ap[:, k, mi * 128:(mi + 1) * 128], w_t.keys), rhs_fn(k, s)) for k in range(nk)],
                       split=(first and mi == 0))
                    evac(bk, mi, s)

        def proj_acc(w_fn, parts, ncol_groups, rhs_fn, evac):
            for mg in range(ncol_groups):
                banks = [{s: nb() for s in CS[0]} for mi in range(2)]
                for pi, (kc0, nkc) in enumerate(parts):
                    wt = wload(w_fn(kc0, nkc, mg), nkc, 256)
                    for mi in range(2):
                        for s in CS[0]:
                            instrs = [("mm", banks[mi][s].ap, wt.ap[:, k, mi * 128:(mi + 1) * 128], rhs_fn(kc0 + k, s).ap,
                                       pi == 0 and k == 0, pi == len(parts) - 1 and k == nkc - 1) for k in range(nkc)]
                            pe(instrs, [wt] + [rhs_fn(kc0 + k, s) for k in range(nkc)], [banks[mi][s]])
                for mi in range(2):
                    for s in CS[0]:
                        evac(banks[mi][s], mg * 2 + mi, s)

        def ffn(l):
            mark("ffn_in")
            def actv(c, s):
                return Rb((c * SUB + s) * 512, 512)
            for g0 in range(0, 11, 2):
              grp = list(range(g0, min(g0 + 2, 11)))
              wts = {mg: (wload(fwg_d[l, :, mg * 256:(mg + 1) * 256], KC, 256),
                          wload(fwu_d[l, :, mg * 256:(mg + 1) * 256], KC, 256)) for mg in grp}
              for s in CS[0]:
                for mg in grp:
                    wg, wu = wts[mg]
                    for mi in range(2):
                        ba = nb()
                        mm(ba, [(T(wg.ap[:, k, mi * 128:(mi + 1) * 128], wg.keys), hv(k, s)) for k in range(KC)],
                           split=(mg == 0 and mi == 0))
                        bu = nb()
                        mm(bu, [(T(wu.ap[:, k, mi * 128:(mi + 1) * 128], wu.keys), hv(k, s)) for k in range(KC)])
                        tm = ntmp()
                        act(tm, ba, AF.Silu)
                        tt(actv(mg * 2 + mi, s), tm, bu, ALU.mult)
            mark("ffn_down")
            proj_acc(lambda kc0, nkc, mg: fwd_d[l, kc0 * 128:(kc0 + nkc) * 128, mg * 256:(mg + 1) * 256],
                     [(0, 6), (6, 5), (11, 6), (17, 5)], 4, actv, resid_add)

        def out_proj(w_d2, nk, rhs_fn):
            mark("out_proj")
            proj_acc(lambda kc0, nkc, mg: w_d2[kc0 * 128:(kc0 + nkc) * 128, mg * 256:(mg + 1) * 256],
                     [(p * 8, 8) for p in range(nk // 8)], 4, rhs_fn, resid_add)

        def conv_mixer(j, tail_only=False):
            CZS = 2 * (G + 4)
            mark("conv_in")

            def czv(fc, c0, n):
                off = fc * CZS + 2 * c0
                return Rf(off, n)
            YO = KC * CZS

            def yv(kc, s):
                return Rb(YO + (kc * SUB + s) * 512, 512)
            HK = T(halo[:, j, :, :], [("halo", j)])
            for fc in range(KC):
                cp(czv(fc, 2, 2), T(halo[:, j, fc, :], HK.keys), eng="act" if False else "dve")
            for fg in range(4):
                wc = wload(cwin_d[j, :, D + fg * 256:D + (fg + 1) * 256], KC, 256)
                wz = wload(cwin_d[j, :, 2 * D + fg * 256:2 * D + (fg + 1) * 256], KC, 256)
                wb = None if tail_only else wload(cwin_d[j, :, fg * 256:(fg + 1) * 256], KC, 256)
                for s in CS[0]:
                    for mi in range(2):
                        fc = fg * 2 + mi
                        bc = nb()
                        mm(bc, [(T(wc.ap[:, k, mi * 128:(mi + 1) * 128], wc.keys), hv(k, s)) for k in range(KC)],
                           split=(fg == 0 and mi == 0))
                        bz = nb()
                        mm(bz, [(T(wz.ap[:, k, mi * 128:(mi + 1) * 128], wz.keys), hv(k, s)) for k in range(KC)])
                        tm = ntmp()
                        act(tm, bc, AF.Copy)
                        tt(czv(fc, 4 + s * 512, 512), tm, bz, ALU.mult)
                        if tail_only:
                            continue
                        bb = nb()
                        mm(bb, [(T(wb.ap[:, k, mi * 128:(mi + 1) * 128], wb.keys), hv(k, s)) for k in range(KC)])
                        t1 = ntmp()
                        c0 = 4 + s * 512
                        ts(t1, czv(fc, c0, 512), gT[:, fc, 9 + j * 3 + 2:9 + j * 3 + 3], None, ALU.mult, extra_reads=[GT])
                        stt(t1, czv(fc, c0 - 1, 512), gT[:, fc, 9 + j * 3 + 1:9 + j * 3 + 2], t1, ALU.mult, ALU.add, extra_reads=[GT])
                        stt(t1, czv(fc, c0 - 2, 512), gT[:, fc, 9 + j * 3:9 + j * 3 + 1], t1, ALU.mult, ALU.add, extra_reads=[GT])
                        tt(yv(fc, s), t1, bb, ALU.mult)
            for fc in range(KC):
                cp(T(halo[:, j, fc, :], HK.keys), czv(fc, 2 + G, 2))
            if not tail_only:
                out_proj(cwout_d[j], KC, yv)

        def sgu_mixer():
            def uv(c, s):
                return Rb((c * SUB + s) * 512, 512)
            NVS = 2
            VO = 16 * SUB * 512

            def vtok(slot):
                return Rf(VO + slot * 4096, 2048)
            dma_multi("sp", "mc", [(mixc[:, 0:2048], C_dram[:, :]),
                                   (mixc[:, 2048:4096], slg_d[0:1, :].partition_broadcast(128))],
                      reads=[CD], writes=[MIXC])
            mark("sgu_u")
            for mg in range(8):
                wt = wload(swin_d[0, :, mg * 256:(mg + 1) * 256], KC, 256)
                proj_fm(wt, 2, hv, KC, lambda bk, mi, s, mg=mg: act(uv(mg * 2 + mi, s), bk, AF.Gelu), first=(mg == 0))
            def vh_of(ch):
                return T(vhb[:, ch % 2, :], [("vhb", ch % 2)]) if SUB == 1 else Rb(24576 + (ch % 2) * 2048, 2048)

            def vproj(q0):
                mark("sgu_v%d" % q0)
                for vt in range(4):
                    wt = wload2(swin_d[0, :, 2048 + vt * 512:2048 + (vt + 1) * 512])
                    for ci in range(NVS):
                        ch = q0 + ci
                        bk = nb()
                        mm(bk, [(hvc(k, ch), T(wt.ap[:, k, :], wt.keys)) for k in range(KC)])
                        vt_t = vtok(ci)
                        col = ci * 16 + vt
                        act(T(vt_t.ap[:, vt * 512:(vt + 1) * 512], vt_t.keys), bk, AF.Gelu,
                            accum=T(st[:, col:col + 1], [("st", col)]))

            def chain(q0):
                for ci in range(NVS):
                    ch = q0 + ci
                    vt_t = vtok(ci)
                    b0 = ci * 16
                    STc = lambda a, n=1, b0=b0: T(st[:, b0 + a:b0 + a + n], [("st", b0 + a + i_) for i_ in range(n)])
                    vh = vh_of(ch)
                    act(vh, vt_t, AF.Square, accum=STc(4))
                    rec.emit("dve", lambda e, a=STc(5), b=STc(0, 4): e.tensor_reduce(out=a.ap, in_=b.ap, axis=mybir.AxisListType.X, op=ALU.add),
                             [STc(0, 4)], [STc(5)])
                    ts(STc(5), STc(5), 1.0 / 2048.0, None, ALU.mult)
                    tt(STc(6), STc(5), STc(5), ALU.mult)
                    stt(STc(7), STc(4), 1.0 / 2048.0, STc(6), ALU.mult, ALU.subtract)
                    act(STc(7), STc(7), AF.Ln, bias=EPSL, extra_reads=[EPST])
                    act(STc(7), STc(7), AF.Exp, scale=-0.5)
                    stt(STc(8), STc(5), -1.0, STc(7), ALU.mult, ALU.mult)
                    act(vt_t, vt_t, AF.Identity, bias=st[:, b0 + 8:b0 + 9], scale=st[:, b0 + 7:b0 + 8], extra_reads=[STc(7), STc(8)])
                    tt(vh, vt_t, T(mixc[:, 2048:4096], MIXC.keys), ALU.mult)

            def spatial(q0):
                for ci in range(NVS):
                    ch = q0 + ci
                    vh = vh_of(ch)
                    s_ = ch // 4
                    cc = ch % 4
                    for b4 in range(4):
                        bk = nb()
                        pe([("mm", bk.ap[:, i_ * 128:(i_ + 1) * 128], vh.ap[:, (b4 * 4 + i_) * 128:(b4 * 4 + i_ + 1) * 128],
                             WT[:, (b4 * 4 + i_) // 2, :], True, True) for i_ in range(4)], [vh, WTt], [bk])
                        tm = ntmp()
                        tt(tm, bk, T(mixc[:, b4 * 512:(b4 + 1) * 512], MIXC.keys), ALU.add)
                        tm3 = T(tm.ap.rearrange("p (c t) -> p c t", c=4), tm.keys)
                        u0 = b4 * 4 * SUB * 512
                        u4 = T(R[:, u0:u0 + 4 * SUB * 512].rearrange("p (c s t) -> p c s t", c=4, s=SUB)[:, :, s_, cc * 128:(cc + 1) * 128],
                               rkeys(u0, 4 * SUB * 512))
                        tt(u4, tm3, u4, ALU.mult)

            qs = list(range(0, NCH, NVS))
            vproj(qs[0])
            chain(qs[0])
            for qi in range(1, len(qs)):
                vproj(qs[qi])
                spatial(qs[qi - 1])
                chain(qs[qi])
            spatial(qs[-1])
            out_proj(swout_d[0], 16, uv)

        def ret_mixer(g, state_only, fsubs=None):
            allsubs = list(range(SUB))
            fsubs = allsubs if fsubs is None else fsubs
            def qv(c, s):
                return Rb((c * SUB + s) * 512, 512)

            def kv(c, s):
                return Rb(KC * SUB * 512 + (c * SUB + s) * 512, 512)
            VO = 2 * KC * SUB * 512

            def vv(ch):
                return Rb(VO + ch * 2048, 2048)
            dma_multi("sp", "mc", [(mixc[:, 0:G], cos_d[:, g * G:(g + 1) * G]),
                                   (mixc[:, 1024:1024 + G], sin_d[:, g * G:(g + 1) * G]),
                                   (mixc[:, 2048:3072], kdec_d[:, :]),
                                   (mixc[:, 3072:3584], dT_d[:, :])], writes=[MIXC])

            def cosv(s):
                return T(mixc[:, s * 512:(s + 1) * 512], MIXC.keys)

            def sinv(s):
                return T(mixc[:, 1024 + s * 512:1024 + (s + 1) * 512], MIXC.keys)

            def rot_proj(col0, dst):
                for h in range(HEADS):
                    wt = wload(rwin_d[0, :, col0 + h * 256:col0 + (h + 1) * 256], KC, 256)
                    for hh in range(1):
                        for s in CS[0]:
                            ba = nb()
                            mm(ba, [(T(wt.ap[:, k, (hh * 2) * 128:(hh * 2 + 1) * 128], wt.keys), hv(k, s)) for k in range(KC)],
                               split=(h == 0 and col0 == D))
                            bb = nb()
                            mm(bb, [(T(wt.ap[:, k, (hh * 2 + 1) * 128:(hh * 2 + 2) * 128], wt.keys), hv(k, s)) for k in range(KC)])
                            t1, t2 = ntmp(), ntmp()
                            tt(t1, ba, cosv(s), ALU.mult)
                            tt(t2, bb, sinv(s), ALU.mult)
                            tt(dst(2 * h, s), t1, t2, ALU.subtract)
                            t3, t4 = ntmp(), ntmp()
                            tt(t3, bb, cosv(s), ALU.mult)
                            tt(t4, ba, sinv(s), ALU.mult)
                            tt(dst(2 * h + 1, s), t3, t4, ALU.add)
            mark("ret_k")
            CS[0] = allsubs
            rot_proj(D, kv)
            if not state_only:
                mark("ret_q")
                CS[0] = fsubs
                rot_proj(0, qv)
            CS[0] = allsubs
            mark("ret_v")
            for h in range(HEADS):
                wt = wload2(rwin_d[0, :, 2 * D + h * 512:2 * D + (h + 1) * 512])
                for ch in range(NCH):
                    bk = nb()
                    mm(bk, [(hvc(k, ch), T(wt.ap[:, k, :], wt.keys)) for k in range(KC)])
                    v_ = vv(ch)
                    act(T(v_.ap[:, h * 512:(h + 1) * 512], v_.keys), bk, AF.Copy)
            Skeys = lambda i: [("S32", i)]
            if not state_only:
                for i in range(8):
                    act(T(Sbf[:, i, :], [("Sbf", i)]), T(S32[:, i, :], Skeys(i)), AF.Copy)
            pending = [None]
            for ch in range(NCH):
                mark("ret_core%d" % ch)
                s_, cc = ch // 4, ch % 4
                v_ = vv(ch)
                tb = ntb()
                pe([("tr", tb.ap[:, c * 128:(c + 1) * 128], kv(c, s_).ap[:, cc * 128:(cc + 1) * 128], identb[:]) for c in range(KC)],
                   [kv(c, s_) for c in range(KC)] + [IDB], [tb])
                kdt = T(kd[:, ch % 2, :], [("kd", ch % 2)])
                tt(kdt, tb, T(mixc[:, 2048:3072], MIXC.keys), ALU.mult)
                full_ch = (not state_only) and (s_ in fsubs)
                if full_ch:
                    bs_ = nb()
                    instrs = []
                    for h in range(HEADS):
                        for i in range(2):
                            c = 2 * h + i
                            instrs.append(("mm", bs_.ap[:, h * 128:(h + 1) * 128], kv(c, s_).ap[:, cc * 128:(cc + 1) * 128],
                                           qv(c, s_).ap[:, cc * 128:(cc + 1) * 128], i == 0, i == 1))
                    pe(instrs, [kv(c, s_) for c in range(KC)] + [qv(c, s_) for c in range(KC)], [bs_])
                    sT = T(sTb[:, 0, :], [("sTb", 0)])
                    tt(sT, bs_, T(mixc[:, 3072:3584], MIXC.keys), ALU.mult)
                    obanks = []
                    for h in range(HEADS):
                        bo = nb()
                        instrs = [("mm", bo.ap, sT.ap[:, h * 128:(h + 1) * 128], v_.ap[:, h * 512:(h + 1) * 512], True, False)]
                        rd = [sT, v_]
                        for i in range(2):
                            c = 2 * h + i
                            instrs.append(("mm", bo.ap, qv(c, s_).ap[:, cc * 128:(cc + 1) * 128], Sbf[:, c, :], False, i == 1))
                            rd += [qv(c, s_), T(Sbf[:, c, :], [("Sbf", c)])]
                        pe(instrs, rd, [bo])
                        obanks.append(bo)
                if full_ch:
                    onb = []
                    for h in range(HEADS):
                        bo = obanks[h]
                        col = 32 + h * 4
                        ssq = T(st[:, col:col + 1], [("st", col)])
                        rr = T(st[:, col + 1:col + 2], [("st", col + 1)])
                        on = T(onbuf[:, (ch % 2) * 4 + h, :], [("on", (ch % 2) * 4 + h)])
                        act(on, bo, AF.Square, scale=qdec[:, h:h + 1], accum=ssq, extra_reads=[QDEC])
                        act(rr, ssq, AF.Ln, bias=EPSR, scale=1.0 / 512.0, extra_reads=[EPST])
                        act(rr, rr, AF.Exp, scale=-0.5)
                        tt(rr, rr, T(qdec[:, h:h + 1], QDEC.keys), ALU.mult)
                        act(on, bo, AF.Copy, scale=st[:, col + 1:col + 2], extra_reads=[rr])
                        onb.append(on)
                for c in range(8):
                    h = c // 2
                    bk = nb()
                    mm(bk, [(T(kdt.ap[:, c * 128:(c + 1) * 128], kdt.keys), T(v_.ap[:, h * 512:(h + 1) * 512], v_.keys))])
                    Sc = T(S32[:, c, :], Skeys(c))
                    stt(Sc, Sc, cdec[h], bk, ALU.mult, ALU.add)
                    if not state_only:
                        act(T(Sbf[:, c, :], [("Sbf", c)]), Sc, AF.Copy)
                if pending[0] is not None:
                    pending[0]()
                    pending[0] = None
                if not full_ch:
                    continue

                def do_tr(onb=onb, v_=v_):
                    for hp in range(2):
                        tb = ntb()
                        instrs = []
                        for hh in range(2):
                            for e4 in range(4):
                                instrs.append(("tr", tb.ap[:, (hh * 4 + e4) * 128:(hh * 4 + e4 + 1) * 128],
                                               onb[hp * 2 + hh].ap[:, e4 * 128:(e4 + 1) * 128], identb[:]))
                        pe(instrs, [onb[hp * 2], onb[hp * 2 + 1], IDB], [tb])
                        cp(T(v_.ap[:, hp * 1024:(hp + 1) * 1024], v_.keys), tb)
                pending[0] = do_tr
            if pending[0] is not None:
                pending[0]()
                pending[0] = None
            if state_only:
                return
            mark("ret_gate")
            CS[0] = fsubs
            def ov(c, s):
                ap = R[:, VO + s * 4 * 2048:VO + (s + 1) * 4 * 2048].rearrange("p (ch c t) -> p ch c t", ch=4, c=16)[:, :, c, :]
                return T(ap, rkeys(VO + s * 4 * 2048, 4 * 2048))
            for mg in range(8):
                wt = wload(rwin_d[0, :, 4 * D + mg * 256:4 * D + (mg + 1) * 256], KC, 256)

                def evac(bk, mi, s, mg=mg):
                    tm = ntmp()
                    act(tm, bk, AF.Silu)
                    o_ = ov(mg * 2 + mi, s)
                    tt(o_, T(tm.ap.rearrange("p (ch t) -> p ch t", ch=4), tm.keys), o_, ALU.mult)
                proj_fm(wt, 2, hv, KC, evac)
            out_proj(rwout_d[0], 16, ov)
            CS[0] = allsubs

        XO = RN - 4096
        for g in range(NG):
            if g < npre - 1:
                mode = "pre"
            elif g == npre - 1:
                mode = "pre_last"
            else:
                mode = "full"
            mark("G%d_%s_load" % (g, mode))
            for ch in range(NCH):
                sl = ch % 2
                xs = Rf(XO + sl * 2048, 1024)
                tok0 = g * G + ch * 128
                dma("sp", "xs%d" % sl, xs.ap, x_d[tok0:tok0 + 128, :], writes=[xs])
                for half in range(2):
                    bk = nb()
                    pe([("tr", bk.ap[:, j * 128:(j + 1) * 128], xs.ap[:, (half * 4 + j) * 128:(half * 4 + j + 1) * 128], ident[:])
                        for j in range(4)], [xs, IDF], [bk])
                    s_ = ch // 4
                    dst = T(xT[:, half * 4:half * 4 + 4, ch * 128:(ch + 1) * 128], [("x", half * 4 + j, s_) for j in range(4)])
                    rec.emit("act" if half else "dve",
                             (lambda e, d=dst, b=bk: e.activation(out=d.ap, in_=b.ap.rearrange("p (c t) -> p c t", c=4), func=AF.Copy)) if half else
                             (lambda e, d=dst, b=bk: e.tensor_copy(out=d.ap, in_=b.ap.rearrange("p (c t) -> p c t", c=4))),
                             [bk], [dst])
            for l in range(DBG_NL):
                kind = l % 3
                if mode == "pre" and l == 3:
                    break
                rmsnorm(l, hv)
                if kind == 0:
                    conv_mixer(l // 3, tail_only=(mode == "pre_last" and l == 3))
                    if mode == "pre_last" and l == 3:
                        break
                elif kind == 1:
                    sgu_mixer()
                else:
                    ret_mixer(g, state_only=(mode == "pre"), fsubs=([SUB - 1] if mode == "pre_last" else None))
                    if mode == "pre":
                        break
                    if mode == "pre_last":
                        CS[0] = [SUB - 1]
                if DBG_FFN:
                    rmsnorm(4 + l, hv)
                    ffn(l)
            CS[0] = list(range(SUB))
            if mode != "full":
                continue
            def fv(kc, s):
                return Rf((kc * SUB + s) * 1024, 512)
            mark("final")
            rmsnorm(8, fv)
            for ch in range(NCH):
                s_, cc = ch // 4, ch % 4
                sl = ch % 2
                os_ = Rf(XO + sl * 2048, 1024)
                for half in range(2):
                    bk = nb()
                    pe([("tr", bk.ap[:, j * 128:(j + 1) * 128], fv(half * 4 + j, s_).ap[:, cc * 128:(cc + 1) * 128], ident[:])
                        for j in range(4)], [fv(half * 4 + j, s_) for j in range(4)] + [IDF], [bk])
                    dst = T(os_.ap[:, half * 512:(half + 1) * 512], os_.keys)
                    if half:
                        act(dst, bk, AF.Copy)
                    else:
                        cp(dst, bk)
                tok0 = (g - npre) * G + ch * 128
                dma("sp", "os%d" % sl, y_d[tok0:tok0 + 128, :], os_.ap, reads=[os_])
        rec.final_wait("sp", ["os0", "os1"])
        with nc.Block() as block:
            rec.run(block)
    return nc


def _tables(positions):
    half = 128
    inv_freq = (10000.0 ** (-(np.arange(half, dtype=np.float32) / np.float32(half)))).astype(np.float32)
    ang = (positions.astype(np.float32)[None, :] * inv_freq[:, None]).astype(np.float32)
    return np.cos(ang).astype(np.float32), np.sin(ang).astype(np.float32)


def _const_tables():
    gam = 1.0 - 2.0 ** (-5.0 - np.arange(HEADS, dtype=np.float64))
    lg = np.log(gam)
    idx = np.arange(128, dtype=np.float64)
    dT = np.zeros((128, HEADS, 128), np.float64)
    for h in range(HEADS):
        kf = np.exp(-(idx + 1.0) * lg[h]) / 16.0
        dT[:, h, :] = (idx[:, None] <= idx[None, :]) * kf[:, None]
    kdec = np.zeros((128, HEADS, 256), np.float64)
    for h in range(HEADS):
        kdec[:, h, :] = (np.exp((127.0 - idx) * lg[h]) / 16.0)[:, None]
    qdec = np.exp((idx[:, None] + 1.0) * lg[None, :])
    tri = (idx[:, None] <= idx[None, :]).astype(np.float32)
    return (dT.reshape(128, 512).astype(np.float32), kdec.reshape(128, 1024).astype(np.float32),
            qdec.astype(np.float32), np.eye(128, dtype=np.float32), tri)


_NC_CACHE = {}


def run_trunk(inputs, seq, npre, nseg, SUB, n_cores):
    G = SUB * 512
    half = seq // 2
    assert npre * G == half and nseg * G == half
    key = (npre, nseg, SUB, DBG_NL, DBG_FFN)
    if key not in _NC_CACHE:
        _NC_CACHE[key] = build(npre, nseg, SUB)
    nc = _NC_CACHE[key]
    x = np.ascontiguousarray(np.asarray(inputs["x"], dtype=np.float32))
    dT, kdec, qdec, ident, tri = _const_tables()
    shared = {k: np.ascontiguousarray(np.asarray(v, dtype=np.float32)) for k, v in inputs.items() if k != "x"}
    shared.update({"t_dT": dT, "t_kdec": kdec, "t_qdec": qdec, "t_ident": ident, "t_tri": tri})
    in_maps = []
    for c in range(n_cores):
        s, p = c // 2, c % 2
        if p == 0:
            xc = np.concatenate([np.zeros((half, D), np.float32), x[s, :half]], axis=0)
            pos = np.arange(-half, half)
        else:
            xc = x[s]
            pos = np.arange(0, seq)
        cos, sin = _tables(pos)
        m = dict(shared)
        m.update({"x": np.ascontiguousarray(xc), "t_cos": cos, "t_sin": sin})
        in_maps.append(m)
    res = run_bass_kernel_spmd(nc, in_maps, core_ids=list(range(n_cores)))
    out = np.zeros((n_cores // 2, seq, D), np.float32)
    for c in range(n_cores):
        s, p = c // 2, c % 2
        out[s, p * half:(p + 1) * half] = res.results[c]["y"]
    return out


SUB_CFG = 2
DBG_NL = 4
MARKS = []
DBG_FFN = True


def kernel(**inputs):
    G = SUB_CFG * 512
    n = (SEQ // 2) // G
    return run_trunk(inputs, SEQ, n, n, SUB_CFG, 8)
```
